# Optimizing a Trainium2 kernel written in Bass

```python
import jax, jax.numpy as jnp
from jax import lax
import numpy as np

D_MODEL = 2048
BATCH = 1
SEQ = 8192
DEPTH = 1

PLE_DIM = 256
RMS_EPS = 1e-6
N_HEADS = 16
QK_NOPE_DIM = 128
QK_ROPE_DIM = 64
V_HEAD_DIM = 128
QK_HEAD_DIM = QK_NOPE_DIM + QK_ROPE_DIM
Q_LORA_RANK = 768
KV_LORA_RANK = 512
ROPE_THETA = 10000.0
Q_BLOCK = 128
D_RNN = D_MODEL
RNN_BLOCKS = 16
RNN_BLOCK_DIM = D_RNN // RNN_BLOCKS
CONV_WIDTH = 4
LRU_C = 8.0
PEER_HEADS = 8
N_KEYS = 128
N_EXPERTS = N_KEYS * N_KEYS
PEER_QUERY_DIM = 256
PEER_HALF = PEER_QUERY_DIM // 2
PEER_TOPK = 16
PEER_CHUNK = 128
IN_SPLITS = (Q_LORA_RANK, KV_LORA_RANK, QK_ROPE_DIM, D_RNN, D_RNN, D_MODEL, D_MODEL)
IN_WIDTH = Q_LORA_RANK + KV_LORA_RANK + QK_ROPE_DIM + 2 * D_RNN + 2 * D_MODEL

kernel_name = 'hybrid_mla_rglru_peer_block'


def rms_norm(x, g):
    xf = x.astype(jnp.float32)
    y = xf * lax.rsqrt(jnp.mean(xf * xf, axis=-1, keepdims=True) + RMS_EPS)
    return (y * g.astype(jnp.float32)).astype(x.dtype)


def rope(x, pos):
    half = QK_ROPE_DIM // 2
    inv_freq = 1.0 / (ROPE_THETA ** (jnp.arange(half, dtype=jnp.float32) / half))
    ang = pos.astype(jnp.float32)[:, None] * inv_freq[None, :]
    cos = jnp.cos(ang)[:, None, :]
    sin = jnp.sin(ang)[:, None, :]
    xf = x.astype(jnp.float32)
    x1, x2 = xf[..., :half], xf[..., half:]
    return jnp.concatenate([x1 * cos - x2 * sin, x2 * cos + x1 * sin], axis=-1).astype(x.dtype)


def split_columns(z):
    offs, acc = [], 0
    for w in IN_SPLITS[:-1]:
        acc += w
        offs.append(acc)
    return jnp.split(z, offs, axis=-1)


def mla_branch(c_q, c_kv, k_r, q_norm, w_uq, kv_norm, w_ukv, w_attn_o):
    B, S, _ = c_q.shape
    pos = jnp.arange(S)
    q = (rms_norm(c_q, q_norm) @ w_uq).reshape(B, S, N_HEADS, QK_HEAD_DIM)
    q_nope = q[..., :QK_NOPE_DIM]
    q_pe = rope(q[..., QK_NOPE_DIM:], pos)
    kv = (rms_norm(c_kv, kv_norm) @ w_ukv).reshape(B, S, N_HEADS, QK_NOPE_DIM + V_HEAD_DIM)
    k_nope, v = kv[..., :QK_NOPE_DIM], kv[..., QK_NOPE_DIM:]
    k_pe = rope(k_r[:, :, None, :], pos)[:, :, 0, :]
    scale = QK_HEAD_DIM ** -0.5
    n_blk = S // Q_BLOCK

    def to_blocks(t):
        return t.reshape(B, n_blk, Q_BLOCK, *t.shape[2:]).swapaxes(0, 1)

    def attend(args):
        qn, qp, blk = args
        s = (jnp.einsum('bqhd,bkhd->bhqk', qn, k_nope, preferred_element_type=jnp.float32)
             + jnp.einsum('bqhd,bkd->bhqk', qp, k_pe, preferred_element_type=jnp.float32))
        q_pos = blk * Q_BLOCK + jnp.arange(Q_BLOCK)
        s = jnp.where(pos[None, :] <= q_pos[:, None], s * scale, -jnp.inf)
        prob = jax.nn.softmax(s, axis=-1)
        return jnp.einsum('bhqk,bkhd->bqhd', prob.astype(v.dtype), v)

    o = lax.map(attend, (to_blocks(q_nope), to_blocks(q_pe), jnp.arange(n_blk)))
    o = o.swapaxes(0, 1).reshape(B, S, N_HEADS * V_HEAD_DIM)
    return o @ w_attn_o


def rglru_branch(x_r, y_r, conv_w, conv_b, w_rg_a, b_rg_a, w_rg_x, b_rg_x, lru_lambda, w_rnn_o):
    B, S, C = x_r.shape
    xc = lax.conv_general_dilated(x_r, conv_w[:, None, :], window_strides=(1,),
                                  padding=[(CONV_WIDTH - 1, 0)],
                                  dimension_numbers=('NWC', 'WIO', 'NWC'),
                                  feature_group_count=C) + conv_b
    xb = xc.reshape(B, S, RNN_BLOCKS, RNN_BLOCK_DIM)
    r = jax.nn.sigmoid(jnp.einsum('bshi,hij->bshj', xb, w_rg_a) + b_rg_a).reshape(B, S, C)
    i = jax.nn.sigmoid(jnp.einsum('bshi,hij->bshj', xb, w_rg_x) + b_rg_x).reshape(B, S, C)
    log_a = -LRU_C * r.astype(jnp.float32) * jax.nn.softplus(-lru_lambda.astype(jnp.float32))
    a = jnp.exp(log_a)
    mult = jnp.sqrt(-jnp.expm1(2.0 * log_a))
    mult = jnp.where((jnp.arange(S) == 0)[None, :, None], 1.0, mult)
    b = mult * (i * xc).astype(jnp.float32)

    def combine(left, right):
        a1, b1 = left
        a2, b2 = right
        return a1 * a2, a2 * b1 + b2

    _, h = lax.associative_scan(combine, (a, b), axis=1)
    y = h.astype(x_r.dtype) * jax.nn.gelu(y_r)
    return y @ w_rnn_o


def peer_ffn(x, w_peer_q, peer_subkeys, peer_u, peer_v):
    B, S, D = x.shape
    q = (x @ w_peer_q).reshape(B, S, PEER_HEADS, 2, PEER_HALF)
    s = jnp.einsum('bshcd,hcnd->bshcn', q, peer_subkeys, preferred_element_type=jnp.float32)
    top_s, top_i = lax.top_k(s, PEER_TOPK)
    cand = top_s[..., 0, :, None] + top_s[..., 1, None, :]
    cand = cand.reshape(B, S, PEER_HEADS, PEER_TOPK * PEER_TOPK)
    best_s, best_c = lax.top_k(cand, PEER_TOPK)
    i1 = jnp.take_along_axis(top_i[..., 0, :], best_c // PEER_TOPK, axis=-1)
    i2 = jnp.take_along_axis(top_i[..., 1, :], best_c % PEER_TOPK, axis=-1)
    experts = i1 * N_KEYS + i2
    gates = jax.nn.softmax(best_s, axis=-1).astype(x.dtype)
    T = B * S
    HK = PEER_HEADS * PEER_TOPK
    xs = x.reshape(T // PEER_CHUNK, PEER_CHUNK, D)
    es = experts.reshape(T // PEER_CHUNK, PEER_CHUNK, HK)
    gs = gates.reshape(T // PEER_CHUNK, PEER_CHUNK, HK)

    def chunk(args):
        xc, ec, gc = args
        u = peer_u[ec]
        act = jax.nn.gelu(jnp.einsum('tkd,td->tk', u, xc))
        return jnp.einsum('tk,tkd->td', gc * act, peer_v[ec])

    return lax.map(chunk, (xs, es, gs)).reshape(B, S, D)


def setup_inputs(seed: int = 0) -> dict:
    key = jax.random.key(seed)
    ks = iter(jax.random.split(key, 40))
    f32 = jnp.float32

    def nrm(shape, scale):
        return jax.random.normal(next(ks), shape, f32) * scale

    def gain(shape):
        return 1.0 + nrm(shape, 0.05)

    L = DEPTH
    u = jax.random.uniform(next(ks), (L, D_RNN), f32, 0.9, 0.999)
    s_root = u ** (1.0 / LRU_C)
    lru_lambda = jnp.log(s_root) - jnp.log1p(-s_root)
    return {
        'x': nrm((BATCH, SEQ, D_MODEL), 1.0),
        'p': nrm((DEPTH, BATCH, SEQ, PLE_DIM), 1.0),
        'attn_norm': gain((L, D_MODEL)),
        'w_in': nrm((L, D_MODEL, IN_WIDTH), D_MODEL ** -0.5),
        'b_gate': nrm((L, 2 * D_MODEL), 0.01),
        'q_norm': gain((L, Q_LORA_RANK)),
        'w_uq': nrm((L, Q_LORA_RANK, N_HEADS * QK_HEAD_DIM), Q_LORA_RANK ** -0.5),
        'kv_norm': gain((L, KV_LORA_RANK)),
        'w_ukv': nrm((L, KV_LORA_RANK, N_HEADS * (QK_NOPE_DIM + V_HEAD_DIM)), KV_LORA_RANK ** -0.5),
        'w_attn_o': nrm((L, N_HEADS * V_HEAD_DIM, D_MODEL), (N_HEADS * V_HEAD_DIM) ** -0.5),
        'conv_w': nrm((L, CONV_WIDTH, D_RNN), CONV_WIDTH ** -0.5),
        'conv_b': nrm((L, D_RNN), 0.01),
        'w_rg_a': nrm((L, RNN_BLOCKS, RNN_BLOCK_DIM, RNN_BLOCK_DIM), RNN_BLOCK_DIM ** -0.5),
        'b_rg_a': nrm((L, RNN_BLOCKS, RNN_BLOCK_DIM), 0.01),
        'w_rg_x': nrm((L, RNN_BLOCKS, RNN_BLOCK_DIM, RNN_BLOCK_DIM), RNN_BLOCK_DIM ** -0.5),
        'b_rg_x': nrm((L, RNN_BLOCKS, RNN_BLOCK_DIM), 0.01),
        'lru_lambda': lru_lambda,
        'w_rnn_o': nrm((L, D_RNN, D_MODEL), D_RNN ** -0.5),
        'w_out': nrm((L, D_MODEL, D_MODEL), D_MODEL ** -0.5),
        'ffn_norm': gain((L, D_MODEL)),
        'w_peer_q': nrm((L, D_MODEL, PEER_HEADS * PEER_QUERY_DIM), D_MODEL ** -0.5),
        'peer_subkeys': nrm((L, PEER_HEADS, 2, N_KEYS, PEER_HALF), PEER_HALF ** -0.5),
        'peer_u': nrm((L, N_EXPERTS, D_MODEL), D_MODEL ** -0.5),
        'peer_v': nrm((L, N_EXPERTS, D_MODEL), PEER_HEADS ** -0.5),
        'ple_norm': gain((L, D_MODEL)),
        'w_ple_gate': nrm((L, D_MODEL, D_MODEL), D_MODEL ** -0.5),
        'w_ple_proj': nrm((L, PLE_DIM, D_MODEL), PLE_DIM ** -0.5),
        'final_norm': gain((D_MODEL,)),
    }


def reference(x, p, attn_norm, w_in, b_gate, q_norm, w_uq, kv_norm, w_ukv, w_attn_o,
              conv_w, conv_b, w_rg_a, b_rg_a, w_rg_x, b_rg_x, lru_lambda, w_rnn_o, w_out,
              ffn_norm, w_peer_q, peer_subkeys, peer_u, peer_v, ple_norm, w_ple_gate,
              w_ple_proj, final_norm):
    h = x
    for l in range(DEPTH):
        n = rms_norm(h, attn_norm[l])
        c_q, c_kv, k_r, x_r, y_r, ga, gr = split_columns(n @ w_in[l])
        g_attn = jax.nn.sigmoid(ga + b_gate[l, :D_MODEL])
        g_rnn = jax.nn.sigmoid(gr + b_gate[l, D_MODEL:])
        y_attn = mla_branch(c_q, c_kv, k_r, q_norm[l], w_uq[l], kv_norm[l], w_ukv[l], w_attn_o[l])
        y_rnn = rglru_branch(x_r, y_r, conv_w[l], conv_b[l], w_rg_a[l], b_rg_a[l],
                             w_rg_x[l], b_rg_x[l], lru_lambda[l], w_rnn_o[l])
        h = h + (g_attn * y_attn + g_rnn * y_rnn) @ w_out[l]
        h = h + peer_ffn(rms_norm(h, ffn_norm[l]), w_peer_q[l], peer_subkeys[l], peer_u[l], peer_v[l])
        gate = jax.nn.sigmoid(rms_norm(h, ple_norm[l]) @ w_ple_gate[l])
        h = h + gate * (p[l] @ w_ple_proj[l])
    return rms_norm(h, final_norm)
```

```python
import numpy as np
from contextlib import ExitStack, contextmanager
import concourse.bass as bass
import concourse.mybir as mybir
from concourse.bass_utils import run_bass_kernel_spmd

F32 = mybir.dt.float32
BF16 = mybir.dt.bfloat16
AF = mybir.ActivationFunctionType
ALU = mybir.AluOpType

NV = 288
C_ATTN, C_QN, C_KVN, C_CB, C_CW, C_BA, C_BX, C_LAM, C_BGA, C_BGR, C_FFN, C_PLE, C_FIN = \
    0, 16, 22, 26, 42, 106, 122, 138, 154, 170, 186, 202, 218
C_EPS, C_ONE, C_ZERO, C_M1, C_VIS, C_OWN = 234, 235, 236, 237, 240, 256
NEG = -30000.0
NCH = 18
TK = 9216


class Buf:
    def __init__(self, name):
        self.name = name
        self.w = None
        self.r = {}


class Tl:
    def __init__(self, t, name):
        self.t = t
        self.b = Buf(name)


_uid = [0]


def _un(name):
    _uid[0] += 1
    return "%s_%d" % (name, _uid[0])


class Sched:
    ENG = ('pe', 'act', 'dve', 'pool', 'sp')

    def __init__(self, nc, stack):
        self.nc = nc
        self.stack = stack
        self.prog = {k: [] for k in self.ENG}
        self.sems = {}
        self.cnt = {}
        self.waited = {k: {} for k in self.ENG}
        self.nsem = 0
        for k in self.ENG:
            self._newsem(k)

    def _newsem(self, key):
        s = self.stack.enter_context(self.nc.semaphore("s%d_%s" % (self.nsem, key[:12])))
        self.nsem += 1
        self.sems[key] = s
        self.cnt[key] = 0
        for e in self.ENG:
            self.waited[e].pop(key, None)

    def _need(self, eng, dep, waits):
        if dep is None:
            return
        key, sem, val = dep
        if self.sems.get(key) is not sem:
            return
        if key == 'pe' and eng == 'pe':
            return
        if self.waited[eng].get(key, 0) >= val:
            return
        waits[key] = max(waits.get(key, 0), val)

    def op(self, eng, fn, reads=(), writes=(), dma_key=None):
        waits = {}
        for b in reads:
            self._need(eng, b.w, waits)
        for b in writes:
            self._need(eng, b.w, waits)
            for k, (s, v) in b.r.items():
                self._need(eng, (k, s, v), waits)
        for k, v in waits.items():
            self.waited[eng][k] = v
        if dma_key is None:
            key, inc = eng, 1
        else:
            key, inc = dma_key, 16
            if key not in self.sems:
                self._newsem(key)
        self.cnt[key] += inc
        val = self.cnt[key]
        assert val < 60000, (key, val)
        sem = self.sems[key]
        self.prog[eng].append(([(self.sems[k], v) for k, v in waits.items()], fn, sem, inc))
        for b in reads:
            b.r[key] = (sem, val)
        for b in writes:
            b.w = (key, sem, val)
            b.r = {}

    def barrier(self, new_epoch=True):
        for eng in self.ENG:
            waits = {}
            for k, v in self.cnt.items():
                if v > 0:
                    self._need(eng, (k, self.sems[k], v), waits)
            for k, v in waits.items():
                self.waited[eng][k] = v
            if waits:
                self.prog[eng].append(([(self.sems[k], v) for k, v in waits.items()], None, None, 0))

    def flush(self):
        progs = self.prog
        with self.nc.Block() as block:
            def mk(engname):
                def body(engine):
                    for waits, fn, sem, inc in progs[engname]:
                        for s, v in waits:
                            engine.wait_ge(s, v)
                        if fn is not None:
                            fn(engine).then_inc(sem, inc)
                return body
            block.tensor(mk('pe'))
            block.scalar(mk('act'))
            block.vector(mk('dve'))
            block.gpsimd(mk('pool'))
            block.sync(mk('sp'))
        self.prog = {k: [] for k in self.ENG}


def build_nc(debug=False):
    nc = bass.Bass("TRN2", target_bir_lowering=False)

    def din(name, shape, dt=F32):
        return nc.dram_tensor(name, list(shape), dt, kind="ExternalInput").ap()

    def dscr(name, shape, dt):
        return nc.dram_tensor(name, list(shape), dt, kind="ExternalOutput" if debug else "Internal").ap()

    xT = din("xT", [2048, 8192]); xoT = din("xoT", [2048, 1024]); poT = din("poT", [256, 1024])
    vecs = din("vecs", [128, NV]); cs = din("cs", [2, 64, TK])
    w_in = din("w_in", [2048, 9536]); w_krsw = din("w_krsw", [2048, 64])
    w_uq = din("w_uq", [768, 3072]); w_uqsw = din("w_uqsw", [768, 1024])
    w_ukv = din("w_ukv", [512, 4096]); w_ao = din("w_attn_o", [2048, 2048])
    w_rga = din("w_rg_a", [16, 128, 128]); w_rgx = din("w_rg_x", [16, 128, 128])
    w_ro = din("w_rnn_o", [2048, 2048]); w_out = din("w_out", [2048, 2048]); w_pq = din("w_peer_q", [2048, 2048])
    skT = din("skT", [128, 16, 128]); puT = din("puT", [2048, 16384]); pv = din("pv", [16384, 2048])
    w_pg = din("w_ple_gate", [2048, 2048]); w_pp = din("w_ple_proj", [256, 2048])
    outT = nc.dram_tensor("outT", [2048, 1024], F32, kind="ExternalOutput").ap()

    nT_all = dscr("nT_all", [2048, 8192], BF16)
    Kt_all = dscr("Kt_all", [16, 128, TK], BF16)
    V_all = dscr("V_all", [16, 128, 72, 128], BF16)
    kpe_all = dscr("kpe_all", [64, TK], BF16)
    QT_all = dscr("QT_all", [16, 128, 1024], BF16)
    QPE_all = dscr("QPE_all", [16, 64, 1024], BF16)
    OT_all = dscr("OT_all", [16, 128, 1024], BF16)
    hown_d = dscr("hown_d", [2048, 1024], BF16)
    nown_d = dscr("nown_d", [2048, 1024], BF16)
    h1_d = dscr("h1_d", [2048, 1024], F32)
    h2_d = dscr("h2_d", [2048, 1024], F32)

    topstack = ExitStack()
    S = Sched(nc, topstack)

    def bl(x):
        return [y.b if isinstance(y, Tl) else y for y in x]

    def MM(ps, lhsT, rhs, start, stop, R, W):
        S.op('pe', lambda e: e.matmul(ps, lhsT=lhsT, rhs=rhs, start=start, stop=stop), bl(R), bl(W))

    def TR(ps, in_, ident, R, W):
        S.op('pe', lambda e: e.transpose(out=ps, in_=in_, identity=ident), bl(R), bl(W))

    def ACT(out, in_, func, R, W, bias=None, scale=None):
        kw = {}
        if bias is not None:
            kw['bias'] = bias
        if scale is not None:
            kw['scale'] = scale
        S.op('act', lambda e: e.activation(out=out, in_=in_, func=func, **kw), bl(R), bl(W))

    def TT(eng, out, in0, in1, op, R, W):
        S.op(eng, lambda e: e.tensor_tensor(out=out, in0=in0, in1=in1, op=op), bl(R), bl(W))

    def TS(eng, out, in0, s1, s2, op0, op1, R, W):
        if s2 is None:
            S.op(eng, lambda e: e.tensor_scalar(out=out, in0=in0, scalar1=s1, scalar2=None, op0=op0), bl(R), bl(W))
        else:
            S.op(eng, lambda e: e.tensor_scalar(out=out, in0=in0, scalar1=s1, scalar2=s2, op0=op0, op1=op1), bl(R), bl(W))

    def STT(out, in0, scalar, in1, op0, op1, R, W):
        S.op('dve', lambda e: e.scalar_tensor_tensor(out=out, in0=in0, scalar=scalar, in1=in1, op0=op0, op1=op1), bl(R), bl(W))

    def CP(eng, out, in_, R, W):
        if eng == 'act':
            S.op('act', lambda e: e.copy(out=out, in_=in_), bl(R), bl(W))
        else:
            S.op(eng, lambda e: e.tensor_copy(out=out, in_=in_), bl(R), bl(W))

    def RCP(out, in_, R, W):
        S.op('dve', lambda e: e.reciprocal(out=out, in_=in_), bl(R), bl(W))

    def MEMSET(eng, ap, val, W):
        S.op(eng, lambda e: e.memset(ap, val), [], bl(W))

    def DMA(eng, out, in_, R, W, key):
        S.op(eng, lambda e: e.dma_start(out=out, in_=in_), bl(R), bl(W), dma_key=key)

    class Phase:
        def __init__(self):
            self.st = ExitStack()
            self.ps = []
            self.psi = 0

        def sb(self, name, shape, dt=F32):
            return Tl(self.st.enter_context(nc.sbuf_tensor(_un(name), list(shape), dt)), name)

        def psum(self, name, shape, dt=F32):
            return Tl(self.st.enter_context(nc.psum_tensor(_un(name), list(shape), dt)), name)

        def psring(self, n, prefix):
            self.ps = [self.psum("%s%d" % (prefix, i), [128, 512]) for i in range(n)]
            self.psi = 0

        def nps(self):
            p = self.ps[self.psi % len(self.ps)]
            self.psi += 1
            return p

    @contextmanager
    def phase():
        ph = Phase()
        try:
            yield ph
            S.barrier()
            S.flush()
        finally:
            ph.st.close()
        for k in S.ENG:
            if S.cnt[k] > 28000:
                S._newsem(k)

    cnt = {'cast': 0, 'ev': 0}

    def cast_eng():
        cnt['cast'] += 1
        return ('pool', 'dve', 'act')[cnt['cast'] % 3] if False else 'pool'

    def ev_eng():
        cnt['ev'] += 1
        return 'act' if cnt['ev'] % 2 else 'dve'

    def consts(ph):
        vec = ph.sb("vec", [128, NV])
        DMA('sp', vec.t[:], vecs, [], [vec], 'vec')
        idf = ph.sb("idf", [128, 128])
        MEMSET('pool', idf.t[:], 0.0, [idf])
        S.op('pool', lambda e: e.affine_select(out=idf.t[:], in_=idf.t[:], pattern=[[-1, 128]],
                                               compare_op=ALU.not_equal, fill=1.0, base=0, channel_multiplier=1),
             [idf.b], [idf.b])
        ident = ph.sb("ident", [128, 128], BF16)
        CP('dve', ident.t[:], idf.t[:], [idf], [ident])
        ones = ph.sb("ones", [128, 128], BF16)
        MEMSET('pool', ones.t[:], 1.0, [ones])
        return vec, ident, ones

    def rmsnorm_T(ph, ones, vec, src_tiles, nkt, T, gcol, dst, D, sq, rt, rstd, psq):
        for kt in range(nkt):
            ap, tl = src_tiles(kt)
            ACT(sq.t[:, kt * T:(kt + 1) * T], ap, AF.Square, [tl], [sq])
        for kt in range(nkt):
            MM(psq.t[:, :T], ones.t[:], sq.t[:, kt * T:(kt + 1) * T], kt == 0, kt == nkt - 1, [ones, sq], [psq])
        ACT(rt.t[:, :T], psq.t[:, :T], AF.Sqrt, [psq, vec], [rt], bias=vec.t[:, C_EPS:C_EPS + 1], scale=1.0 / D)
        RCP(rstd.t[:, :T], rt.t[:, :T], [rt], [rstd])
        for kt in range(nkt):
            ap, tl = src_tiles(kt)
            STT(dst.t[:, kt * T:(kt + 1) * T], ap, vec.t[:, gcol + kt:gcol + kt + 1], rstd.t[:, :T], ALU.mult, ALU.mult,
                [tl, vec, rstd], [dst])

    with phase() as ph:
        vec, ident, ones = consts(ph)
        ph.psring(8, "g1p")
        wkv = ph.sb("wkv", [128, 16, 640], BF16)
        wk = ph.sb("wk", [128, 4, 2048], BF16)
        wv = ph.sb("wv", [128, 4, 2048], BF16)
        for kt in range(16):
            DMA('pool', wkv.t[:, kt, 0:576], w_in[kt * 128:(kt + 1) * 128, 768:1344], [], [wkv], 'wkv')
            DMA('pool', wkv.t[:, kt, 576:640], w_krsw[kt * 128:(kt + 1) * 128, :], [], [wkv], 'wkv')
        for kt in range(4):
            sv = w_ukv[kt * 128:(kt + 1) * 128, :].rearrange("p (h two d) -> p h two d", two=2, d=128)
            DMA('pool', wk.t[:, kt, :].rearrange("p (h d) -> p h d", d=128), sv[:, :, 0, :], [], [wk], 'wk')
            DMA('pool', wv.t[:, kt, :].rearrange("p (h d) -> p h d", d=128), sv[:, :, 1, :], [], [wv], 'wv')
        xp = [ph.sb("xp%d" % i, [128, 2048]) for i in range(4)]
        sq = ph.sb("sq", [128, 8192], BF16)
        nT = ph.sb("nT", [128, 8192], BF16)
        rt = ph.sb("rt", [128, 512]); rstd = ph.sb("rstd", [128, 512])
        rt2 = ph.sb("rt2", [128, 512]); rstd2 = ph.sb("rstd2", [128, 512])
        sqk = ph.sb("sqk", [128, 2048], BF16)
        ckvn2 = [ph.sb("ckvn%d" % i, [128, 2048], BF16) for i in range(2)]
        cst = ph.sb("cst", [64, 2, 512])
        t1 = ph.sb("t1", [64, 512]); t2 = ph.sb("t2", [64, 512])
        kpst = ph.sb("kpst", [64, 512], BF16)
        Kst = ph.sb("Kst", [128, 16, 512], BF16)
        Vst = ph.sb("Vst", [128, 4, 2048], BF16)
        P = ph.ps
        kvr = [0]

        def kvps():
            p = P[6 + (kvr[0] % 2)]
            kvr[0] += 1
            return p

        def partA(j):
            src = xT[:, 512 * j:512 * j + 512] if j < 16 else xoT[:, 512 * (j - 16):512 * (j - 16) + 512]
            for q in range(4):
                DMA('sp', xp[q].t[:].rearrange("p (k t) -> p k t", t=512),
                    src[q * 512:(q + 1) * 512, :].rearrange("(k p) t -> p k t", p=128), [], [xp[q]], xp[q].b.name)
            DMA('sp', cst.t[:], cs[:, :, 512 * j:512 * j + 512].rearrange("c p t -> p c t"), [], [cst], 'cst')
            rmsnorm_T(ph, ones, vec, lambda kt: (xp[kt // 4].t[:, (kt % 4) * 512:(kt % 4 + 1) * 512], xp[kt // 4]),
                      16, 512, C_ATTN, nT, 2048.0, sq, rt, rstd, P[0])
            if j < 16:
                DMA('pool', nT_all[:, 512 * j:512 * j + 512].rearrange("(k p) t -> p k t", p=128),
                    nT.t[:].rearrange("p (k t) -> p k t", t=512), [nT], [], 'nTst')

        def partB(j):
            ckvn = ckvn2[j % 2]
            pck = P[1:5]
            pkr = P[5]
            for o in range(4):
                for kt in range(16):
                    MM(pck[o].t[:], wkv.t[:, kt, o * 128:(o + 1) * 128], nT.t[:, kt * 512:(kt + 1) * 512], kt == 0, kt == 15,
                       [wkv, nT], [pck[o]])
            for kt in range(16):
                MM(pkr.t[0:64, :], wkv.t[:, kt, 512:576], nT.t[:, kt * 512:(kt + 1) * 512], kt == 0, kt == 15, [wkv, nT], [pkr])
            TT('dve', t1.t[:], pkr.t[0:64, :], cst.t[:, 0, :], ALU.mult, [pkr, cst], [t1])
            for kt in range(16):
                MM(pkr.t[0:64, :], wkv.t[:, kt, 576:640], nT.t[:, kt * 512:(kt + 1) * 512], kt == 0, kt == 15, [wkv, nT], [pkr])
            TT('dve', t2.t[:], pkr.t[0:64, :], cst.t[:, 1, :], ALU.mult, [pkr, cst], [t2])
            TT('dve', kpst.t[:], t1.t[:], t2.t[:], ALU.add, [t1, t2], [kpst])
            DMA('pool', kpe_all[:, 512 * j:512 * j + 512], kpst.t[:], [kpst], [], 'kpst')
            rmsnorm_T(ph, ones, vec, lambda o: (pck[o].t[:], pck[o]), 4, 512, C_KVN, ckvn, 512.0, sqk, rt2, rstd2, P[0])

        def partK(j):
            ckvn = ckvn2[j % 2]
            for h in range(16):
                p = kvps()
                for kt in range(4):
                    MM(p.t[:], wk.t[:, kt, h * 128:(h + 1) * 128], ckvn.t[:, kt * 512:(kt + 1) * 512], kt == 0, kt == 3, [wk, ckvn], [p])
                CP('act', Kst.t[:, h, :], p.t[:], [p], [Kst])
            DMA('pool', Kt_all[:, :, 512 * j:512 * j + 512].rearrange("h p t -> p h t"), Kst.t[:], [Kst], [], 'Kst')

        def partV(j):
            ckvn = ckvn2[j % 2]
            for tt in range(4):
                for cg in range(4):
                    p = kvps()
                    for kt in range(4):
                        MM(p.t[:], ckvn.t[:, kt * 512 + tt * 128:kt * 512 + tt * 128 + 128], wv.t[:, kt, cg * 512:(cg + 1) * 512],
                           kt == 0, kt == 3, [wv, ckvn], [p])
                    CP(ev_eng(), Vst.t[:, tt, cg * 512:(cg + 1) * 512], p.t[:], [p], [Vst])
            for tt in range(4):
                DMA('pool', V_all[:, :, 4 * j + tt, :].rearrange("h p d -> p h d"),
                    Vst.t[:, tt, :].rearrange("p (h d) -> p h d", d=128), [Vst], [], 'Vst')

        for j in range(NCH + 1):
            if j < NCH:
                partA(j)
            if j >= 1:
                partK(j - 1)
            if j < NCH:
                partB(j)
            if j >= 1:
                partV(j - 1)

    with phase() as ph:
        vec, ident, ones = consts(ph)
        ph.psring(6, "g2p")
        wxr = ph.sb("wxr", [128, 16, 2048], BF16)
        wga = ph.sb("wga", [128, 16, 128], BF16)
        wgx = ph.sb("wgx", [128, 16, 128], BF16)
        for kt in range(16):
            DMA('pool', wxr.t[:, kt, :], w_in[kt * 128:(kt + 1) * 128, 1344:3392], [], [wxr], 'wxr')
        DMA('pool', wga.t[:], w_rga.rearrange("b i j -> i b j"), [], [wga], 'wga')
        DMA('pool', wgx.t[:], w_rgx.rearrange("b i j -> i b j"), [], [wgx], 'wgx')
        cf = ph.sb("cf", [128, 48])
        ACT(cf.t[:, 0:16], vec.t[:, C_LAM:C_LAM + 16], AF.Exp, [vec], [cf], scale=-1.0)
        ACT(cf.t[:, 0:16], cf.t[:, 0:16], AF.Ln, [cf, vec], [cf], bias=vec.t[:, C_ONE:C_ONE + 1])
        TS('dve', cf.t[:, 16:32], cf.t[:, 0:16], -8.0, None, ALU.mult, None, [cf], [cf])
        TS('dve', cf.t[:, 32:48], cf.t[:, 0:16], -16.0, None, ALU.mult, None, [cf], [cf])
        hown = ph.sb("hown", [128, 16, 1024], BF16)
        MEMSET('pool', hown.t[:], 0.0, [hown])
        carry = ph.sb("carry", [128, 16]); MEMSET('pool', carry.t[:], 0.0, [carry])
        halo = ph.sb("halo", [128, 16, 4]); MEMSET('pool', halo.t[:], 0.0, [halo])
        nTb = [ph.sb("nTb%d" % i, [128, 8192], BF16) for i in range(2)]
        TS('dve', cf.t[:, 0:16], cf.t[:, 16:32], 0.5, None, ALU.mult, None, [cf], [cf])
        hb = ph.sb("hb", [128, 32])
        TS('dve', hb.t[:, 0:16], vec.t[:, C_BA:C_BA + 16], 0.5, None, ALU.mult, None, [vec], [hb])
        TS('dve', hb.t[:, 16:32], vec.t[:, C_BX:C_BX + 16], 0.5, None, ALU.mult, None, [vec], [hb])
        xre = [ph.sb("xre%d" % i, [128, 516]) for i in range(3)]
        xc = [ph.sb("xc%d" % i, [128, 512]) for i in range(8)]
        xcb = [ph.sb("xcb%d" % i, [128, 512], BF16) for i in range(2)]
        rr = [ph.sb("rr%d" % i, [128, 512]) for i in range(2)]
        ig = [ph.sb("ig%d" % i, [128, 512]) for i in range(5)]
        aa = [ph.sb("aa%d" % i, [128, 512]) for i in range(4)]
        mu = [ph.sb("mu%d" % i, [128, 512]) for i in range(3)]
        bb = [ph.sb("bb%d" % i, [128, 512]) for i in range(2)]
        hs = [ph.sb("hs%d" % i, [128, 512]) for i in range(2)]
        nbl = set()

        def stA(n):
            j, ct = n // 16, n % 16
            nb = nTb[j % 2]
            if j not in nbl:
                nbl.add(j)
                DMA('sp', nb.t[:].rearrange("p (k t) -> p k t", t=512),
                    nT_all[:, 512 * j:512 * j + 512].rearrange("(k p) t -> p k t", p=128), [], [nb], nb.b.name)
            pxr = ph.ps[n % 2]
            for kt in range(16):
                MM(pxr.t[:], wxr.t[:, kt, ct * 128:(ct + 1) * 128], nb.t[:, kt * 512:(kt + 1) * 512], kt == 0, kt == 15, [wxr, nb], [pxr])

        def stB(n):
            j, ct = n // 16, n % 16
            x_ = xre[n % 3]; pxr = ph.ps[n % 2]
            CP('pool', x_.t[:, 0:3], halo.t[:, ct, 0:3], [halo], [x_])
            CP('act', x_.t[:, 3:515], pxr.t[:], [pxr], [x_])
            CP('pool', halo.t[:, ct, 0:3], x_.t[:, 512:515], [x_], [halo])

        def stC(n):
            j, ct = n // 16, n % 16
            x_ = xre[n % 3]; c_ = xc[n % 8]
            cw = C_CW + 4 * ct
            TS('dve', c_.t[:], x_.t[:, 0:512], vec.t[:, cw:cw + 1], vec.t[:, C_CB + ct:C_CB + ct + 1], ALU.mult, ALU.add,
               [x_, vec], [c_])
            for w in range(1, 4):
                STT(c_.t[:], x_.t[:, w:w + 512], vec.t[:, cw + w:cw + w + 1], c_.t[:], ALU.mult, ALU.add,
                    [x_, vec, c_], [c_])

        def stD(n):
            CP('pool', xcb[n % 2].t[:], xc[n % 8].t[:], [xc[n % 8]], [xcb[n % 2]])

        def stE(n):
            j, ct = n // 16, n % 16
            cb_ = xcb[n % 2]
            pr = ph.ps[2 + (n % 2)]; pi = ph.ps[4 + (n % 2)]
            MM(pr.t[:], wga.t[:, ct, :], cb_.t[:], True, True, [wga, cb_], [pr])
            MM(pi.t[:], wgx.t[:, ct, :], cb_.t[:], True, True, [wgx, cb_], [pi])

        def stF(n):
            j, ct = n // 16, n % 16
            pr = ph.ps[2 + (n % 2)]; pi = ph.ps[4 + (n % 2)]
            ACT(rr[n % 2].t[:], pr.t[:], AF.Tanh, [pr, hb], [rr[n % 2]], bias=hb.t[:, ct:ct + 1], scale=0.5)
            ACT(ig[n % 5].t[:], pi.t[:], AF.Tanh, [pi, hb], [ig[n % 5]], bias=hb.t[:, 16 + ct:17 + ct], scale=0.5)

        def stG(n):
            j, ct = n // 16, n % 16
            ACT(aa[n % 4].t[:], rr[n % 2].t[:], AF.Exp, [rr[n % 2], cf], [aa[n % 4]], bias=cf.t[:, ct:ct + 1], scale=cf.t[:, ct:ct + 1])

        def stH(n):
            TT('pool', mu[n % 3].t[:], aa[n % 4].t[:], aa[n % 4].t[:], ALU.mult, [aa[n % 4]], [mu[n % 3]])

        def stI(n):
            j, ct = n // 16, n % 16
            m_ = mu[n % 3]
            ACT(m_.t[:], m_.t[:], AF.Sqrt, [m_, vec], [m_], bias=vec.t[:, C_ONE:C_ONE + 1], scale=-1.0)

        def stJ(n):
            j, ct = n // 16, n % 16
            m_ = mu[n % 3]; b_ = bb[n % 2]; h_ = hs[n % 2]; c_ = xc[n % 8]; a_ = aa[n % 4]; i_ = ig[n % 5]
            if j == 0:
                MEMSET('dve', m_.t[:, 0:1], 1.0, [m_])
            STT(b_.t[:], i_.t[:], 1.0, m_.t[:], ALU.add, ALU.mult, [i_, m_], [b_])
            STT(b_.t[:], b_.t[:], 0.5, c_.t[:], ALU.mult, ALU.mult, [b_, c_], [b_])
            S.op('dve', lambda e: e.tensor_tensor_scan(out=h_.t[:], data0=a_.t[:], data1=b_.t[:], initial=carry.t[:, ct:ct + 1],
                                                       op0=ALU.mult, op1=ALU.add), bl([a_, b_, carry]), bl([h_]))
            CP('pool', carry.t[:, ct:ct + 1], h_.t[:, 511:512], [h_], [carry])
            for half in range(2):
                mc = C_OWN + 2 * j + half
                STT(hown.t[:, ct, half * 512:(half + 1) * 512], h_.t[:], vec.t[:, mc:mc + 1],
                    hown.t[:, ct, half * 512:(half + 1) * 512], ALU.mult, ALU.add, [h_, vec, hown], [hown])

        NU2 = 256
        stages = [stA, stB, stC, stD, stE, stF, stG, stH, stI, stJ]
        for m in range(NU2 + len(stages)):
            for k, st in enumerate(stages):
                n = m - k
                if 0 <= n < NU2:
                    st(n)
        DMA('sp', hown_d.rearrange("(k p) t -> p k t", p=128), hown.t[:], [hown], [], 'hownst')

    def linearT(ph, w_ap, KT, N, in_tl, T, evac, stgs, wbfs, cb=256):
        nblk = N // cb
        for b in range(nblk):
            wb = wbfs[b % len(wbfs)]
            DMA('pool', wb.t[:, :KT * cb].rearrange("p (k c) -> p k c", c=cb),
                w_ap[:, b * cb:(b + 1) * cb].rearrange("(k p) c -> p k c", p=128), [], [wb], wb.b.name)
            for o in range(cb // 128):
                for th in range(T // 512):
                    p = ph.nps()
                    for kt in range(KT):
                        MM(p.t[:], wb.t[:, kt * cb + o * 128:kt * cb + o * 128 + 128],
                           in_tl.t[:, kt * T + th * 512:kt * T + th * 512 + 512], kt == 0, kt == KT - 1, [wb, in_tl], [p])
                    evac(b * (cb // 128) + o, th, p)

    def load_norm_own(ph, ones, vec, src_d, gcol, dst, keep=None):
        sub = ExitStack()
        ph2 = Phase(); ph2.st = sub
        xh = [ph2.sb("lnx%d" % i, [128, 2048]) for i in range(4)]
        sq = ph2.sb("lnsq", [128, 8192], BF16)
        rt = ph2.sb("lnrt", [128, 512]); rstd = ph2.sb("lnrs", [128, 512])
        for th in range(2):
            for q in range(4):
                DMA('sp', xh[q].t[:].rearrange("p (k t) -> p k t", t=512),
                    src_d[q * 512:(q + 1) * 512, th * 512:(th + 1) * 512].rearrange("(k p) t -> p k t", p=128), [], [xh[q]], xh[q].b.name)
            psq = ph.nps()
            for kt in range(16):
                ACT(sq.t[:, kt * 512:(kt + 1) * 512], xh[kt // 4].t[:, (kt % 4) * 512:(kt % 4 + 1) * 512], AF.Square, [xh[kt // 4]], [sq])
            for kt in range(16):
                MM(psq.t[:], ones.t[:], sq.t[:, kt * 512:(kt + 1) * 512], kt == 0, kt == 15, [ones, sq], [psq])
            ACT(rt.t[:], psq.t[:], AF.Sqrt, [psq, vec], [rt], bias=vec.t[:, C_EPS:C_EPS + 1], scale=1.0 / 2048.0)
            RCP(rstd.t[:], rt.t[:], [rt], [rstd])
            for kt in range(16):
                STT(dst.t[:, kt * 1024 + th * 512:kt * 1024 + th * 512 + 512], xh[kt // 4].t[:, (kt % 4) * 512:(kt % 4 + 1) * 512],
                    vec.t[:, gcol + kt:gcol + kt + 1], rstd.t[:], ALU.mult, ALU.mult, [xh[kt // 4], vec, rstd], [dst])
                if keep is not None:
                    CP('pool', keep.t[:, kt * 1024 + th * 512:kt * 1024 + th * 512 + 512],
                       xh[kt // 4].t[:, (kt % 4) * 512:(kt % 4 + 1) * 512], [xh[kt // 4]], [keep])
        S.barrier()
        S.flush()
        sub.close()

    with phase() as ph:
        vec, ident, ones = consts(ph)
        ph.psring(8, "o1p")
        nown = ph.sb("nown", [128, 16384], BF16)
        load_norm_own(ph, ones, vec, xoT, C_ATTN, nown)
        DMA('pool', nown_d.rearrange("(k p) t -> p k t", p=128), nown.t[:].rearrange("p (k t) -> p k t", t=1024), [nown], [], 'nownst')
        stgs = None
        wbfs = [ph.sb("wbf%d" % i, [128, 4096], BF16) for i in range(4)]
        cq = ph.sb("cq", [128, 6 * 1024])
        linearT(ph, w_in[:, 0:768], 16, 768, nown, 1024,
                lambda ot, th, p: CP(ev_eng(), cq.t[:, ot * 1024 + th * 512:ot * 1024 + th * 512 + 512], p.t[:], [p], [cq]),
                stgs, wbfs)
        cqn = ph.sb("cqn", [128, 6 * 1024], BF16)
        sq6 = ph.sb("sq6", [128, 6 * 512], BF16)
        rt = ph.sb("rt", [128, 512]); rstd = ph.sb("rstd", [128, 512])
        for th in range(2):
            psq = ph.nps()
            for kt in range(6):
                ACT(sq6.t[:, kt * 512:(kt + 1) * 512], cq.t[:, kt * 1024 + th * 512:kt * 1024 + th * 512 + 512], AF.Square, [cq], [sq6])
            for kt in range(6):
                MM(psq.t[:], ones.t[:], sq6.t[:, kt * 512:(kt + 1) * 512], kt == 0, kt == 5, [ones, sq6], [psq])
            ACT(rt.t[:], psq.t[:], AF.Sqrt, [psq, vec], [rt], bias=vec.t[:, C_EPS:C_EPS + 1], scale=1.0 / 768.0)
            RCP(rstd.t[:], rt.t[:], [rt], [rstd])
            for kt in range(6):
                STT(cqn.t[:, kt * 1024 + th * 512:kt * 1024 + th * 512 + 512], cq.t[:, kt * 1024 + th * 512:kt * 1024 + th * 512 + 512],
                    vec.t[:, C_QN + kt:C_QN + kt + 1], rstd.t[:], ALU.mult, ALU.mult, [cq, vec, rstd], [cqn])
        cso = ph.sb("cso", [64, 2, 1024])
        DMA('sp', cso.t[:], cs[:, :, 8192:9216].rearrange("c p t -> p c t"), [], [cso], 'cso')
        qst = [ph.sb("qst%d" % i, [128, 1024], BF16) for i in range(2)]
        qpst = [ph.sb("qpst%d" % i, [64, 1024], BF16) for i in range(2)]
        q1 = ph.sb("q1", [64, 512]); q2 = ph.sb("q2", [64, 512])
        for h in range(16):
            wb = wbfs[h % 4]
            DMA('pool', wb.t[:, 0:6 * 256].rearrange("p (k c) -> p k c", c=256)[:, :, 0:192],
                w_uq[:, h * 192:(h + 1) * 192].rearrange("(k p) c -> p k c", p=128), [], [wb], wb.b.name)
            DMA('pool', wb.t[:, 0:6 * 256].rearrange("p (k c) -> p k c", c=256)[:, :, 192:256],
                w_uqsw[:, h * 64:(h + 1) * 64].rearrange("(k p) c -> p k c", p=128), [], [wb], wb.b.name)
            qs = qst[h % 2]; qp = qpst[h % 2]
            for th in range(2):
                p = ph.nps(); pp = ph.nps(); pw = ph.nps()
                for kt in range(6):
                    MM(p.t[:], wb.t[:, kt * 256:kt * 256 + 128], cqn.t[:, kt * 1024 + th * 512:kt * 1024 + th * 512 + 512], kt == 0, kt == 5, [wb, cqn], [p])
                for kt in range(6):
                    MM(pp.t[0:64, :], wb.t[:, kt * 256 + 128:kt * 256 + 192], cqn.t[:, kt * 1024 + th * 512:kt * 1024 + th * 512 + 512], kt == 0, kt == 5, [wb, cqn], [pp])
                for kt in range(6):
                    MM(pw.t[0:64, :], wb.t[:, kt * 256 + 192:kt * 256 + 256], cqn.t[:, kt * 1024 + th * 512:kt * 1024 + th * 512 + 512], kt == 0, kt == 5, [wb, cqn], [pw])
                CP('act', qs.t[:, th * 512:(th + 1) * 512], p.t[:], [p], [qs])
                TT('dve', q1.t[:], pp.t[0:64, :], cso.t[:, 0, th * 512:(th + 1) * 512], ALU.mult, [pp, cso], [q1])
                TT('dve', q2.t[:], pw.t[0:64, :], cso.t[:, 1, th * 512:(th + 1) * 512], ALU.mult, [pw, cso], [q2])
                TT('dve', qp.t[:, th * 512:(th + 1) * 512], q1.t[:], q2.t[:], ALU.add, [q1, q2], [qp])
            DMA('pool', QT_all[h], qs.t[:], [qs], [], qs.b.name + 'st')
            DMA('pool', QPE_all[h], qp.t[:], [qp], [], qp.b.name + 'st')

    with phase() as ph:
        vec, ident, ones = consts(ph)
        pS = [ph.psum("pS%d" % i, [128, 1024]) for i in range(3)]
        pO = [ph.psum("pO%d" % i, [128, 512]) for i in range(2)]
        ring = [0]
        slot = {}
        kpe = ph.sb("kpe", [64, TK], BF16)
        DMA('sp', kpe.t[:], kpe_all, [], [kpe], 'kpe')
        msk = ph.sb("msk", [128, 4, 512])
        MEMSET('pool', msk.t[:], 0.0, [msk])
        for d in range(4):
            S.op('pool', (lambda d=d: (lambda e: e.affine_select(out=msk.t[:, d, :], in_=msk.t[:, d, :], pattern=[[1, 512]],
                                                                 compare_op=ALU.is_ge, fill=-1.0e5, base=-128 * d,
                                                                 channel_multiplier=-1)))(), [msk.b], [msk.b])
        KTb = [ph.sb("KTb%d" % i, [128, TK], BF16) for i in range(2)]
        Vb = [ph.sb("Vb%d" % i, [128, 72, 128], BF16) for i in range(2)]
        Qb = [ph.sb("Qb%d" % i, [128, 1024], BF16) for i in range(2)]
        QPb = [ph.sb("QPb%d" % i, [64, 1024], BF16) for i in range(2)]
        PT = [ph.sb("PT%d" % i, [128, 1024], BF16) for i in range(3)]
        tmpm = [ph.sb("tmpm%d" % i, [128, 512]) for i in range(2)]
        rl = ph.sb("rl", [128, 1024])
        Ost = [ph.sb("Ost%d" % i, [128, 1024], BF16) for i in range(2)]
        scale = 192.0 ** -0.5
        Pacc = [ph.sb("Pacc%d" % i, [128, 1024]) for i in range(2)]
        Phi = ph.sb("Phi", [128, 1024], BF16)
        Plo = ph.sb("Plo", [128, 1024], BF16)
        units = [(h, kb) for h in range(16) for kb in range(72)]
        loaded = set()

        def bufs(h):
            return KTb[h % 2], Vb[h % 2], Qb[h % 2], QPb[h % 2]

        def issue_qk(u):
            h, kb = units[u]
            kt_, vb, qb, qpb = bufs(h)
            if h not in loaded:
                loaded.add(h)
                DMA('sp', kt_.t[:], Kt_all[h], [], [kt_], kt_.b.name)
                DMA('sp', vb.t[:], V_all[h], [], [vb], vb.b.name)
                DMA('sp', qb.t[:], QT_all[h], [], [qb], qb.b.name)
                DMA('sp', qpb.t[:], QPE_all[h], [], [qpb], qpb.b.name)
            slot[u] = [x for x in range(3) if x not in slot.values()][0]
            ps = pS[slot[u]]
            gs = [0, 1] if kb < 68 else [1]
            for g in gs:
                MM(ps.t[:, g * 512:(g + 1) * 512], kt_.t[:, kb * 128:(kb + 1) * 128], qb.t[:, g * 512:(g + 1) * 512], True, False, [kt_, qb], [ps])
            for g in gs:
                MM(ps.t[:, g * 512:(g + 1) * 512], kpe.t[0:64, kb * 128:(kb + 1) * 128], qpb.t[0:64, g * 512:(g + 1) * 512], False, True, [kpe, qpb], [ps])

        def issue_pv(u):
            h, kb = units[u]
            kt_, vb, qb, qpb = bufs(h)
            ps = pS[slot.pop(u)]; pt = PT[u % 3]
            pa = Pacc[h % 2]
            if kb < 64:
                bc = C_VIS + kb // 4
                ACT(pt.t[:], ps.t[:], AF.Exp, [ps, vec], [pt], bias=vec.t[:, bc:bc + 1], scale=scale)
                gs = [0, 1]
            else:
                jj = kb - 64
                if jj < 4:
                    tm = tmpm[u % 2]
                    TT('dve', tm.t[:], ps.t[:, 0:512], msk.t[:, jj, :], ALU.add, [ps, msk], [tm])
                    ACT(pt.t[:, 0:512], tm.t[:], AF.Exp, [tm], [pt], scale=scale)
                    ACT(pt.t[:, 512:1024], ps.t[:, 512:1024], AF.Exp, [ps], [pt], scale=scale)
                    gs = [0, 1]
                else:
                    tm = tmpm[u % 2]
                    TT('dve', tm.t[:], ps.t[:, 512:1024], msk.t[:, jj - 4, :], ALU.add, [ps, msk], [tm])
                    ACT(pt.t[:, 512:1024], tm.t[:], AF.Exp, [tm], [pt], scale=scale)
                    gs = [1]
            for g in gs:
                last = (kb == 67) if g == 0 else (kb == 71)
                MM(pO[g].t[:], vb.t[:, kb, :], pt.t[:, g * 512:(g + 1) * 512], kb == 0, last, [vb, pt], [pO[g]])
            lo_, hi_ = gs[0] * 512, (gs[-1] + 1) * 512
            if kb == 0:
                CP('dve', pa.t[:], pt.t[:], [pt], [pa])
            else:
                TT('dve', pa.t[:, lo_:hi_], pt.t[:, lo_:hi_], pa.t[:, lo_:hi_], ALU.add, [pt, pa], [pa])
            if kb == 71:
                CP('dve', Phi.t[:], pa.t[:], [pa], [Phi])
                TT('dve', Plo.t[:], pa.t[:], Phi.t[:], ALU.subtract, [pa, Phi], [Plo])
                pL = pS[[x for x in range(3) if x not in slot.values()][0]]
                for g in range(2):
                    MM(pL.t[:, g * 512:(g + 1) * 512], ones.t[:], Phi.t[:, g * 512:(g + 1) * 512], True, False, [ones, Phi], [pL])
                    MM(pL.t[:, g * 512:(g + 1) * 512], ones.t[:], Plo.t[:, g * 512:(g + 1) * 512], False, True, [ones, Plo], [pL])
                os_ = Ost[h % 2]
                RCP(rl.t[:], pL.t[:], [pL], [rl])
                for g in range(2):
                    TT('dve', os_.t[:, g * 512:(g + 1) * 512], pO[g].t[:], rl.t[:, g * 512:(g + 1) * 512], ALU.mult, [pO[g], rl], [os_])
                DMA('pool', OT_all[h], os_.t[:], [os_], [], os_.b.name + 'st')

        NU = len(units)
        for u in range(NU + 2):
            if u < NU:
                issue_qk(u)
            if u >= 2:
                issue_pv(u - 2)

    with phase() as ph:
        vec, ident, ones = consts(ph)
        ph.psring(8, "o2p")
        stgs = None
        wbfs = [ph.sb("wbf%d" % i, [128, 4096], BF16) for i in range(4)]
        yg = ph.sb("yg", [128, 16384], BF16)
        DMA('sp', yg.t[:].rearrange("p (k t) -> p k t", t=1024), hown_d.rearrange("(k p) t -> p k t", p=128), [], [yg], 'ygl')
        mix = ph.sb("mix", [128, 16384], BF16)
        gas = ph.sb("gas", [128, 16384], BF16)
        gtmp = [ph.sb("gtmp%d" % i, [128, 512]) for i in range(2)]
        cgt = [0]
        with ExitStack() as sub:
            ph2 = Phase(); ph2.st = sub; ph2.ps = ph.ps
            ph2.nps = ph.nps
            nown = ph2.sb("nown", [128, 16384], BF16)
            DMA('sp', nown.t[:].rearrange("p (k t) -> p k t", t=1024), nown_d.rearrange("(k p) t -> p k t", p=128), [], [nown], 'nownl')

            def ev_yr(ot, th, p):
                g_ = gtmp[cgt[0] % 2]; cgt[0] += 1
                ACT(g_.t[:], p.t[:], AF.Gelu_apprx_tanh, [p], [g_])
                sl = slice(ot * 1024 + th * 512, ot * 1024 + th * 512 + 512)
                TT('dve', yg.t[:, sl], g_.t[:], yg.t[:, sl], ALU.mult, [g_, yg], [yg])
            linearT(ph, w_in[:, 3392:5440], 16, 2048, nown, 1024, ev_yr, stgs, wbfs)

            def ev_gr(ot, th, p):
                sl = slice(ot * 1024 + th * 512, ot * 1024 + th * 512 + 512)
                ACT(mix.t[:, sl], p.t[:], AF.Sigmoid, [p, vec], [mix], bias=vec.t[:, C_BGR + ot:C_BGR + ot + 1])
            linearT(ph, w_in[:, 7488:9536], 16, 2048, nown, 1024, ev_gr, stgs, wbfs)

            def ev_ga(ot, th, p):
                sl = slice(ot * 1024 + th * 512, ot * 1024 + th * 512 + 512)
                ACT(gas.t[:, sl], p.t[:], AF.Sigmoid, [p, vec], [gas], bias=vec.t[:, C_BGA + ot:C_BGA + ot + 1])
            linearT(ph, w_in[:, 5440:7488], 16, 2048, nown, 1024, ev_ga, stgs, wbfs)
            S.barrier()
            S.flush()

        def ev_rnn(ot, th, p):
            sl = slice(ot * 1024 + th * 512, ot * 1024 + th * 512 + 512)
            TT('dve', mix.t[:, sl], p.t[:], mix.t[:, sl], ALU.mult, [p, mix], [mix])
        linearT(ph, w_ro, 16, 2048, yg, 1024, ev_rnn, stgs, wbfs)
        S.barrier()
        DMA('sp', yg.t[:].rearrange("p (k t) -> p k t", t=1024), OT_all.rearrange("h p t -> p h t"), [], [yg], 'ygl')

        def ev_att(ot, th, p):
            g_ = gtmp[cgt[0] % 2]; cgt[0] += 1
            sl = slice(ot * 1024 + th * 512, ot * 1024 + th * 512 + 512)
            TT('dve', g_.t[:], p.t[:], gas.t[:, sl], ALU.mult, [p, gas], [g_])
            TT('dve', mix.t[:, sl], g_.t[:], mix.t[:, sl], ALU.add, [g_, mix], [mix])
        linearT(ph, w_ao, 16, 2048, yg, 1024, ev_att, stgs, wbfs)
        xr_ = [ph.sb("xres%d" % i, [128, 512]) for i in range(2)]

        def ev_out(ot, th, p):
            x_ = xr_[cgt[0] % 2]; cgt[0] += 1
            DMA('sp', x_.t[:], xoT[ot * 128:(ot + 1) * 128, th * 512:(th + 1) * 512], [], [x_], x_.b.name)
            TT('dve', x_.t[:], p.t[:], x_.t[:], ALU.add, [p, x_], [x_])
            DMA('pool', h1_d[ot * 128:(ot + 1) * 128, th * 512:(th + 1) * 512], x_.t[:], [x_], [], x_.b.name)
        linearT(ph, w_out, 16, 2048, mix, 1024, ev_out, stgs, wbfs)

    for half in range(2):
        with phase() as ph:
            vec, ident, ones = consts(ph)
            ph.psring(8, "pp")
            T0 = half * 512
            hn = ph.sb("hn", [128, 16 * 512], BF16)
            s12 = ph.sb("s12", [128, 4, 16, 128])
            tau = ph.sb("tau", [128, 32]); cbias = ph.sb("cbias", [128, 32])
            ntau = ph.sb("ntau", [128, 32]); b3 = ph.sb("b3", [128, 32])
            with ExitStack() as sub:
                ph2 = Phase(); ph2.st = sub; ph2.ps = ph.ps; ph2.nps = ph.nps
                xh = [ph2.sb("px%d" % i, [128, 2048]) for i in range(4)]
                sq = ph2.sb("psq", [128, 8192], BF16)
                rt = ph2.sb("prt", [128, 512]); rstd = ph2.sb("prs", [128, 512])
                for q in range(4):
                    DMA('sp', xh[q].t[:].rearrange("p (k t) -> p k t", t=512),
                        h1_d[q * 512:(q + 1) * 512, T0:T0 + 512].rearrange("(k p) t -> p k t", p=128), [], [xh[q]], xh[q].b.name)
                psq = ph.nps()
                rmsnorm_T(ph, ones, vec, lambda kt: (xh[kt // 4].t[:, (kt % 4) * 512:(kt % 4 + 1) * 512], xh[kt // 4]),
                          16, 512, C_FFN, hn, 2048.0, sq, rt, rstd, psq)
                stgs = None
                wbfs = [ph2.sb("wbf%d" % i, [128, 4096], BF16) for i in range(4)]
                qp = ph2.sb("qp", [128, 16 * 512], BF16)
                linearT(ph, w_pq, 16, 2048, hn, 512,
                        lambda ot, th, p: CP(ev_eng(), qp.t[:, ot * 512:(ot + 1) * 512], p.t[:], [p], [qp]), stgs, wbfs)
                skb = ph2.sb("skb", [128, 16, 128], BF16)
                DMA('pool', skb.t[:], skT, [], [skb], 'skb')
                top = ph2.sb("top", [128, 16, 16]); wk1 = ph2.sb("wk1", [128, 128])
                cand = ph2.sb("cand", [128, 8, 256]); wk2 = ph2.sb("wk2", [128, 256])
                best = ph2.sb("best", [128, 8, 16]); negm = ph2.sb("negm", [128, 8])
                e16 = ph2.sb("e16", [128, 8, 16]); Z = ph2.sb("Z", [128, 8]); lnZ = ph2.sb("lnZ", [128, 8])
                pst_top = top.t[:].ap[0][0]
                for tt in range(4):
                    for hh in range(16):
                        if hh % 4 == 0:
                            p = ph.nps()
                        MM(p.t[:, (hh % 4) * 128:(hh % 4 + 1) * 128], qp.t[:, hh * 512 + tt * 128:hh * 512 + tt * 128 + 128], skb.t[:, hh, :],
                           True, True, [qp, skb], [p])
                        if hh % 4 == 3:
                            CP(ev_eng(), s12.t[:, tt, hh - 3:hh + 1, :].rearrange("p a b -> p (a b)"), p.t[:], [p], [s12])
                    for hh in range(16):
                        S.op('dve', (lambda tt=tt, hh=hh: (lambda e: e.max(out=top.t[:, hh, 0:8], in_=s12.t[:, tt, hh, :])))(), bl([s12]), bl([top]))
                        S.op('dve', (lambda tt=tt, hh=hh: (lambda e: e.match_replace(out=wk1.t[:], in_to_replace=top.t[:, hh, 0:8],
                                                                                   in_values=s12.t[:, tt, hh, :], imm_value=-1.0e30)))(),
                             bl([s12, top]), bl([wk1]))
                        S.op('dve', (lambda hh=hh: (lambda e: e.max(out=top.t[:, hh, 8:16], in_=wk1.t[:])))(), bl([wk1]), bl([top]))
                    for h in range(8):
                        in0 = bass.AP(top.t, (2 * h) * 16, [[pst_top, 128], [1, 16], [0, 16]])
                        in1 = bass.AP(top.t, (2 * h + 1) * 16, [[pst_top, 128], [0, 16], [1, 16]])
                        TT('dve', cand.t[:, h, :].rearrange("p (a b) -> p a b", b=16), in0, in1, ALU.add, [top], [cand])
                        S.op('dve', (lambda h=h: (lambda e: e.max(out=best.t[:, h, 0:8], in_=cand.t[:, h, :])))(), bl([cand]), bl([best]))
                        S.op('dve', (lambda h=h: (lambda e: e.match_replace(out=wk2.t[:], in_to_replace=best.t[:, h, 0:8],
                                                                           in_values=cand.t[:, h, :], imm_value=-1.0e30)))(),
                             bl([cand, best]), bl([wk2]))
                        S.op('dve', (lambda h=h: (lambda e: e.max(out=best.t[:, h, 8:16], in_=wk2.t[:])))(), bl([wk2]), bl([best]))
                    TS('dve', negm.t[:], best.t[:, :, 0], -1.0, None, ALU.mult, None, [best], [negm])
                    CP('dve', tau.t[:, tt * 8:(tt + 1) * 8], best.t[:, :, 15], [best], [tau])
                    pst_b = best.t[:].ap[0][0]
                    TT('dve', e16.t[:], best.t[:], bass.AP(best.t, 0, [[pst_b, 128], [16, 8], [0, 16]]), ALU.subtract, [best], [e16])
                    ACT(e16.t[:], e16.t[:], AF.Exp, [e16], [e16])
                    S.op('dve', lambda e: e.tensor_reduce(out=Z.t[:], in_=e16.t[:], axis=mybir.AxisListType.X, op=ALU.add), bl([e16]), bl([Z]))
                    ACT(lnZ.t[:], Z.t[:], AF.Ln, [Z], [lnZ])
                    TT('dve', cbias.t[:, tt * 8:(tt + 1) * 8], negm.t[:], lnZ.t[:], ALU.subtract, [negm, lnZ], [cbias])
                    TS('dve', tau.t[:, tt * 8:(tt + 1) * 8], tau.t[:, tt * 8:(tt + 1) * 8], -1.0e-6, None, ALU.add, None, [tau], [tau])
                    TS('dve', ntau.t[:, tt * 8:(tt + 1) * 8], tau.t[:, tt * 8:(tt + 1) * 8], -1.0, None, ALU.mult, None, [tau], [ntau])
                    TT('dve', b3.t[:, tt * 8:(tt + 1) * 8], tau.t[:, tt * 8:(tt + 1) * 8], cbias.t[:, tt * 8:(tt + 1) * 8], ALU.add, [tau, cbias], [b3])
                S.barrier()
                S.flush()
            acc = ph.sb("acc", [128, 16 * 512])
            ubf2 = [ph.sb("ubf%d" % i, [128, 16, 512], BF16) for i in range(1)]
            vbf2 = [ph.sb("vbf%d" % i, [128, 4, 2048], BF16) for i in range(2)]
            gate = [ph.sb("gate%d" % i, [128, 8, 512], BF16) for i in range(2)]
            sc = [ph.sb("sc%d" % i, [128, 512]) for i in range(3)]
            ex = [ph.sb("ex%d" % i, [128, 512]) for i in range(3)]
            pst_s = s12.t[:].ap[0][0]
            GT = [ph.sb("GT%d" % i, [128, 4, 512], BF16) for i in range(2)]
            ge = [ph.sb("ge%d" % i, [128, 512]) for i in range(2)]
            WT = [ph.sb("WT%d" % i, [128, 4, 512], BF16) for i in range(4)]
            pG = ph.ps[0:2]; pA = ph.ps[2:4]; pD = ph.ps[4:8]
            NG = 32
            deferred = []
            gu = [0]
            kk = [0]
            pend = []

            def flush_pend():
                while pend:
                    g2, h2, s2_, e2_, t2 = pend.pop(0)
                    STT(g2.t[:, h2, :], s2_.t[:], tau.t[:, t2 * 8 + h2:t2 * 8 + h2 + 1], e2_.t[:], ALU.is_ge, ALU.mult, [s2_, tau, e2_], [g2])

            def gate_unit(G, u):
                tt, h = u // 8, u % 8
                k = kk[0] % 3
                kk[0] += 1
                s_ = sc[k]; e_ = ex[k]
                gt_ = gate[(G * 4 + tt) % 2]
                in0 = bass.AP(s12.t, tt * 2048 + (2 * h + 1) * 128, [[pst_s, 128], [0, 4], [1, 128]])
                in1 = bass.AP(s12.t, tt * 2048 + (2 * h) * 128 + G * 4, [[pst_s, 128], [1, 4], [0, 128]])
                TT('dve', s_.t[:].rearrange("p (a b) -> p a b", b=128), in0, in1, ALU.add, [s12], [s_])
                c_ = tt * 8 + h
                if h >= 3:
                    S.op('act', lambda e: e.activation(out=e_.t[:], in_=s_.t[:], func=AF.Prelu, bias=ntau.t[:, c_:c_ + 1], scale=1.0, alpha=1.0e9),
                         bl([s_, ntau]), bl([e_]))
                    ACT(gt_.t[:, h, :], e_.t[:], AF.Exp, [e_, b3], [gt_], bias=b3.t[:, c_:c_ + 1])
                    flush_pend()
                    return
                ACT(e_.t[:], s_.t[:], AF.Exp, [s_, cbias], [e_], bias=cbias.t[:, tt * 8 + h:tt * 8 + h + 1])
                pend.append((gt_, h, s_, e_, tt))
                if len(pend) > 1:
                    g2, h2, s2_, e2_, t2 = pend.pop(0)
                    STT(g2.t[:, h2, :], s2_.t[:], tau.t[:, t2 * 8 + h2:t2 * 8 + h2 + 1], e2_.t[:], ALU.is_ge, ALU.mult, [s2_, tau, e2_], [g2])

            def ident_mm(G, tt):
                gt_ = gate[(G * 4 + tt) % 2]
                pg = pG[(G * 4 + tt) % 2]
                for i in range(4):
                    for hh in range(8):
                        MM(pg.t[:, i * 128:(i + 1) * 128], gt_.t[:, hh, i * 128:(i + 1) * 128], ident.t[:], hh == 0, hh == 7, [gt_, ident], [pg])
                CP('act', GT[G % 2].t[:, :, tt * 128:(tt + 1) * 128], pg.t[:].rearrange("p (a b) -> p a b", b=128), [pg], [GT[G % 2]])

            def load_u(G):
                ub = ubf2[0]
                for q in range(4):
                    DMA('pool', ub.t[:, 4 * q:4 * q + 4, :],
                        puT[q * 512:(q + 1) * 512, G * 512:(G + 1) * 512].rearrange("(k p) c -> p k c", p=128), [], [ub], ub.b.name)

            def load_v(G):
                vb = vbf2[G % 2]
                for d in range(4):
                    DMA('pool', vb.t[:, d, :], pv[G * 512 + d * 128:G * 512 + d * 128 + 128, :], [], [vb], vb.b.name)

            def act_chain(G, i):
                pa = pA[i % 2]
                ub = ubf2[0]
                for kt in range(16):
                    MM(pa.t[:], ub.t[:, kt, i * 128:(i + 1) * 128], hn.t[:, kt * 512:(kt + 1) * 512], kt == 0, kt == 15, [ub, hn], [pa])
                g_ = ge[i % 2]
                ACT(g_.t[:], pa.t[:], AF.Gelu_apprx_tanh, [pa], [g_])
                TT('pool', WT[G % 4].t[:, i, :], g_.t[:], GT[G % 2].t[:, i, :], ALU.mult, [g_, GT[G % 2]], [WT[G % 4]])

            def v_chain(Ga, dt):
                pd = pD[dt % 4]
                n = 0
                for Gx in (Ga, Ga + 1):
                    vb = vbf2[Gx % 2]; wt = WT[Gx % 4]
                    for d in range(4):
                        MM(pd.t[:], vb.t[:, d, dt * 128:(dt + 1) * 128], wt.t[:, d, :], n == 0, n == 7, [vb, wt], [pd])
                        n += 1

                def add():
                    if Ga == 0:
                        CP('dve', acc.t[:, dt * 512:(dt + 1) * 512], pd.t[:], [pd], [acc])
                    else:
                        TT('dve', acc.t[:, dt * 512:(dt + 1) * 512], pd.t[:], acc.t[:, dt * 512:(dt + 1) * 512], ALU.add, [pd, acc], [acc])
                deferred.append((gu[0] + 3, add))

            def run_deferred(force=False):
                while deferred and (force or deferred[0][0] <= gu[0]):
                    deferred.pop(0)[1]()

            load_u(0); load_v(0); load_v(1)
            for G in range(NG + 1):
                for u in range(32):
                    gu[0] = G * 32 + u
                    if G < NG:
                        gate_unit(G, u)
                    elif u == 0:
                        flush_pend()
                    run_deferred()
                    if u % 8 == 4:
                        tq = u // 8 - 1
                        if tq >= 0 and G < NG:
                            ident_mm(G, tq)
                        elif tq < 0 and G >= 1:
                            ident_mm(G - 1, 3)
                    if G >= 1:
                        Gp = G - 1
                        if u in (6, 8, 10, 12):
                            act_chain(Gp, (u - 6) // 2)
                        if u == 13 and G < NG:
                            load_u(G)
                        if G % 2 == 0:
                            if 14 <= u < 30:
                                v_chain(G - 2, u - 14)
                            if u == 30:
                                if G < NG:
                                    load_v(G)
                                if G + 1 < NG:
                                    load_v(G + 1)
            run_deferred(force=True)
            hx = [ph.sb("hx%d" % i, [128, 512]) for i in range(2)]
            for dt in range(16):
                x_ = hx[dt % 2]
                DMA('sp', x_.t[:], h1_d[dt * 128:(dt + 1) * 128, T0:T0 + 512], [], [x_], x_.b.name)
                TT('dve', x_.t[:], x_.t[:], acc.t[:, dt * 512:(dt + 1) * 512], ALU.add, [x_, acc], [x_])
                DMA('pool', h2_d[dt * 128:(dt + 1) * 128, T0:T0 + 512], x_.t[:], [x_], [], x_.b.name)

    with phase() as ph:
        vec, ident, ones = consts(ph)
        ph.psring(8, "ep")
        h2 = ph.sb("h2", [128, 16384])
        hn3 = ph.sb("hn3", [128, 16384], BF16)
        load_norm_own(ph, ones, vec, h2_d, C_PLE, hn3, keep=h2)
        stgs = None
        wbfs = [ph.sb("wbf%d" % i, [128, 2048], BF16) for i in range(4)]
        gsb = ph.sb("gsb", [128, 16384], BF16)

        def ev_g(ot, th, p):
            sl = slice(ot * 1024 + th * 512, ot * 1024 + th * 512 + 512)
            ACT(gsb.t[:, sl], p.t[:], AF.Sigmoid, [p], [gsb])
        linearT(ph, w_pg, 16, 2048, hn3, 1024, ev_g, stgs, wbfs, cb=128)
        pb = ph.sb("pb", [128, 2048], BF16)
        DMA('pool', pb.t[:].rearrange("p (k t) -> p k t", t=1024), poT.rearrange("(k p) t -> p k t", p=128), [], [pb], 'pb')
        et = [ph.sb("et%d" % i, [128, 512]) for i in range(2)]
        ce = [0]

        def ev_p(ot, th, p):
            t_ = et[ce[0] % 2]; ce[0] += 1
            sl = slice(ot * 1024 + th * 512, ot * 1024 + th * 512 + 512)
            TT('dve', t_.t[:], p.t[:], gsb.t[:, sl], ALU.mult, [p, gsb], [t_])
            TT('dve', h2.t[:, sl], t_.t[:], h2.t[:, sl], ALU.add, [t_, h2], [h2])
        linearT(ph, w_pp, 2, 2048, pb, 1024, ev_p, stgs, wbfs, cb=128)
        sq = ph.sb("fsq", [128, 8192], BF16)
        rt = ph.sb("frt", [128, 512]); rstd = ph.sb("frs", [128, 512])
        ob = [ph.sb("ob%d" % i, [128, 512]) for i in range(2)]
        for th in range(2):
            psq = ph.nps()
            for kt in range(16):
                ACT(sq.t[:, kt * 512:(kt + 1) * 512], h2.t[:, kt * 1024 + th * 512:kt * 1024 + th * 512 + 512], AF.Square, [h2], [sq])
            for kt in range(16):
                MM(psq.t[:], ones.t[:], sq.t[:, kt * 512:(kt + 1) * 512], kt == 0, kt == 15, [ones, sq], [psq])
            ACT(rt.t[:], psq.t[:], AF.Sqrt, [psq, vec], [rt], bias=vec.t[:, C_EPS:C_EPS + 1], scale=1.0 / 2048.0)
            RCP(rstd.t[:], rt.t[:], [rt], [rstd])
            for kt in range(16):
                o_ = ob[kt % 2]
                STT(o_.t[:], h2.t[:, kt * 1024 + th * 512:kt * 1024 + th * 512 + 512], vec.t[:, C_FIN + kt:C_FIN + kt + 1], rstd.t[:],
                    ALU.mult, ALU.mult, [h2, vec, rstd], [o_])
                DMA('sp', outT[kt * 128:(kt + 1) * 128, th * 512:(th + 1) * 512], o_.t[:], [o_], [], o_.b.name)

    topstack.close()
    return nc


def _cm(v, n):
    return np.ascontiguousarray(np.asarray(v, np.float32).reshape(n, 128).T)


def prep_inputs(inp):
    f = lambda a: np.ascontiguousarray(np.asarray(a, dtype=np.float32))
    x = f(inp['x'])[0]
    p = f(inp['p'])[0, 0]
    xT = np.ascontiguousarray(x.T)
    w_in = f(inp['w_in'])[0]
    perm = np.concatenate([np.arange(32, 64), np.arange(0, 32)])
    w_krsw = np.ascontiguousarray(w_in[:, 1280:1344][:, perm])
    w_uq = f(inp['w_uq'])[0]
    w_uqsw = np.ascontiguousarray(w_uq.reshape(768, 16, 192)[:, :, 128:][:, :, perm].reshape(768, 1024))
    half = 32
    inv_freq = (1.0 / (np.float32(10000.0) ** (np.arange(half, dtype=np.float32) / np.float32(half)))).astype(np.float32)

    def tables(pos):
        ang = pos.astype(np.float32)[:, None] * inv_freq[None, :]
        c = np.cos(ang).astype(np.float32).T
        s = np.sin(ang).astype(np.float32).T
        return np.concatenate([c, c], 0), np.concatenate([-s, s], 0)
    vec_common = np.zeros((128, NV), np.float32)
    vec_common[:, C_ATTN:C_ATTN + 16] = _cm(inp['attn_norm'][0], 16)
    vec_common[:, C_QN:C_QN + 6] = _cm(inp['q_norm'][0], 6)
    vec_common[:, C_KVN:C_KVN + 4] = _cm(inp['kv_norm'][0], 4)
    vec_common[:, C_CB:C_CB + 16] = _cm(inp['conv_b'][0], 16)
    cw = np.asarray(inp['conv_w'], np.float32)[0]
    vec_common[:, C_CW:C_CW + 64] = cw.T.reshape(16, 128, 4).transpose(1, 0, 2).reshape(128, 64)
    vec_common[:, C_BA:C_BA + 16] = _cm(np.asarray(inp['b_rg_a'])[0].reshape(-1), 16)
    vec_common[:, C_BX:C_BX + 16] = _cm(np.asarray(inp['b_rg_x'])[0].reshape(-1), 16)
    vec_common[:, C_LAM:C_LAM + 16] = _cm(inp['lru_lambda'][0], 16)
    bg = np.asarray(inp['b_gate'], np.float32)[0]
    vec_common[:, C_BGA:C_BGA + 16] = _cm(bg[:2048], 16)
    vec_common[:, C_BGR:C_BGR + 16] = _cm(bg[2048:], 16)
    vec_common[:, C_FFN:C_FFN + 16] = _cm(inp['ffn_norm'][0], 16)
    vec_common[:, C_PLE:C_PLE + 16] = _cm(inp['ple_norm'][0], 16)
    vec_common[:, C_FIN:C_FIN + 16] = _cm(inp['final_norm'], 16)
    vec_common[:, C_EPS] = 1e-6
    vec_common[:, C_ONE] = 1.0
    vec_common[:, C_M1] = -1.0
    shared = {
        'xT': xT, 'w_in': w_in, 'w_krsw': w_krsw, 'w_uq': w_uq, 'w_uqsw': w_uqsw,
        'w_ukv': f(inp['w_ukv'])[0], 'w_attn_o': f(inp['w_attn_o'])[0],
        'w_rg_a': f(inp['w_rg_a'])[0], 'w_rg_x': f(inp['w_rg_x'])[0],
        'w_rnn_o': f(inp['w_rnn_o'])[0], 'w_out': f(inp['w_out'])[0], 'w_peer_q': f(inp['w_peer_q'])[0],
        'skT': np.ascontiguousarray(f(inp['peer_subkeys'])[0].reshape(16, 128, 128).transpose(2, 0, 1)),
        'puT': np.ascontiguousarray(f(inp['peer_u'])[0].T), 'pv': f(inp['peer_v'])[0],
        'w_ple_gate': f(inp['w_ple_gate'])[0], 'w_ple_proj': f(inp['w_ple_proj'])[0],
    }
    cg, sg = tables(np.arange(8192))
    maps = []
    for c in range(8):
        m = dict(shared)
        m['xoT'] = np.ascontiguousarray(xT[:, 1024 * c:1024 * (c + 1)])
        m['poT'] = np.ascontiguousarray(p[1024 * c:1024 * (c + 1)].T)
        v = vec_common.copy()
        for j in range(16):
            v[:, C_VIS + j] = 0.0 if j < 2 * c else NEG
            for hf in range(2):
                v[:, C_OWN + 2 * j + hf] = 1.0 if j == 2 * c + hf else 0.0
        m['vecs'] = v
        cs_ = np.zeros((2, 64, TK), np.float32)
        cs_[0, :, :8192] = cg; cs_[1, :, :8192] = sg
        cs_[0, :, 8192:] = cg[:, 1024 * c:1024 * (c + 1)]; cs_[1, :, 8192:] = sg[:, 1024 * c:1024 * (c + 1)]
        m['cs'] = cs_
        maps.append(m)
    return maps


def kernel(**inputs):
    maps = prep_inputs(inputs)
    nc = build_nc(False)
    res = run_bass_kernel_spmd(nc, maps, core_ids=list(range(8)))
    outT = np.concatenate([np.asarray(r["outT"]) for r in res.results], axis=1)
    return np.ascontiguousarray(outT.T)[None].astype(np.float32)
```

```python
import numpy as np
from contextlib import ExitStack, contextmanager
import concourse.bass as bass
import concourse.mybir as mybir
from concourse.bass_utils import run_bass_kernel_spmd

F32 = mybir.dt.float32
BF16 = mybir.dt.bfloat16
AF = mybir.ActivationFunctionType
ALU = mybir.AluOpType

NV = 288
C_ATTN, C_QN, C_KVN, C_CB, C_CW, C_BA, C_BX, C_LAM, C_BGA, C_BGR, C_FFN, C_PLE, C_FIN = \
    0, 16, 22, 26, 42, 106, 122, 138, 154, 170, 186, 202, 218
C_EPS, C_ONE, C_ZERO, C_M1, C_VIS, C_OWN = 234, 235, 236, 237, 240, 256
NEG = -30000.0
NCH = 18
TK = 9216


class Buf:
    def __init__(self, name):
        self.name = name
        self.w = None
        self.r = {}


class Tl:
    def __init__(self, t, name):
        self.t = t
        self.b = Buf(name)


_uid = [0]


def _un(name):
    _uid[0] += 1
    return "%s_%d" % (name, _uid[0])


class Sched:
    ENG = ('pe', 'act', 'dve', 'pool', 'sp')

    def __init__(self, nc, stack):
        self.nc = nc
        self.stack = stack
        self.prog = {k: [] for k in self.ENG}
        self.sems = {}
        self.cnt = {}
        self.waited = {k: {} for k in self.ENG}
        self.nsem = 0
        for k in self.ENG:
            self._newsem(k)

    def _newsem(self, key):
        s = self.stack.enter_context(self.nc.semaphore("s%d_%s" % (self.nsem, key[:12])))
        self.nsem += 1
        self.sems[key] = s
        self.cnt[key] = 0
        for e in self.ENG:
            self.waited[e].pop(key, None)

    def _need(self, eng, dep, waits):
        if dep is None:
            return
        key, sem, val = dep
        if self.sems.get(key) is not sem:
            return
        if key == 'pe' and eng == 'pe':
            return
        if self.waited[eng].get(key, 0) >= val:
            return
        waits[key] = max(waits.get(key, 0), val)

    def op(self, eng, fn, reads=(), writes=(), dma_key=None):
        waits = {}
        for b in reads:
            self._need(eng, b.w, waits)
        for b in writes:
            self._need(eng, b.w, waits)
            for k, (s, v) in b.r.items():
                self._need(eng, (k, s, v), waits)
        for k, v in waits.items():
            self.waited[eng][k] = v
        if dma_key is None:
            key, inc = eng, 1
        else:
            key, inc = dma_key, 16
            if key not in self.sems:
                self._newsem(key)
        self.cnt[key] += inc
        val = self.cnt[key]
        assert val < 60000, (key, val)
        sem = self.sems[key]
        self.prog[eng].append(([(self.sems[k], v) for k, v in waits.items()], fn, sem, inc))
        for b in reads:
            b.r[key] = (sem, val)
        for b in writes:
            b.w = (key, sem, val)
            b.r = {}

    def barrier(self, new_epoch=True):
        for eng in self.ENG:
            waits = {}
            for k, v in self.cnt.items():
                if v > 0:
                    self._need(eng, (k, self.sems[k], v), waits)
            for k, v in waits.items():
                self.waited[eng][k] = v
            if waits:
                self.prog[eng].append(([(self.sems[k], v) for k, v in waits.items()], None, None, 0))

    def flush(self):
        progs = self.prog
        with self.nc.Block() as block:
            def mk(engname):
                def body(engine):
                    for waits, fn, sem, inc in progs[engname]:
                        for s, v in waits:
                            engine.wait_ge(s, v)
                        if fn is not None:
                            fn(engine).then_inc(sem, inc)
                return body
            block.tensor(mk('pe'))
            block.scalar(mk('act'))
            block.vector(mk('dve'))
            block.gpsimd(mk('pool'))
            block.sync(mk('sp'))
        self.prog = {k: [] for k in self.ENG}


def build_nc(debug=False):
    nc = bass.Bass("TRN2", target_bir_lowering=False)

    def din(name, shape, dt=F32):
        return nc.dram_tensor(name, list(shape), dt, kind="ExternalInput").ap()

    def dscr(name, shape, dt):
        return nc.dram_tensor(name, list(shape), dt, kind="ExternalOutput" if debug else "Internal").ap()

    xT = din("xT", [2048, 8192]); xoT = din("xoT", [2048, 1024]); poT = din("poT", [256, 1024])
    vecs = din("vecs", [128, NV]); cs = din("cs", [2, 64, TK])
    w_in = din("w_in", [2048, 9536]); w_krsw = din("w_krsw", [2048, 64])
    w_uq = din("w_uq", [768, 3072]); w_uqsw = din("w_uqsw", [768, 1024])
    w_ukv = din("w_ukv", [512, 4096]); w_ao = din("w_attn_o", [2048, 2048])
    w_rga = din("w_rg_a", [16, 128, 128]); w_rgx = din("w_rg_x", [16, 128, 128])
    w_ro = din("w_rnn_o", [2048, 2048]); w_out = din("w_out", [2048, 2048]); w_pq = din("w_peer_q", [2048, 2048])
    skT = din("skT", [128, 16, 128]); puT = din("puT", [2048, 16384]); pv = din("pv", [16384, 2048])
    w_pg = din("w_ple_gate", [2048, 2048]); w_pp = din("w_ple_proj", [256, 2048])
    outT = nc.dram_tensor("outT", [2048, 1024], F32, kind="ExternalOutput").ap()

    nT_all = dscr("nT_all", [2048, 8192], BF16)
    Kt_all = dscr("Kt_all", [16, 128, TK], BF16)
    V_all = dscr("V_all", [16, 128, 72, 128], BF16)
    kpe_all = dscr("kpe_all", [64, TK], BF16)
    QT_all = dscr("QT_all", [16, 128, 1024], BF16)
    QPE_all = dscr("QPE_all", [16, 64, 1024], BF16)
    OT_all = dscr("OT_all", [16, 128, 1024], BF16)
    hown_d = dscr("hown_d", [2048, 1024], BF16)
    nown_d = dscr("nown_d", [2048, 1024], BF16)
    h1_d = dscr("h1_d", [2048, 1024], F32)
    h2_d = dscr("h2_d", [2048, 1024], F32)

    topstack = ExitStack()
    S = Sched(nc, topstack)

    def bl(x):
        return [y.b if isinstance(y, Tl) else y for y in x]

    def MM(ps, lhsT, rhs, start, stop, R, W):
        S.op('pe', lambda e: e.matmul(ps, lhsT=lhsT, rhs=rhs, start=start, stop=stop), bl(R), bl(W))

    def TR(ps, in_, ident, R, W):
        S.op('pe', lambda e: e.transpose(out=ps, in_=in_, identity=ident), bl(R), bl(W))

    def ACT(out, in_, func, R, W, bias=None, scale=None):
        kw = {}
        if bias is not None:
            kw['bias'] = bias
        if scale is not None:
            kw['scale'] = scale
        S.op('act', lambda e: e.activation(out=out, in_=in_, func=func, **kw), bl(R), bl(W))

    def TT(eng, out, in0, in1, op, R, W):
        S.op(eng, lambda e: e.tensor_tensor(out=out, in0=in0, in1=in1, op=op), bl(R), bl(W))

    def TS(eng, out, in0, s1, s2, op0, op1, R, W):
        if s2 is None:
            S.op(eng, lambda e: e.tensor_scalar(out=out, in0=in0, scalar1=s1, scalar2=None, op0=op0), bl(R), bl(W))
        else:
            S.op(eng, lambda e: e.tensor_scalar(out=out, in0=in0, scalar1=s1, scalar2=s2, op0=op0, op1=op1), bl(R), bl(W))

    def STT(out, in0, scalar, in1, op0, op1, R, W):
        S.op('dve', lambda e: e.scalar_tensor_tensor(out=out, in0=in0, scalar=scalar, in1=in1, op0=op0, op1=op1), bl(R), bl(W))

    def CP(eng, out, in_, R, W):
        if eng == 'act':
            S.op('act', lambda e: e.copy(out=out, in_=in_), bl(R), bl(W))
        else:
            S.op(eng, lambda e: e.tensor_copy(out=out, in_=in_), bl(R), bl(W))

    def RCP(out, in_, R, W):
        S.op('dve', lambda e: e.reciprocal(out=out, in_=in_), bl(R), bl(W))

    def MEMSET(eng, ap, val, W):
        S.op(eng, lambda e: e.memset(ap, val), [], bl(W))

    def DMA(eng, out, in_, R, W, key):
        S.op(eng, lambda e: e.dma_start(out=out, in_=in_), bl(R), bl(W), dma_key=key)

    class Phase:
        def __init__(self):
            self.st = ExitStack()
            self.ps = []
            self.psi = 0

        def sb(self, name, shape, dt=F32):
            return Tl(self.st.enter_context(nc.sbuf_tensor(_un(name), list(shape), dt)), name)

        def psum(self, name, shape, dt=F32):
            return Tl(self.st.enter_context(nc.psum_tensor(_un(name), list(shape), dt)), name)

        def psring(self, n, prefix):
            self.ps = [self.psum("%s%d" % (prefix, i), [128, 512]) for i in range(n)]
            self.psi = 0

        def nps(self):
            p = self.ps[self.psi % len(self.ps)]
            self.psi += 1
            return p

    @contextmanager
    def phase():
        ph = Phase()
        try:
            yield ph
            S.barrier()
            S.flush()
        finally:
            ph.st.close()
        for k in S.ENG:
            if S.cnt[k] > 28000:
                S._newsem(k)

    cnt = {'cast': 0, 'ev': 0}

    def cast_eng():
        cnt['cast'] += 1
        return ('pool', 'dve', 'act')[cnt['cast'] % 3] if False else 'pool'

    def ev_eng():
        cnt['ev'] += 1
        return 'act' if cnt['ev'] % 2 else 'dve'

    def consts(ph):
        vec = ph.sb("vec", [128, NV])
        DMA('sp', vec.t[:], vecs, [], [vec], 'vec')
        idf = ph.sb("idf", [128, 128])
        MEMSET('pool', idf.t[:], 0.0, [idf])
        S.op('pool', lambda e: e.affine_select(out=idf.t[:], in_=idf.t[:], pattern=[[-1, 128]],
                                               compare_op=ALU.not_equal, fill=1.0, base=0, channel_multiplier=1),
             [idf.b], [idf.b])
        ident = ph.sb("ident", [128, 128], BF16)
        CP('dve', ident.t[:], idf.t[:], [idf], [ident])
        ones = ph.sb("ones", [128, 128], BF16)
        MEMSET('pool', ones.t[:], 1.0, [ones])
        return vec, ident, ones

    def rmsnorm_T(ph, ones, vec, src_tiles, nkt, T, gcol, dst, D, sq, rt, rstd, psq):
        for kt in range(nkt):
            ap, tl = src_tiles(kt)
            ACT(sq.t[:, kt * T:(kt + 1) * T], ap, AF.Square, [tl], [sq])
        for kt in range(nkt):
            MM(psq.t[:, :T], ones.t[:], sq.t[:, kt * T:(kt + 1) * T], kt == 0, kt == nkt - 1, [ones, sq], [psq])
        ACT(rt.t[:, :T], psq.t[:, :T], AF.Sqrt, [psq, vec], [rt], bias=vec.t[:, C_EPS:C_EPS + 1], scale=1.0 / D)
        RCP(rstd.t[:, :T], rt.t[:, :T], [rt], [rstd])
        for kt in range(nkt):
            ap, tl = src_tiles(kt)
            STT(dst.t[:, kt * T:(kt + 1) * T], ap, vec.t[:, gcol + kt:gcol + kt + 1], rstd.t[:, :T], ALU.mult, ALU.mult,
                [tl, vec, rstd], [dst])

    with phase() as ph:
        vec, ident, ones = consts(ph)
        ph.psring(8, "g1p")
        wkv = ph.sb("wkv", [128, 16, 640], BF16)
        wk = ph.sb("wk", [128, 4, 2048], BF16)
        wv = ph.sb("wv", [128, 4, 2048], BF16)
        for kt in range(16):
            DMA('pool', wkv.t[:, kt, 0:576], w_in[kt * 128:(kt + 1) * 128, 768:1344], [], [wkv], 'wkv')
            DMA('pool', wkv.t[:, kt, 576:640], w_krsw[kt * 128:(kt + 1) * 128, :], [], [wkv], 'wkv')
        for kt in range(4):
            sv = w_ukv[kt * 128:(kt + 1) * 128, :].rearrange("p (h two d) -> p h two d", two=2, d=128)
            DMA('pool', wk.t[:, kt, :].rearrange("p (h d) -> p h d", d=128), sv[:, :, 0, :], [], [wk], 'wk')
            DMA('pool', wv.t[:, kt, :].rearrange("p (h d) -> p h d", d=128), sv[:, :, 1, :], [], [wv], 'wv')
        xp = [ph.sb("xp%d" % i, [128, 2048]) for i in range(4)]
        sq = ph.sb("sq", [128, 8192], BF16)
        nT = ph.sb("nT", [128, 8192], BF16)
        rt = ph.sb("rt", [128, 512]); rstd = ph.sb("rstd", [128, 512])
        rt2 = ph.sb("rt2", [128, 512]); rstd2 = ph.sb("rstd2", [128, 512])
        sqk = ph.sb("sqk", [128, 2048], BF16)
        ckvn2 = [ph.sb("ckvn%d" % i, [128, 2048], BF16) for i in range(2)]
        cst = ph.sb("cst", [64, 2, 512])
        t1 = ph.sb("t1", [64, 512]); t2 = ph.sb("t2", [64, 512])
        kpst = ph.sb("kpst", [64, 512], BF16)
        Kst = ph.sb("Kst", [128, 16, 512], BF16)
        Vst = ph.sb("Vst", [128, 4, 2048], BF16)
        P = ph.ps
        kvr = [0]

        def kvps():
            p = P[6 + (kvr[0] % 2)]
            kvr[0] += 1
            return p

        def partA(j):
            src = xT[:, 512 * j:512 * j + 512] if j < 16 else xoT[:, 512 * (j - 16):512 * (j - 16) + 512]
            for q in range(4):
                DMA('sp', xp[q].t[:].rearrange("p (k t) -> p k t", t=512),
                    src[q * 512:(q + 1) * 512, :].rearrange("(k p) t -> p k t", p=128), [], [xp[q]], xp[q].b.name)
            DMA('sp', cst.t[:], cs[:, :, 512 * j:512 * j + 512].rearrange("c p t -> p c t"), [], [cst], 'cst')
            rmsnorm_T(ph, ones, vec, lambda kt: (xp[kt // 4].t[:, (kt % 4) * 512:(kt % 4 + 1) * 512], xp[kt // 4]),
                      16, 512, C_ATTN, nT, 2048.0, sq, rt, rstd, P[0])
            if j < 16:
                DMA('pool', nT_all[:, 512 * j:512 * j + 512].rearrange("(k p) t -> p k t", p=128),
                    nT.t[:].rearrange("p (k t) -> p k t", t=512), [nT], [], 'nTst')

        def partB(j):
            ckvn = ckvn2[j % 2]
            pck = P[1:5]
            pkr = P[5]
            for o in range(4):
                for kt in range(16):
                    MM(pck[o].t[:], wkv.t[:, kt, o * 128:(o + 1) * 128], nT.t[:, kt * 512:(kt + 1) * 512], kt == 0, kt == 15,
                       [wkv, nT], [pck[o]])
            for kt in range(16):
                MM(pkr.t[0:64, :], wkv.t[:, kt, 512:576], nT.t[:, kt * 512:(kt + 1) * 512], kt == 0, kt == 15, [wkv, nT], [pkr])
            TT('dve', t1.t[:], pkr.t[0:64, :], cst.t[:, 0, :], ALU.mult, [pkr, cst], [t1])
            for kt in range(16):
                MM(pkr.t[0:64, :], wkv.t[:, kt, 576:640], nT.t[:, kt * 512:(kt + 1) * 512], kt == 0, kt == 15, [wkv, nT], [pkr])
            TT('dve', t2.t[:], pkr.t[0:64, :], cst.t[:, 1, :], ALU.mult, [pkr, cst], [t2])
            TT('dve', kpst.t[:], t1.t[:], t2.t[:], ALU.add, [t1, t2], [kpst])
            DMA('pool', kpe_all[:, 512 * j:512 * j + 512], kpst.t[:], [kpst], [], 'kpst')
            rmsnorm_T(ph, ones, vec, lambda o: (pck[o].t[:], pck[o]), 4, 512, C_KVN, ckvn, 512.0, sqk, rt2, rstd2, P[0])

        def partK(j):
            ckvn = ckvn2[j % 2]
            for h in range(16):
                p = kvps()
                for kt in range(4):
                    MM(p.t[:], wk.t[:, kt, h * 128:(h + 1) * 128], ckvn.t[:, kt * 512:(kt + 1) * 512], kt == 0, kt == 3, [wk, ckvn], [p])
                CP('act', Kst.t[:, h, :], p.t[:], [p], [Kst])
            DMA('pool', Kt_all[:, :, 512 * j:512 * j + 512].rearrange("h p t -> p h t"), Kst.t[:], [Kst], [], 'Kst')

        def partV(j):
            ckvn = ckvn2[j % 2]
            for tt in range(4):
                for cg in range(4):
                    p = kvps()
                    for kt in range(4):
                        MM(p.t[:], ckvn.t[:, kt * 512 + tt * 128:kt * 512 + tt * 128 + 128], wv.t[:, kt, cg * 512:(cg + 1) * 512],
                           kt == 0, kt == 3, [wv, ckvn], [p])
                    CP(ev_eng(), Vst.t[:, tt, cg * 512:(cg + 1) * 512], p.t[:], [p], [Vst])
            for tt in range(4):
                DMA('pool', V_all[:, :, 4 * j + tt, :].rearrange("h p d -> p h d"),
                    Vst.t[:, tt, :].rearrange("p (h d) -> p h d", d=128), [Vst], [], 'Vst')

        for j in range(NCH + 1):
            if j < NCH:
                partA(j)
            if j >= 1:
                partK(j - 1)
            if j < NCH:
                partB(j)
            if j >= 1:
                partV(j - 1)

    with phase() as ph:
        vec, ident, ones = consts(ph)
        ph.psring(6, "g2p")
        wxr = ph.sb("wxr", [128, 16, 2048], BF16)
        wga = ph.sb("wga", [128, 16, 128], BF16)
        wgx = ph.sb("wgx", [128, 16, 128], BF16)
        for kt in range(16):
            DMA('pool', wxr.t[:, kt, :], w_in[kt * 128:(kt + 1) * 128, 1344:3392], [], [wxr], 'wxr')
        DMA('pool', wga.t[:], w_rga.rearrange("b i j -> i b j"), [], [wga], 'wga')
        DMA('pool', wgx.t[:], w_rgx.rearrange("b i j -> i b j"), [], [wgx], 'wgx')
        cf = ph.sb("cf", [128, 48])
        ACT(cf.t[:, 0:16], vec.t[:, C_LAM:C_LAM + 16], AF.Exp, [vec], [cf], scale=-1.0)
        ACT(cf.t[:, 0:16], cf.t[:, 0:16], AF.Ln, [cf, vec], [cf], bias=vec.t[:, C_ONE:C_ONE + 1])
        TS('dve', cf.t[:, 16:32], cf.t[:, 0:16], -8.0, None, ALU.mult, None, [cf], [cf])
        TS('dve', cf.t[:, 32:48], cf.t[:, 0:16], -16.0, None, ALU.mult, None, [cf], [cf])
        hown = ph.sb("hown", [128, 16, 1024], BF16)
        MEMSET('pool', hown.t[:], 0.0, [hown])
        carry = ph.sb("carry", [128, 16]); MEMSET('pool', carry.t[:], 0.0, [carry])
        halo = ph.sb("halo", [128, 16, 4]); MEMSET('pool', halo.t[:], 0.0, [halo])
        nTb = [ph.sb("nTb%d" % i, [128, 8192], BF16) for i in range(2)]
        TS('dve', cf.t[:, 0:16], cf.t[:, 16:32], 0.5, None, ALU.mult, None, [cf], [cf])
        hb = ph.sb("hb", [128, 32])
        TS('dve', hb.t[:, 0:16], vec.t[:, C_BA:C_BA + 16], 0.5, None, ALU.mult, None, [vec], [hb])
        TS('dve', hb.t[:, 16:32], vec.t[:, C_BX:C_BX + 16], 0.5, None, ALU.mult, None, [vec], [hb])
        xre = [ph.sb("xre%d" % i, [128, 516]) for i in range(3)]
        xc = [ph.sb("xc%d" % i, [128, 512]) for i in range(8)]
        xcb = [ph.sb("xcb%d" % i, [128, 512], BF16) for i in range(2)]
        rr = [ph.sb("rr%d" % i, [128, 512]) for i in range(2)]
        ig = [ph.sb("ig%d" % i, [128, 512]) for i in range(5)]
        aa = [ph.sb("aa%d" % i, [128, 512]) for i in range(4)]
        mu = [ph.sb("mu%d" % i, [128, 512]) for i in range(3)]
        bb = [ph.sb("bb%d" % i, [128, 512]) for i in range(2)]
        hs = [ph.sb("hs%d" % i, [128, 512]) for i in range(2)]
        nbl = set()

        def stA(n):
            j, ct = n // 16, n % 16
            nb = nTb[j % 2]
            if j not in nbl:
                nbl.add(j)
                DMA('sp', nb.t[:].rearrange("p (k t) -> p k t", t=512),
                    nT_all[:, 512 * j:512 * j + 512].rearrange("(k p) t -> p k t", p=128), [], [nb], nb.b.name)
            pxr = ph.ps[n % 2]
            for kt in range(16):
                MM(pxr.t[:], wxr.t[:, kt, ct * 128:(ct + 1) * 128], nb.t[:, kt * 512:(kt + 1) * 512], kt == 0, kt == 15, [wxr, nb], [pxr])

        def stB(n):
            j, ct = n // 16, n % 16
            x_ = xre[n % 3]; pxr = ph.ps[n % 2]
            CP('pool', x_.t[:, 0:3], halo.t[:, ct, 0:3], [halo], [x_])
            CP('act', x_.t[:, 3:515], pxr.t[:], [pxr], [x_])
            CP('pool', halo.t[:, ct, 0:3], x_.t[:, 512:515], [x_], [halo])

        def stC(n):
            j, ct = n // 16, n % 16
            x_ = xre[n % 3]; c_ = xc[n % 8]
            cw = C_CW + 4 * ct
            TS('dve', c_.t[:], x_.t[:, 0:512], vec.t[:, cw:cw + 1], vec.t[:, C_CB + ct:C_CB + ct + 1], ALU.mult, ALU.add,
               [x_, vec], [c_])
            for w in range(1, 4):
                STT(c_.t[:], x_.t[:, w:w + 512], vec.t[:, cw + w:cw + w + 1], c_.t[:], ALU.mult, ALU.add,
                    [x_, vec, c_], [c_])

        def stD(n):
            CP('pool', xcb[n % 2].t[:], xc[n % 8].t[:], [xc[n % 8]], [xcb[n % 2]])

        def stE(n):
            j, ct = n // 16, n % 16
            cb_ = xcb[n % 2]
            pr = ph.ps[2 + (n % 2)]; pi = ph.ps[4 + (n % 2)]
            MM(pr.t[:], wga.t[:, ct, :], cb_.t[:], True, True, [wga, cb_], [pr])
            MM(pi.t[:], wgx.t[:, ct, :], cb_.t[:], True, True, [wgx, cb_], [pi])

        def stF(n):
            j, ct = n // 16, n % 16
            pr = ph.ps[2 + (n % 2)]; pi = ph.ps[4 + (n % 2)]
            ACT(rr[n % 2].t[:], pr.t[:], AF.Tanh, [pr, hb], [rr[n % 2]], bias=hb.t[:, ct:ct + 1], scale=0.5)
            ACT(ig[n % 5].t[:], pi.t[:], AF.Tanh, [pi, hb], [ig[n % 5]], bias=hb.t[:, 16 + ct:17 + ct], scale=0.5)

        def stG(n):
            j, ct = n // 16, n % 16
            ACT(aa[n % 4].t[:], rr[n % 2].t[:], AF.Exp, [rr[n % 2], cf], [aa[n % 4]], bias=cf.t[:, ct:ct + 1], scale=cf.t[:, ct:ct + 1])

        def stH(n):
            TT('pool', mu[n % 3].t[:], aa[n % 4].t[:], aa[n % 4].t[:], ALU.mult, [aa[n % 4]], [mu[n % 3]])

        def stI(n):
            j, ct = n // 16, n % 16
            m_ = mu[n % 3]
            ACT(m_.t[:], m_.t[:], AF.Sqrt, [m_, vec], [m_], bias=vec.t[:, C_ONE:C_ONE + 1], scale=-1.0)

        def stJ(n):
            j, ct = n // 16, n % 16
            m_ = mu[n % 3]; b_ = bb[n % 2]; h_ = hs[n % 2]; c_ = xc[n % 8]; a_ = aa[n % 4]; i_ = ig[n % 5]
            if j == 0:
                MEMSET('dve', m_.t[:, 0:1], 1.0, [m_])
            STT(b_.t[:], i_.t[:], 1.0, m_.t[:], ALU.add, ALU.mult, [i_, m_], [b_])
            STT(b_.t[:], b_.t[:], 0.5, c_.t[:], ALU.mult, ALU.mult, [b_, c_], [b_])
            S.op('dve', lambda e: e.tensor_tensor_scan(out=h_.t[:], data0=a_.t[:], data1=b_.t[:], initial=carry.t[:, ct:ct + 1],
                                                       op0=ALU.mult, op1=ALU.add), bl([a_, b_, carry]), bl([h_]))
            CP('pool', carry.t[:, ct:ct + 1], h_.t[:, 511:512], [h_], [carry])
            for half in range(2):
                mc = C_OWN + 2 * j + half
                STT(hown.t[:, ct, half * 512:(half + 1) * 512], h_.t[:], vec.t[:, mc:mc + 1],
                    hown.t[:, ct, half * 512:(half + 1) * 512], ALU.mult, ALU.add, [h_, vec, hown], [hown])

        NU2 = 256
        stages = [stA, stB, stC, stD, stE, stF, stG, stH, stI, stJ]
        for m in range(NU2 + len(stages)):
            for k, st in enumerate(stages):
                n = m - k
                if 0 <= n < NU2:
                    st(n)
        DMA('sp', hown_d.rearrange("(k p) t -> p k t", p=128), hown.t[:], [hown], [], 'hownst')

    def linearT(ph, w_ap, KT, N, in_tl, T, evac, stgs, wbfs, cb=256):
        nblk = N // cb
        for b in range(nblk):
            wb = wbfs[b % len(wbfs)]
            DMA('pool', wb.t[:, :KT * cb].rearrange("p (k c) -> p k c", c=cb),
                w_ap[:, b * cb:(b + 1) * cb].rearrange("(k p) c -> p k c", p=128), [], [wb], wb.b.name)
            for o in range(cb // 128):
                for th in range(T // 512):
                    p = ph.nps()
                    for kt in range(KT):
                        MM(p.t[:], wb.t[:, kt * cb + o * 128:kt * cb + o * 128 + 128],
                           in_tl.t[:, kt * T + th * 512:kt * T + th * 512 + 512], kt == 0, kt == KT - 1, [wb, in_tl], [p])
                    evac(b * (cb // 128) + o, th, p)

    def load_norm_own(ph, ones, vec, src_d, gcol, dst, keep=None):
        sub = ExitStack()
        ph2 = Phase(); ph2.st = sub
        xh = [ph2.sb("lnx%d" % i, [128, 2048]) for i in range(4)]
        sq = ph2.sb("lnsq", [128, 8192], BF16)
        rt = ph2.sb("lnrt", [128, 512]); rstd = ph2.sb("lnrs", [128, 512])
        for th in range(2):
            for q in range(4):
                DMA('sp', xh[q].t[:].rearrange("p (k t) -> p k t", t=512),
                    src_d[q * 512:(q + 1) * 512, th * 512:(th + 1) * 512].rearrange("(k p) t -> p k t", p=128), [], [xh[q]], xh[q].b.name)
            psq = ph.nps()
            for kt in range(16):
                ACT(sq.t[:, kt * 512:(kt + 1) * 512], xh[kt // 4].t[:, (kt % 4) * 512:(kt % 4 + 1) * 512], AF.Square, [xh[kt // 4]], [sq])
            for kt in range(16):
                MM(psq.t[:], ones.t[:], sq.t[:, kt * 512:(kt + 1) * 512], kt == 0, kt == 15, [ones, sq], [psq])
            ACT(rt.t[:], psq.t[:], AF.Sqrt, [psq, vec], [rt], bias=vec.t[:, C_EPS:C_EPS + 1], scale=1.0 / 2048.0)
            RCP(rstd.t[:], rt.t[:], [rt], [rstd])
            for kt in range(16):
                STT(dst.t[:, kt * 1024 + th * 512:kt * 1024 + th * 512 + 512], xh[kt // 4].t[:, (kt % 4) * 512:(kt % 4 + 1) * 512],
                    vec.t[:, gcol + kt:gcol + kt + 1], rstd.t[:], ALU.mult, ALU.mult, [xh[kt // 4], vec, rstd], [dst])
                if keep is not None:
                    CP('pool', keep.t[:, kt * 1024 + th * 512:kt * 1024 + th * 512 + 512],
                       xh[kt // 4].t[:, (kt % 4) * 512:(kt % 4 + 1) * 512], [xh[kt // 4]], [keep])
        S.barrier()
        S.flush()
        sub.close()

    with phase() as ph:
        vec, ident, ones = consts(ph)
        ph.psring(8, "o1p")
        nown = ph.sb("nown", [128, 16384], BF16)
        load_norm_own(ph, ones, vec, xoT, C_ATTN, nown)
        DMA('pool', nown_d.rearrange("(k p) t -> p k t", p=128), nown.t[:].rearrange("p (k t) -> p k t", t=1024), [nown], [], 'nownst')
        stgs = None
        wbfs = [ph.sb("wbf%d" % i, [128, 4096], BF16) for i in range(4)]
        cq = ph.sb("cq", [128, 6 * 1024])
        linearT(ph, w_in[:, 0:768], 16, 768, nown, 1024,
                lambda ot, th, p: CP(ev_eng(), cq.t[:, ot * 1024 + th * 512:ot * 1024 + th * 512 + 512], p.t[:], [p], [cq]),
                stgs, wbfs)
        cqn = ph.sb("cqn", [128, 6 * 1024], BF16)
        sq6 = ph.sb("sq6", [128, 6 * 512], BF16)
        rt = ph.sb("rt", [128, 512]); rstd = ph.sb("rstd", [128, 512])
        for th in range(2):
            psq = ph.nps()
            for kt in range(6):
                ACT(sq6.t[:, kt * 512:(kt + 1) * 512], cq.t[:, kt * 1024 + th * 512:kt * 1024 + th * 512 + 512], AF.Square, [cq], [sq6])
            for kt in range(6):
                MM(psq.t[:], ones.t[:], sq6.t[:, kt * 512:(kt + 1) * 512], kt == 0, kt == 5, [ones, sq6], [psq])
            ACT(rt.t[:], psq.t[:], AF.Sqrt, [psq, vec], [rt], bias=vec.t[:, C_EPS:C_EPS + 1], scale=1.0 / 768.0)
            RCP(rstd.t[:], rt.t[:], [rt], [rstd])
            for kt in range(6):
                STT(cqn.t[:, kt * 1024 + th * 512:kt * 1024 + th * 512 + 512], cq.t[:, kt * 1024 + th * 512:kt * 1024 + th * 512 + 512],
                    vec.t[:, C_QN + kt:C_QN + kt + 1], rstd.t[:], ALU.mult, ALU.mult, [cq, vec, rstd], [cqn])
        cso = ph.sb("cso", [64, 2, 1024])
        DMA('sp', cso.t[:], cs[:, :, 8192:9216].rearrange("c p t -> p c t"), [], [cso], 'cso')
        qst = [ph.sb("qst%d" % i, [128, 1024], BF16) for i in range(2)]
        qpst = [ph.sb("qpst%d" % i, [64, 1024], BF16) for i in range(2)]
        q1 = ph.sb("q1", [64, 512]); q2 = ph.sb("q2", [64, 512])
        for h in range(16):
            wb = wbfs[h % 4]
            DMA('pool', wb.t[:, 0:6 * 256].rearrange("p (k c) -> p k c", c=256)[:, :, 0:192],
                w_uq[:, h * 192:(h + 1) * 192].rearrange("(k p) c -> p k c", p=128), [], [wb], wb.b.name)
            DMA('pool', wb.t[:, 0:6 * 256].rearrange("p (k c) -> p k c", c=256)[:, :, 192:256],
                w_uqsw[:, h * 64:(h + 1) * 64].rearrange("(k p) c -> p k c", p=128), [], [wb], wb.b.name)
            qs = qst[h % 2]; qp = qpst[h % 2]
            for th in range(2):
                p = ph.nps(); pp = ph.nps(); pw = ph.nps()
                for kt in range(6):
                    MM(p.t[:], wb.t[:, kt * 256:kt * 256 + 128], cqn.t[:, kt * 1024 + th * 512:kt * 1024 + th * 512 + 512], kt == 0, kt == 5, [wb, cqn], [p])
                for kt in range(6):
                    MM(pp.t[0:64, :], wb.t[:, kt * 256 + 128:kt * 256 + 192], cqn.t[:, kt * 1024 + th * 512:kt * 1024 + th * 512 + 512], kt == 0, kt == 5, [wb, cqn], [pp])
                for kt in range(6):
                    MM(pw.t[0:64, :], wb.t[:, kt * 256 + 192:kt * 256 + 256], cqn.t[:, kt * 1024 + th * 512:kt * 1024 + th * 512 + 512], kt == 0, kt == 5, [wb, cqn], [pw])
                CP('act', qs.t[:, th * 512:(th + 1) * 512], p.t[:], [p], [qs])
                TT('dve', q1.t[:], pp.t[0:64, :], cso.t[:, 0, th * 512:(th + 1) * 512], ALU.mult, [pp, cso], [q1])
                TT('dve', q2.t[:], pw.t[0:64, :], cso.t[:, 1, th * 512:(th + 1) * 512], ALU.mult, [pw, cso], [q2])
                TT('dve', qp.t[:, th * 512:(th + 1) * 512], q1.t[:], q2.t[:], ALU.add, [q1, q2], [qp])
            DMA('pool', QT_all[h], qs.t[:], [qs], [], qs.b.name + 'st')
            DMA('pool', QPE_all[h], qp.t[:], [qp], [], qp.b.name + 'st')

    with phase() as ph:
        vec, ident, ones = consts(ph)
        pS = [ph.psum("pS%d" % i, [128, 1024]) for i in range(3)]
        pO = [ph.psum("pO%d" % i, [128, 512]) for i in range(2)]
        ring = [0]
        slot = {}
        kpe = ph.sb("kpe", [64, TK], BF16)
        DMA('sp', kpe.t[:], kpe_all, [], [kpe], 'kpe')
        msk = ph.sb("msk", [128, 4, 512])
        MEMSET('pool', msk.t[:], 0.0, [msk])
        for d in range(4):
            S.op('pool', (lambda d=d: (lambda e: e.affine_select(out=msk.t[:, d, :], in_=msk.t[:, d, :], pattern=[[1, 512]],
                                                                 compare_op=ALU.is_ge, fill=-1.0e5, base=-128 * d,
                                                                 channel_multiplier=-1)))(), [msk.b], [msk.b])
        KTb = [ph.sb("KTb%d" % i, [128, TK], BF16) for i in range(2)]
        Vb = [ph.sb("Vb%d" % i, [128, 72, 128], BF16) for i in range(2)]
        Qb = [ph.sb("Qb%d" % i, [128, 1024], BF16) for i in range(2)]
        QPb = [ph.sb("QPb%d" % i, [64, 1024], BF16) for i in range(2)]
        PT = [ph.sb("PT%d" % i, [128, 1024], BF16) for i in range(3)]
        tmpm = [ph.sb("tmpm%d" % i, [128, 512]) for i in range(2)]
        rl = ph.sb("rl", [128, 1024])
        Ost = [ph.sb("Ost%d" % i, [128, 1024], BF16) for i in range(2)]
        scale = 192.0 ** -0.5
        Pacc = [ph.sb("Pacc%d" % i, [128, 1024]) for i in range(2)]
        Phi = ph.sb("Phi", [128, 1024], BF16)
        Plo = ph.sb("Plo", [128, 1024], BF16)
        units = [(h, kb) for h in range(16) for kb in range(72)]
        loaded = set()

        def bufs(h):
            return KTb[h % 2], Vb[h % 2], Qb[h % 2], QPb[h % 2]

        def issue_qk(u):
            h, kb = units[u]
            kt_, vb, qb, qpb = bufs(h)
            if h not in loaded:
                loaded.add(h)
                DMA('sp', kt_.t[:], Kt_all[h], [], [kt_], kt_.b.name)
                DMA('sp', vb.t[:], V_all[h], [], [vb], vb.b.name)
                DMA('sp', qb.t[:], QT_all[h], [], [qb], qb.b.name)
                DMA('sp', qpb.t[:], QPE_all[h], [], [qpb], qpb.b.name)
            slot[u] = [x for x in range(3) if x not in slot.values()][0]
            ps = pS[slot[u]]
            gs = [0, 1] if kb < 68 else [1]
            for g in gs:
                MM(ps.t[:, g * 512:(g + 1) * 512], kt_.t[:, kb * 128:(kb + 1) * 128], qb.t[:, g * 512:(g + 1) * 512], True, False, [kt_, qb], [ps])
            for g in gs:
                MM(ps.t[:, g * 512:(g + 1) * 512], kpe.t[0:64, kb * 128:(kb + 1) * 128], qpb.t[0:64, g * 512:(g + 1) * 512], False, True, [kpe, qpb], [ps])

        def issue_pv(u):
            h, kb = units[u]
            kt_, vb, qb, qpb = bufs(h)
            ps = pS[slot.pop(u)]; pt = PT[u % 3]
            pa = Pacc[h % 2]
            if kb < 64:
                bc = C_VIS + kb // 4
                ACT(pt.t[:], ps.t[:], AF.Exp, [ps, vec], [pt], bias=vec.t[:, bc:bc + 1], scale=scale)
                gs = [0, 1]
            else:
                jj = kb - 64
                if jj < 4:
                    tm = tmpm[u % 2]
                    TT('dve', tm.t[:], ps.t[:, 0:512], msk.t[:, jj, :], ALU.add, [ps, msk], [tm])
                    ACT(pt.t[:, 0:512], tm.t[:], AF.Exp, [tm], [pt], scale=scale)
                    ACT(pt.t[:, 512:1024], ps.t[:, 512:1024], AF.Exp, [ps], [pt], scale=scale)
                    gs = [0, 1]
                else:
                    tm = tmpm[u % 2]
                    TT('dve', tm.t[:], ps.t[:, 512:1024], msk.t[:, jj - 4, :], ALU.add, [ps, msk], [tm])
                    ACT(pt.t[:, 512:1024], tm.t[:], AF.Exp, [tm], [pt], scale=scale)
                    gs = [1]
            for g in gs:
                last = (kb == 67) if g == 0 else (kb == 71)
                MM(pO[g].t[:], vb.t[:, kb, :], pt.t[:, g * 512:(g + 1) * 512], kb == 0, last, [vb, pt], [pO[g]])
            lo_, hi_ = gs[0] * 512, (gs[-1] + 1) * 512
            if kb == 0:
                CP('dve', pa.t[:], pt.t[:], [pt], [pa])
            else:
                TT('dve', pa.t[:, lo_:hi_], pt.t[:, lo_:hi_], pa.t[:, lo_:hi_], ALU.add, [pt, pa], [pa])
            if kb == 71:
                CP('dve', Phi.t[:], pa.t[:], [pa], [Phi])
                TT('dve', Plo.t[:], pa.t[:], Phi.t[:], ALU.subtract, [pa, Phi], [Plo])
                pL = pS[[x for x in range(3) if x not in slot.values()][0]]
                for g in range(2):
                    MM(pL.t[:, g * 512:(g + 1) * 512], ones.t[:], Phi.t[:, g * 512:(g + 1) * 512], True, False, [ones, Phi], [pL])
                    MM(pL.t[:, g * 512:(g + 1) * 512], ones.t[:], Plo.t[:, g * 512:(g + 1) * 512], False, True, [ones, Plo], [pL])
                os_ = Ost[h % 2]
                RCP(rl.t[:], pL.t[:], [pL], [rl])
                for g in range(2):
                    TT('dve', os_.t[:, g * 512:(g + 1) * 512], pO[g].t[:], rl.t[:, g * 512:(g + 1) * 512], ALU.mult, [pO[g], rl], [os_])
                DMA('pool', OT_all[h], os_.t[:], [os_], [], os_.b.name + 'st')

        NU = len(units)
        for u in range(NU + 2):
            if u < NU:
                issue_qk(u)
            if u >= 2:
                issue_pv(u - 2)

    with phase() as ph:
        vec, ident, ones = consts(ph)
        ph.psring(8, "o2p")
        stgs = None
        wbfs = [ph.sb("wbf%d" % i, [128, 4096], BF16) for i in range(4)]
        yg = ph.sb("yg", [128, 16384], BF16)
        DMA('sp', yg.t[:].rearrange("p (k t) -> p k t", t=1024), hown_d.rearrange("(k p) t -> p k t", p=128), [], [yg], 'ygl')
        mix = ph.sb("mix", [128, 16384], BF16)
        gas = ph.sb("gas", [128, 16384], BF16)
        gtmp = [ph.sb("gtmp%d" % i, [128, 512]) for i in range(2)]
        cgt = [0]
        with ExitStack() as sub:
            ph2 = Phase(); ph2.st = sub; ph2.ps = ph.ps
            ph2.nps = ph.nps
            nown = ph2.sb("nown", [128, 16384], BF16)
            DMA('sp', nown.t[:].rearrange("p (k t) -> p k t", t=1024), nown_d.rearrange("(k p) t -> p k t", p=128), [], [nown], 'nownl')

            def ev_yr(ot, th, p):
                g_ = gtmp[cgt[0] % 2]; cgt[0] += 1
                ACT(g_.t[:], p.t[:], AF.Gelu_apprx_tanh, [p], [g_])
                sl = slice(ot * 1024 + th * 512, ot * 1024 + th * 512 + 512)
                TT('dve', yg.t[:, sl], g_.t[:], yg.t[:, sl], ALU.mult, [g_, yg], [yg])
            linearT(ph, w_in[:, 3392:5440], 16, 2048, nown, 1024, ev_yr, stgs, wbfs)

            def ev_gr(ot, th, p):
                sl = slice(ot * 1024 + th * 512, ot * 1024 + th * 512 + 512)
                ACT(mix.t[:, sl], p.t[:], AF.Sigmoid, [p, vec], [mix], bias=vec.t[:, C_BGR + ot:C_BGR + ot + 1])
            linearT(ph, w_in[:, 7488:9536], 16, 2048, nown, 1024, ev_gr, stgs, wbfs)

            def ev_ga(ot, th, p):
                sl = slice(ot * 1024 + th * 512, ot * 1024 + th * 512 + 512)
                ACT(gas.t[:, sl], p.t[:], AF.Sigmoid, [p, vec], [gas], bias=vec.t[:, C_BGA + ot:C_BGA + ot + 1])
            linearT(ph, w_in[:, 5440:7488], 16, 2048, nown, 1024, ev_ga, stgs, wbfs)
            S.barrier()
            S.flush()

        def ev_rnn(ot, th, p):
            sl = slice(ot * 1024 + th * 512, ot * 1024 + th * 512 + 512)
            TT('dve', mix.t[:, sl], p.t[:], mix.t[:, sl], ALU.mult, [p, mix], [mix])
        linearT(ph, w_ro, 16, 2048, yg, 1024, ev_rnn, stgs, wbfs)
        S.barrier()
        DMA('sp', yg.t[:].rearrange("p (k t) -> p k t", t=1024), OT_all.rearrange("h p t -> p h t"), [], [yg], 'ygl')

        def ev_att(ot, th, p):
            g_ = gtmp[cgt[0] % 2]; cgt[0] += 1
            sl = slice(ot * 1024 + th * 512, ot * 1024 + th * 512 + 512)
            TT('dve', g_.t[:], p.t[:], gas.t[:, sl], ALU.mult, [p, gas], [g_])
            TT('dve', mix.t[:, sl], g_.t[:], mix.t[:, sl], ALU.add, [g_, mix], [mix])
        linearT(ph, w_ao, 16, 2048, yg, 1024, ev_att, stgs, wbfs)
        xr_ = [ph.sb("xres%d" % i, [128, 512]) for i in range(2)]

        def ev_out(ot, th, p):
            x_ = xr_[cgt[0] % 2]; cgt[0] += 1
            DMA('sp', x_.t[:], xoT[ot * 128:(ot + 1) * 128, th * 512:(th + 1) * 512], [], [x_], x_.b.name)
            TT('dve', x_.t[:], p.t[:], x_.t[:], ALU.add, [p, x_], [x_])
            DMA('pool', h1_d[ot * 128:(ot + 1) * 128, th * 512:(th + 1) * 512], x_.t[:], [x_], [], x_.b.name)
        linearT(ph, w_out, 16, 2048, mix, 1024, ev_out, stgs, wbfs)

    for half in range(2):
        with phase() as ph:
            vec, ident, ones = consts(ph)
            ph.psring(8, "pp")
            T0 = half * 512
            hn = ph.sb("hn", [128, 16 * 512], BF16)
            s12 = ph.sb("s12", [128, 4, 16, 128])
            tau = ph.sb("tau", [128, 32]); cbias = ph.sb("cbias", [128, 32])
            ntau = ph.sb("ntau", [128, 32]); b3 = ph.sb("b3", [128, 32])
            with ExitStack() as sub:
                ph2 = Phase(); ph2.st = sub; ph2.ps = ph.ps; ph2.nps = ph.nps
                xh = [ph2.sb("px%d" % i, [128, 2048]) for i in range(4)]
                sq = ph2.sb("psq", [128, 8192], BF16)
                rt = ph2.sb("prt", [128, 512]); rstd = ph2.sb("prs", [128, 512])
                for q in range(4):
                    DMA('sp', xh[q].t[:].rearrange("p (k t) -> p k t", t=512),
                        h1_d[q * 512:(q + 1) * 512, T0:T0 + 512].rearrange("(k p) t -> p k t", p=128), [], [xh[q]], xh[q].b.name)
                psq = ph.nps()
                rmsnorm_T(ph, ones, vec, lambda kt: (xh[kt // 4].t[:, (kt % 4) * 512:(kt % 4 + 1) * 512], xh[kt // 4]),
                          16, 512, C_FFN, hn, 2048.0, sq, rt, rstd, psq)
                stgs = None
                wbfs = [ph2.sb("wbf%d" % i, [128, 4096], BF16) for i in range(4)]
                qp = ph2.sb("qp", [128, 16 * 512], BF16)
                linearT(ph, w_pq, 16, 2048, hn, 512,
                        lambda ot, th, p: CP(ev_eng(), qp.t[:, ot * 512:(ot + 1) * 512], p.t[:], [p], [qp]), stgs, wbfs)
                skb = ph2.sb("skb", [128, 16, 128], BF16)
                DMA('pool', skb.t[:], skT, [], [skb], 'skb')
                top = ph2.sb("top", [128, 16, 16]); wk1 = ph2.sb("wk1", [128, 128])
                cand = ph2.sb("cand", [128, 8, 256]); wk2 = ph2.sb("wk2", [128, 256])
                best = ph2.sb("best", [128, 8, 16]); negm = ph2.sb("negm", [128, 8])
                e16 = ph2.sb("e16", [128, 8, 16]); Z = ph2.sb("Z", [128, 8]); lnZ = ph2.sb("lnZ", [128, 8])
                pst_top = top.t[:].ap[0][0]
                for tt in range(4):
                    for hh in range(16):
                        if hh % 4 == 0:
                            p = ph.nps()
                        MM(p.t[:, (hh % 4) * 128:(hh % 4 + 1) * 128], qp.t[:, hh * 512 + tt * 128:hh * 512 + tt * 128 + 128], skb.t[:, hh, :],
                           True, True, [qp, skb], [p])
                        if hh % 4 == 3:
                            CP(ev_eng(), s12.t[:, tt, hh - 3:hh + 1, :].rearrange("p a b -> p (a b)"), p.t[:], [p], [s12])
                    for hh in range(16):
                        S.op('dve', (lambda tt=tt, hh=hh: (lambda e: e.max(out=top.t[:, hh, 0:8], in_=s12.t[:, tt, hh, :])))(), bl([s12]), bl([top]))
                        S.op('dve', (lambda tt=tt, hh=hh: (lambda e: e.match_replace(out=wk1.t[:], in_to_replace=top.t[:, hh, 0:8],
                                                                                   in_values=s12.t[:, tt, hh, :], imm_value=-1.0e30)))(),
                             bl([s12, top]), bl([wk1]))
                        S.op('dve', (lambda hh=hh: (lambda e: e.max(out=top.t[:, hh, 8:16], in_=wk1.t[:])))(), bl([wk1]), bl([top]))
                    for h in range(8):
                        in0 = bass.AP(top.t, (2 * h) * 16, [[pst_top, 128], [1, 16], [0, 16]])
                        in1 = bass.AP(top.t, (2 * h + 1) * 16, [[pst_top, 128], [0, 16], [1, 16]])
                        TT('dve', cand.t[:, h, :].rearrange("p (a b) -> p a b", b=16), in0, in1, ALU.add, [top], [cand])
                        S.op('dve', (lambda h=h: (lambda e: e.max(out=best.t[:, h, 0:8], in_=cand.t[:, h, :])))(), bl([cand]), bl([best]))
                        S.op('dve', (lambda h=h: (lambda e: e.match_replace(out=wk2.t[:], in_to_replace=best.t[:, h, 0:8],
                                                                           in_values=cand.t[:, h, :], imm_value=-1.0e30)))(),
                             bl([cand, best]), bl([wk2]))
                        S.op('dve', (lambda h=h: (lambda e: e.max(out=best.t[:, h, 8:16], in_=wk2.t[:])))(), bl([wk2]), bl([best]))
                    TS('dve', negm.t[:], best.t[:, :, 0], -1.0, None, ALU.mult, None, [best], [negm])
                    CP('dve', tau.t[:, tt * 8:(tt + 1) * 8], best.t[:, :, 15], [best], [tau])
                    pst_b = best.t[:].ap[0][0]
                    TT('dve', e16.t[:], best.t[:], bass.AP(best.t, 0, [[pst_b, 128], [16, 8], [0, 16]]), ALU.subtract, [best], [e16])
                    ACT(e16.t[:], e16.t[:], AF.Exp, [e16], [e16])
                    S.op('dve', lambda e: e.tensor_reduce(out=Z.t[:], in_=e16.t[:], axis=mybir.AxisListType.X, op=ALU.add), bl([e16]), bl([Z]))
                    ACT(lnZ.t[:], Z.t[:], AF.Ln, [Z], [lnZ])
                    TT('dve', cbias.t[:, tt * 8:(tt + 1) * 8], negm.t[:], lnZ.t[:], ALU.subtract, [negm, lnZ], [cbias])
                    TS('dve', tau.t[:, tt * 8:(tt + 1) * 8], tau.t[:, tt * 8:(tt + 1) * 8], -1.0e-6, None, ALU.add, None, [tau], [tau])
                    TS('dve', ntau.t[:, tt * 8:(tt + 1) * 8], tau.t[:, tt * 8:(tt + 1) * 8], -1.0, None, ALU.mult, None, [tau], [ntau])
                    TT('dve', b3.t[:, tt * 8:(tt + 1) * 8], tau.t[:, tt * 8:(tt + 1) * 8], cbias.t[:, tt * 8:(tt + 1) * 8], ALU.add, [tau, cbias], [b3])
                S.barrier()
                S.flush()
            acc = ph.sb("acc", [128, 16 * 512])
            ubf2 = [ph.sb("ubf%d" % i, [128, 16, 512], BF16) for i in range(1)]
            vbf2 = [ph.sb("vbf%d" % i, [128, 4, 2048], BF16) for i in range(2)]
            gate = [ph.sb("gate%d" % i, [128, 8, 512], BF16) for i in range(2)]
            sc = [ph.sb("sc%d" % i, [128, 512]) for i in range(3)]
            ex = [ph.sb("ex%d" % i, [128, 512]) for i in range(3)]
            pst_s = s12.t[:].ap[0][0]
            GT = [ph.sb("GT%d" % i, [128, 4, 512], BF16) for i in range(2)]
            ge = [ph.sb("ge%d" % i, [128, 512]) for i in range(2)]
            WT = [ph.sb("WT%d" % i, [128, 4, 512], BF16) for i in range(4)]
            pG = ph.ps[0:2]; pA = ph.ps[2:4]; pD = ph.ps[4:8]
            NG = 32
            deferred = []
            gu = [0]
            kk = [0]
            pend = []

            def flush_pend():
                while pend:
                    g2, h2, s2_, e2_, t2 = pend.pop(0)
                    STT(g2.t[:, h2, :], s2_.t[:], tau.t[:, t2 * 8 + h2:t2 * 8 + h2 + 1], e2_.t[:], ALU.is_ge, ALU.mult, [s2_, tau, e2_], [g2])

            def gate_unit(G, u):
                tt, h = u // 8, u % 8
                k = kk[0] % 3
                kk[0] += 1
                s_ = sc[k]; e_ = ex[k]
                gt_ = gate[(G * 4 + tt) % 2]
                in0 = bass.AP(s12.t, tt * 2048 + (2 * h + 1) * 128, [[pst_s, 128], [0, 4], [1, 128]])
                in1 = bass.AP(s12.t, tt * 2048 + (2 * h) * 128 + G * 4, [[pst_s, 128], [1, 4], [0, 128]])
                TT('dve', s_.t[:].rearrange("p (a b) -> p a b", b=128), in0, in1, ALU.add, [s12], [s_])
                c_ = tt * 8 + h
                if h >= 5:
                    S.op('act', lambda e: e.activation(out=e_.t[:], in_=s_.t[:], func=AF.Prelu, bias=ntau.t[:, c_:c_ + 1], scale=1.0, alpha=1.0e9),
                         bl([s_, ntau]), bl([e_]))
                    ACT(gt_.t[:, h, :], e_.t[:], AF.Exp, [e_, b3], [gt_], bias=b3.t[:, c_:c_ + 1])
                    flush_pend()
                    return
                ACT(e_.t[:], s_.t[:], AF.Exp, [s_, cbias], [e_], bias=cbias.t[:, tt * 8 + h:tt * 8 + h + 1])
                pend.append((gt_, h, s_, e_, tt))
                if len(pend) > 1:
                    g2, h2, s2_, e2_, t2 = pend.pop(0)
                    STT(g2.t[:, h2, :], s2_.t[:], tau.t[:, t2 * 8 + h2:t2 * 8 + h2 + 1], e2_.t[:], ALU.is_ge, ALU.mult, [s2_, tau, e2_], [g2])

            def ident_mm(G, tt):
                gt_ = gate[(G * 4 + tt) % 2]
                pg = pG[(G * 4 + tt) % 2]
                for i in range(4):
                    for hh in range(8):
                        MM(pg.t[:, i * 128:(i + 1) * 128], gt_.t[:, hh, i * 128:(i + 1) * 128], ident.t[:], hh == 0, hh == 7, [gt_, ident], [pg])
                CP('act', GT[G % 2].t[:, :, tt * 128:(tt + 1) * 128], pg.t[:].rearrange("p (a b) -> p a b", b=128), [pg], [GT[G % 2]])

            def load_u(G):
                ub = ubf2[0]
                for q in range(4):
                    DMA('pool', ub.t[:, 4 * q:4 * q + 4, :],
                        puT[q * 512:(q + 1) * 512, G * 512:(G + 1) * 512].rearrange("(k p) c -> p k c", p=128), [], [ub], ub.b.name)

            def load_v(G):
                vb = vbf2[G % 2]
                for d in range(4):
                    DMA('pool', vb.t[:, d, :], pv[G * 512 + d * 128:G * 512 + d * 128 + 128, :], [], [vb], vb.b.name)

            def act_chain(G, i):
                pa = pA[i % 2]
                ub = ubf2[0]
                for kt in range(16):
                    MM(pa.t[:], ub.t[:, kt, i * 128:(i + 1) * 128], hn.t[:, kt * 512:(kt + 1) * 512], kt == 0, kt == 15, [ub, hn], [pa])
                g_ = ge[i % 2]
                ACT(g_.t[:], pa.t[:], AF.Gelu_apprx_tanh, [pa], [g_])
                TT('pool', WT[G % 4].t[:, i, :], g_.t[:], GT[G % 2].t[:, i, :], ALU.mult, [g_, GT[G % 2]], [WT[G % 4]])

            def v_chain(Ga, dt):
                pd = pD[dt % 4]
                n = 0
                for Gx in (Ga, Ga + 1):
                    vb = vbf2[Gx % 2]; wt = WT[Gx % 4]
                    for d in range(4):
                        MM(pd.t[:], vb.t[:, d, dt * 128:(dt + 1) * 128], wt.t[:, d, :], n == 0, n == 7, [vb, wt], [pd])
                        n += 1

                def add():
                    if Ga == 0:
                        CP('dve', acc.t[:, dt * 512:(dt + 1) * 512], pd.t[:], [pd], [acc])
                    else:
                        TT('dve', acc.t[:, dt * 512:(dt + 1) * 512], pd.t[:], acc.t[:, dt * 512:(dt + 1) * 512], ALU.add, [pd, acc], [acc])
                deferred.append((gu[0] + 3, add))

            def run_deferred(force=False):
                while deferred and (force or deferred[0][0] <= gu[0]):
                    deferred.pop(0)[1]()

            load_u(0); load_v(0); load_v(1)
            for G in range(NG + 1):
                for u in range(32):
                    gu[0] = G * 32 + u
                    if G < NG:
                        gate_unit(G, u)
                    elif u == 0:
                        flush_pend()
                    run_deferred()
                    if u % 8 == 4:
                        tq = u // 8 - 1
                        if tq >= 0 and G < NG:
                            ident_mm(G, tq)
                        elif tq < 0 and G >= 1:
                            ident_mm(G - 1, 3)
                    if G >= 1:
                        Gp = G - 1
                        if u in (6, 8, 10, 12):
                            act_chain(Gp, (u - 6) // 2)
                        if u == 13 and G < NG:
                            load_u(G)
                        if G % 2 == 0:
                            if 14 <= u < 30:
                                v_chain(G - 2, u - 14)
                            if u == 30:
                                if G < NG:
                                    load_v(G)
                                if G + 1 < NG:
                                    load_v(G + 1)
            run_deferred(force=True)
            hx = [ph.sb("hx%d" % i, [128, 512]) for i in range(2)]
            for dt in range(16):
                x_ = hx[dt % 2]
                DMA('sp', x_.t[:], h1_d[dt * 128:(dt + 1) * 128, T0:T0 + 512], [], [x_], x_.b.name)
                TT('dve', x_.t[:], x_.t[:], acc.t[:, dt * 512:(dt + 1) * 512], ALU.add, [x_, acc], [x_])
                DMA('pool', h2_d[dt * 128:(dt + 1) * 128, T0:T0 + 512], x_.t[:], [x_], [], x_.b.name)

    with phase() as ph:
        vec, ident, ones = consts(ph)
        ph.psring(8, "ep")
        h2 = ph.sb("h2", [128, 16384])
        hn3 = ph.sb("hn3", [128, 16384], BF16)
        load_norm_own(ph, ones, vec, h2_d, C_PLE, hn3, keep=h2)
        stgs = None
        wbfs = [ph.sb("wbf%d" % i, [128, 2048], BF16) for i in range(4)]
        gsb = ph.sb("gsb", [128, 16384], BF16)

        def ev_g(ot, th, p):
            sl = slice(ot * 1024 + th * 512, ot * 1024 + th * 512 + 512)
            ACT(gsb.t[:, sl], p.t[:], AF.Sigmoid, [p], [gsb])
        linearT(ph, w_pg, 16, 2048, hn3, 1024, ev_g, stgs, wbfs, cb=128)
        pb = ph.sb("pb", [128, 2048], BF16)
        DMA('pool', pb.t[:].rearrange("p (k t) -> p k t", t=1024), poT.rearrange("(k p) t -> p k t", p=128), [], [pb], 'pb')
        et = [ph.sb("et%d" % i, [128, 512]) for i in range(2)]
        ce = [0]

        def ev_p(ot, th, p):
            t_ = et[ce[0] % 2]; ce[0] += 1
            sl = slice(ot * 1024 + th * 512, ot * 1024 + th * 512 + 512)
            TT('dve', t_.t[:], p.t[:], gsb.t[:, sl], ALU.mult, [p, gsb], [t_])
            TT('dve', h2.t[:, sl], t_.t[:], h2.t[:, sl], ALU.add, [t_, h2], [h2])
        linearT(ph, w_pp, 2, 2048, pb, 1024, ev_p, stgs, wbfs, cb=128)
        sq = ph.sb("fsq", [128, 8192], BF16)
        rt = ph.sb("frt", [128, 512]); rstd = ph.sb("frs", [128, 512])
        ob = [ph.sb("ob%d" % i, [128, 512]) for i in range(2)]
        for th in range(2):
            psq = ph.nps()
            for kt in range(16):
                ACT(sq.t[:, kt * 512:(kt + 1) * 512], h2.t[:, kt * 1024 + th * 512:kt * 1024 + th * 512 + 512], AF.Square, [h2], [sq])
            for kt in range(16):
                MM(psq.t[:], ones.t[:], sq.t[:, kt * 512:(kt + 1) * 512], kt == 0, kt == 15, [ones, sq], [psq])
            ACT(rt.t[:], psq.t[:], AF.Sqrt, [psq, vec], [rt], bias=vec.t[:, C_EPS:C_EPS + 1], scale=1.0 / 2048.0)
            RCP(rstd.t[:], rt.t[:], [rt], [rstd])
            for kt in range(16):
                o_ = ob[kt % 2]
                STT(o_.t[:], h2.t[:, kt * 1024 + th * 512:kt * 1024 + th * 512 + 512], vec.t[:, C_FIN + kt:C_FIN + kt + 1], rstd.t[:],
                    ALU.mult, ALU.mult, [h2, vec, rstd], [o_])
                DMA('sp', outT[kt * 128:(kt + 1) * 128, th * 512:(th + 1) * 512], o_.t[:], [o_], [], o_.b.name)

    topstack.close()
    return nc


def _cm(v, n):
    return np.ascontiguousarray(np.asarray(v, np.float32).reshape(n, 128).T)


def prep_inputs(inp):
    f = lambda a: np.ascontiguousarray(np.asarray(a, dtype=np.float32))
    x = f(inp['x'])[0]
    p = f(inp['p'])[0, 0]
    xT = np.ascontiguousarray(x.T)
    w_in = f(inp['w_in'])[0]
    perm = np.concatenate([np.arange(32, 64), np.arange(0, 32)])
    w_krsw = np.ascontiguousarray(w_in[:, 1280:1344][:, perm])
    w_uq = f(inp['w_uq'])[0]
    w_uqsw = np.ascontiguousarray(w_uq.reshape(768, 16, 192)[:, :, 128:][:, :, perm].reshape(768, 1024))
    half = 32
    inv_freq = (1.0 / (np.float32(10000.0) ** (np.arange(half, dtype=np.float32) / np.float32(half)))).astype(np.float32)

    def tables(pos):
        ang = pos.astype(np.float32)[:, None] * inv_freq[None, :]
        c = np.cos(ang).astype(np.float32).T
        s = np.sin(ang).astype(np.float32).T
        return np.concatenate([c, c], 0), np.concatenate([-s, s], 0)
    vec_common = np.zeros((128, NV), np.float32)
    vec_common[:, C_ATTN:C_ATTN + 16] = _cm(inp['attn_norm'][0], 16)
    vec_common[:, C_QN:C_QN + 6] = _cm(inp['q_norm'][0], 6)
    vec_common[:, C_KVN:C_KVN + 4] = _cm(inp['kv_norm'][0], 4)
    vec_common[:, C_CB:C_CB + 16] = _cm(inp['conv_b'][0], 16)
    cw = np.asarray(inp['conv_w'], np.float32)[0]
    vec_common[:, C_CW:C_CW + 64] = cw.T.reshape(16, 128, 4).transpose(1, 0, 2).reshape(128, 64)
    vec_common[:, C_BA:C_BA + 16] = _cm(np.asarray(inp['b_rg_a'])[0].reshape(-1), 16)
    vec_common[:, C_BX:C_BX + 16] = _cm(np.asarray(inp['b_rg_x'])[0].reshape(-1), 16)
    vec_common[:, C_LAM:C_LAM + 16] = _cm(inp['lru_lambda'][0], 16)
    bg = np.asarray(inp['b_gate'], np.float32)[0]
    vec_common[:, C_BGA:C_BGA + 16] = _cm(bg[:2048], 16)
    vec_common[:, C_BGR:C_BGR + 16] = _cm(bg[2048:], 16)
    vec_common[:, C_FFN:C_FFN + 16] = _cm(inp['ffn_norm'][0], 16)
    vec_common[:, C_PLE:C_PLE + 16] = _cm(inp['ple_norm'][0], 16)
    vec_common[:, C_FIN:C_FIN + 16] = _cm(inp['final_norm'], 16)
    vec_common[:, C_EPS] = 1e-6
    vec_common[:, C_ONE] = 1.0
    vec_common[:, C_M1] = -1.0
    shared = {
        'xT': xT, 'w_in': w_in, 'w_krsw': w_krsw, 'w_uq': w_uq, 'w_uqsw': w_uqsw,
        'w_ukv': f(inp['w_ukv'])[0], 'w_attn_o': f(inp['w_attn_o'])[0],
        'w_rg_a': f(inp['w_rg_a'])[0], 'w_rg_x': f(inp['w_rg_x'])[0],
        'w_rnn_o': f(inp['w_rnn_o'])[0], 'w_out': f(inp['w_out'])[0], 'w_peer_q': f(inp['w_peer_q'])[0],
        'skT': np.ascontiguousarray(f(inp['peer_subkeys'])[0].reshape(16, 128, 128).transpose(2, 0, 1)),
        'puT': np.ascontiguousarray(f(inp['peer_u'])[0].T), 'pv': f(inp['peer_v'])[0],
        'w_ple_gate': f(inp['w_ple_gate'])[0], 'w_ple_proj': f(inp['w_ple_proj'])[0],
    }
    cg, sg = tables(np.arange(8192))
    maps = []
    for c in range(8):
        m = dict(shared)
        m['xoT'] = np.ascontiguousarray(xT[:, 1024 * c:1024 * (c + 1)])
        m['poT'] = np.ascontiguousarray(p[1024 * c:1024 * (c + 1)].T)
        v = vec_common.copy()
        for j in range(16):
            v[:, C_VIS + j] = 0.0 if j < 2 * c else NEG
            for hf in range(2):
                v[:, C_OWN + 2 * j + hf] = 1.0 if j == 2 * c + hf else 0.0
        m['vecs'] = v
        cs_ = np.zeros((2, 64, TK), np.float32)
        cs_[0, :, :8192] = cg; cs_[1, :, :8192] = sg
        cs_[0, :, 8192:] = cg[:, 1024 * c:1024 * (c + 1)]; cs_[1, :, 8192:] = sg[:, 1024 * c:1024 * (c + 1)]
        m['cs'] = cs_
        maps.append(m)
    return maps


def kernel(**inputs):
    maps = prep_inputs(inputs)
    nc = build_nc(False)
    res = run_bass_kernel_spmd(nc, maps, core_ids=list(range(8)))
    outT = np.concatenate([np.asarray(r["outT"]) for r in res.results], axis=1)
    return np.ascontiguousarray(outT.T)[None].astype(np.float32)
```

```python
import numpy as np
from contextlib import ExitStack, contextmanager
import concourse.bass as bass
import concourse.mybir as mybir
from concourse.bass_utils import run_bass_kernel_spmd

F32 = mybir.dt.float32
BF16 = mybir.dt.bfloat16
AF = mybir.ActivationFunctionType
ALU = mybir.AluOpType

NV = 304
C_ATTN, C_QN, C_KVN, C_CB, C_CW, C_BA, C_BX, C_LAM, C_BGA, C_BGR, C_FFN, C_PLE, C_FIN = \
    0, 16, 22, 26, 42, 106, 122, 138, 154, 170, 186, 202, 218
C_EPS, C_ONE, C_ZERO, C_M1, C_VIS, C_OWN, C_VIS1 = 234, 235, 236, 237, 240, 256, 288
NEG = -30000.0
NCH = 18
TK = 9216


class Buf:
    def __init__(self, name):
        self.name = name
        self.w = None
        self.r = {}


class Tl:
    def __init__(self, t, name):
        self.t = t
        self.b = Buf(name)


_uid = [0]


def _un(name):
    _uid[0] += 1
    return "%s_%d" % (name, _uid[0])


class Sched:
    ENG = ('pe', 'act', 'dve', 'pool', 'sp')

    def __init__(self, nc, stack):
        self.nc = nc
        self.stack = stack
        self.prog = {k: [] for k in self.ENG}
        self.sems = {}
        self.cnt = {}
        self.waited = {k: {} for k in self.ENG}
        self.nsem = 0
        for k in self.ENG:
            self._newsem(k)

    def _newsem(self, key):
        s = self.stack.enter_context(self.nc.semaphore("s%d_%s" % (self.nsem, key[:12])))
        self.nsem += 1
        self.sems[key] = s
        self.cnt[key] = 0
        for e in self.ENG:
            self.waited[e].pop(key, None)

    def _need(self, eng, dep, waits):
        if dep is None:
            return
        key, sem, val = dep
        if self.sems.get(key) is not sem:
            return
        if key == 'pe' and eng == 'pe':
            return
        if self.waited[eng].get(key, 0) >= val:
            return
        waits[key] = max(waits.get(key, 0), val)

    def op(self, eng, fn, reads=(), writes=(), dma_key=None):
        waits = {}
        for b in reads:
            self._need(eng, b.w, waits)
        for b in writes:
            self._need(eng, b.w, waits)
            for k, (s, v) in b.r.items():
                self._need(eng, (k, s, v), waits)
        for k, v in waits.items():
            self.waited[eng][k] = v
        if dma_key is None:
            key, inc = eng, 1
        else:
            key, inc = dma_key, 16
            if key not in self.sems:
                self._newsem(key)
        self.cnt[key] += inc
        val = self.cnt[key]
        assert val < 60000, (key, val)
        sem = self.sems[key]
        self.prog[eng].append(([(self.sems[k], v) for k, v in waits.items()], fn, sem, inc))
        for b in reads:
            b.r[key] = (sem, val)
        for b in writes:
            b.w = (key, sem, val)
            b.r = {}

    def barrier(self, new_epoch=True):
        for eng in self.ENG:
            waits = {}
            for k, v in self.cnt.items():
                if v > 0:
                    self._need(eng, (k, self.sems[k], v), waits)
            for k, v in waits.items():
                self.waited[eng][k] = v
            if waits:
                self.prog[eng].append(([(self.sems[k], v) for k, v in waits.items()], None, None, 0))

    def flush(self):
        progs = self.prog
        with self.nc.Block() as block:
            def mk(engname):
                def body(engine):
                    for waits, fn, sem, inc in progs[engname]:
                        for s, v in waits:
                            engine.wait_ge(s, v)
                        if fn is not None:
                            fn(engine).then_inc(sem, inc)
                return body
            block.tensor(mk('pe'))
            block.scalar(mk('act'))
            block.vector(mk('dve'))
            block.gpsimd(mk('pool'))
            block.sync(mk('sp'))
        self.prog = {k: [] for k in self.ENG}


def build_nc(debug=False):
    nc = bass.Bass("TRN2", target_bir_lowering=False)

    def din(name, shape, dt=F32):
        return nc.dram_tensor(name, list(shape), dt, kind="ExternalInput").ap()

    def dscr(name, shape, dt):
        return nc.dram_tensor(name, list(shape), dt, kind="ExternalOutput" if debug else "Internal").ap()

    xT = din("xT", [2048, 8192]); xoT = din("xoT", [2048, 1024]); poT = din("poT", [256, 1024])
    vecs = din("vecs", [128, NV]); cs = din("cs", [2, 64, TK])
    w_in = din("w_in", [2048, 9536]); w_krsw = din("w_krsw", [2048, 64])
    w_uq = din("w_uq", [768, 3072]); w_uqsw = din("w_uqsw", [768, 1024])
    w_ukv = din("w_ukv", [512, 4096]); w_ao = din("w_attn_o", [2048, 2048])
    w_rga = din("w_rg_a", [16, 128, 128]); w_rgx = din("w_rg_x", [16, 128, 128])
    w_ro = din("w_rnn_o", [2048, 2048]); w_out = din("w_out", [2048, 2048]); w_pq = din("w_peer_q", [2048, 2048])
    skT = din("skT", [128, 16, 128]); puT = din("puT", [2048, 16384]); pv = din("pv", [16384, 2048])
    w_pg = din("w_ple_gate", [2048, 2048]); w_pp = din("w_ple_proj", [256, 2048])
    outT = nc.dram_tensor("outT", [2048, 1024], F32, kind="ExternalOutput").ap()

    nT_all = dscr("nT_all", [2048, 8192], BF16)
    Kt_all = dscr("Kt_all", [16, 128, TK], BF16)
    V_all = dscr("V_all", [16, 128, 72, 128], BF16)
    kpe_all = dscr("kpe_all", [64, TK], BF16)
    QT_all = dscr("QT_all", [16, 128, 1024], BF16)
    QPE_all = dscr("QPE_all", [16, 64, 1024], BF16)
    OT_all = dscr("OT_all", [16, 128, 1024], BF16)
    hown_d = dscr("hown_d", [2048, 1024], BF16)
    nown_d = dscr("nown_d", [2048, 1024], BF16)
    h1_d = dscr("h1_d", [2048, 1024], F32)
    h2_d = dscr("h2_d", [2048, 1024], F32)

    topstack = ExitStack()
    S = Sched(nc, topstack)

    def bl(x):
        return [y.b if isinstance(y, Tl) else y for y in x]

    def MM(ps, lhsT, rhs, start, stop, R, W):
        S.op('pe', lambda e: e.matmul(ps, lhsT=lhsT, rhs=rhs, start=start, stop=stop), bl(R), bl(W))

    def TR(ps, in_, ident, R, W):
        S.op('pe', lambda e: e.transpose(out=ps, in_=in_, identity=ident), bl(R), bl(W))

    def ACT(out, in_, func, R, W, bias=None, scale=None):
        kw = {}
        if bias is not None:
            kw['bias'] = bias
        if scale is not None:
            kw['scale'] = scale
        S.op('act', lambda e: e.activation(out=out, in_=in_, func=func, **kw), bl(R), bl(W))

    def TT(eng, out, in0, in1, op, R, W):
        S.op(eng, lambda e: e.tensor_tensor(out=out, in0=in0, in1=in1, op=op), bl(R), bl(W))

    def TS(eng, out, in0, s1, s2, op0, op1, R, W):
        if s2 is None:
            S.op(eng, lambda e: e.tensor_scalar(out=out, in0=in0, scalar1=s1, scalar2=None, op0=op0), bl(R), bl(W))
        else:
            S.op(eng, lambda e: e.tensor_scalar(out=out, in0=in0, scalar1=s1, scalar2=s2, op0=op0, op1=op1), bl(R), bl(W))

    def STT(out, in0, scalar, in1, op0, op1, R, W):
        S.op('dve', lambda e: e.scalar_tensor_tensor(out=out, in0=in0, scalar=scalar, in1=in1, op0=op0, op1=op1), bl(R), bl(W))

    def CP(eng, out, in_, R, W):
        if eng == 'act':
            S.op('act', lambda e: e.copy(out=out, in_=in_), bl(R), bl(W))
        else:
            S.op(eng, lambda e: e.tensor_copy(out=out, in_=in_), bl(R), bl(W))

    def RCP(out, in_, R, W):
        S.op('dve', lambda e: e.reciprocal(out=out, in_=in_), bl(R), bl(W))

    def MEMSET(eng, ap, val, W):
        S.op(eng, lambda e: e.memset(ap, val), [], bl(W))

    def DMA(eng, out, in_, R, W, key):
        S.op(eng, lambda e: e.dma_start(out=out, in_=in_), bl(R), bl(W), dma_key=key)

    class Phase:
        def __init__(self):
            self.st = ExitStack()
            self.ps = []
            self.psi = 0

        def sb(self, name, shape, dt=F32):
            return Tl(self.st.enter_context(nc.sbuf_tensor(_un(name), list(shape), dt)), name)

        def psum(self, name, shape, dt=F32):
            return Tl(self.st.enter_context(nc.psum_tensor(_un(name), list(shape), dt)), name)

        def psring(self, n, prefix):
            self.ps = [self.psum("%s%d" % (prefix, i), [128, 512]) for i in range(n)]
            self.psi = 0

        def nps(self):
            p = self.ps[self.psi % len(self.ps)]
            self.psi += 1
            return p

    @contextmanager
    def phase():
        ph = Phase()
        try:
            yield ph
            S.barrier()
            S.flush()
        finally:
            ph.st.close()
        for k in S.ENG:
            if S.cnt[k] > 28000:
                S._newsem(k)

    cnt = {'cast': 0, 'ev': 0}

    def cast_eng():
        cnt['cast'] += 1
        return ('pool', 'dve', 'act')[cnt['cast'] % 3] if False else 'pool'

    def ev_eng():
        cnt['ev'] += 1
        return 'act' if cnt['ev'] % 2 else 'dve'

    def consts(ph):
        vec = ph.sb("vec", [128, NV])
        DMA('sp', vec.t[:], vecs, [], [vec], 'vec')
        idf = ph.sb("idf", [128, 128])
        MEMSET('pool', idf.t[:], 0.0, [idf])
        S.op('pool', lambda e: e.affine_select(out=idf.t[:], in_=idf.t[:], pattern=[[-1, 128]],
                                               compare_op=ALU.not_equal, fill=1.0, base=0, channel_multiplier=1),
             [idf.b], [idf.b])
        ident = ph.sb("ident", [128, 128], BF16)
        CP('dve', ident.t[:], idf.t[:], [idf], [ident])
        ones = ph.sb("ones", [128, 128], BF16)
        MEMSET('pool', ones.t[:], 1.0, [ones])
        return vec, ident, ones

    def rmsnorm_T(ph, ones, vec, src_tiles, nkt, T, gcol, dst, D, sq, rt, rstd, psq):
        for kt in range(nkt):
            ap, tl = src_tiles(kt)
            ACT(sq.t[:, kt * T:(kt + 1) * T], ap, AF.Square, [tl], [sq])
        for kt in range(nkt):
            MM(psq.t[:, :T], ones.t[:], sq.t[:, kt * T:(kt + 1) * T], kt == 0, kt == nkt - 1, [ones, sq], [psq])
        ACT(rt.t[:, :T], psq.t[:, :T], AF.Sqrt, [psq, vec], [rt], bias=vec.t[:, C_EPS:C_EPS + 1], scale=1.0 / D)
        RCP(rstd.t[:, :T], rt.t[:, :T], [rt], [rstd])
        for kt in range(nkt):
            ap, tl = src_tiles(kt)
            STT(dst.t[:, kt * T:(kt + 1) * T], ap, vec.t[:, gcol + kt:gcol + kt + 1], rstd.t[:, :T], ALU.mult, ALU.mult,
                [tl, vec, rstd], [dst])

    with phase() as ph:
        vec, ident, ones = consts(ph)
        ph.psring(8, "g1p")
        wkv = ph.sb("wkv", [128, 16, 640], BF16)
        wk = ph.sb("wk", [128, 4, 2048], BF16)
        wv = ph.sb("wv", [128, 4, 2048], BF16)
        for kt in range(16):
            DMA('pool', wkv.t[:, kt, 0:576], w_in[kt * 128:(kt + 1) * 128, 768:1344], [], [wkv], 'wkv')
            DMA('pool', wkv.t[:, kt, 576:640], w_krsw[kt * 128:(kt + 1) * 128, :], [], [wkv], 'wkv')
        for kt in range(4):
            sv = w_ukv[kt * 128:(kt + 1) * 128, :].rearrange("p (h two d) -> p h two d", two=2, d=128)
            DMA('pool', wk.t[:, kt, :].rearrange("p (h d) -> p h d", d=128), sv[:, :, 0, :], [], [wk], 'wk')
            DMA('pool', wv.t[:, kt, :].rearrange("p (h d) -> p h d", d=128), sv[:, :, 1, :], [], [wv], 'wv')
        xp = [ph.sb("xp%d" % i, [128, 2048]) for i in range(4)]
        sq = ph.sb("sq", [128, 8192], BF16)
        nT = ph.sb("nT", [128, 8192], BF16)
        rt = ph.sb("rt", [128, 512]); rstd = ph.sb("rstd", [128, 512])
        rt2 = ph.sb("rt2", [128, 512]); rstd2 = ph.sb("rstd2", [128, 512])
        sqk = ph.sb("sqk", [128, 2048], BF16)
        ckvn2 = [ph.sb("ckvn%d" % i, [128, 2048], BF16) for i in range(2)]
        cst = ph.sb("cst", [64, 2, 512])
        t1 = ph.sb("t1", [64, 512]); t2 = ph.sb("t2", [64, 512])
        kpst = ph.sb("kpst", [64, 512], BF16)
        Kst = ph.sb("Kst", [128, 16, 512], BF16)
        Vst = ph.sb("Vst", [128, 4, 2048], BF16)
        P = ph.ps
        kvr = [0]

        def kvps():
            p = P[6 + (kvr[0] % 2)]
            kvr[0] += 1
            return p

        def partA(j):
            src = xT[:, 512 * j:512 * j + 512] if j < 16 else xoT[:, 512 * (j - 16):512 * (j - 16) + 512]
            for q in range(4):
                DMA('sp', xp[q].t[:].rearrange("p (k t) -> p k t", t=512),
                    src[q * 512:(q + 1) * 512, :].rearrange("(k p) t -> p k t", p=128), [], [xp[q]], xp[q].b.name)
            DMA('sp', cst.t[:], cs[:, :, 512 * j:512 * j + 512].rearrange("c p t -> p c t"), [], [cst], 'cst')
            rmsnorm_T(ph, ones, vec, lambda kt: (xp[kt // 4].t[:, (kt % 4) * 512:(kt % 4 + 1) * 512], xp[kt // 4]),
                      16, 512, C_ATTN, nT, 2048.0, sq, rt, rstd, P[0])
            if j < 16:
                DMA('pool', nT_all[:, 512 * j:512 * j + 512].rearrange("(k p) t -> p k t", p=128),
                    nT.t[:].rearrange("p (k t) -> p k t", t=512), [nT], [], 'nTst')

        def partB(j):
            ckvn = ckvn2[j % 2]
            pck = P[1:5]
            pkr = P[5]
            for o in range(4):
                for kt in range(16):
                    MM(pck[o].t[:], wkv.t[:, kt, o * 128:(o + 1) * 128], nT.t[:, kt * 512:(kt + 1) * 512], kt == 0, kt == 15,
                       [wkv, nT], [pck[o]])
            for kt in range(16):
                MM(pkr.t[0:64, :], wkv.t[:, kt, 512:576], nT.t[:, kt * 512:(kt + 1) * 512], kt == 0, kt == 15, [wkv, nT], [pkr])
            TT('dve', t1.t[:], pkr.t[0:64, :], cst.t[:, 0, :], ALU.mult, [pkr, cst], [t1])
            for kt in range(16):
                MM(pkr.t[0:64, :], wkv.t[:, kt, 576:640], nT.t[:, kt * 512:(kt + 1) * 512], kt == 0, kt == 15, [wkv, nT], [pkr])
            TT('dve', t2.t[:], pkr.t[0:64, :], cst.t[:, 1, :], ALU.mult, [pkr, cst], [t2])
            TT('dve', kpst.t[:], t1.t[:], t2.t[:], ALU.add, [t1, t2], [kpst])
            DMA('pool', kpe_all[:, 512 * j:512 * j + 512], kpst.t[:], [kpst], [], 'kpst')
            rmsnorm_T(ph, ones, vec, lambda o: (pck[o].t[:], pck[o]), 4, 512, C_KVN, ckvn, 512.0, sqk, rt2, rstd2, P[0])

        def partK(j):
            ckvn = ckvn2[j % 2]
            for h in range(16):
                p = kvps()
                for kt in range(4):
                    MM(p.t[:], wk.t[:, kt, h * 128:(h + 1) * 128], ckvn.t[:, kt * 512:(kt + 1) * 512], kt == 0, kt == 3, [wk, ckvn], [p])
                CP('act', Kst.t[:, h, :], p.t[:], [p], [Kst])
            DMA('pool', Kt_all[:, :, 512 * j:512 * j + 512].rearrange("h p t -> p h t"), Kst.t[:], [Kst], [], 'Kst')

        def partV(j):
            ckvn = ckvn2[j % 2]
            for tt in range(4):
                for cg in range(4):
                    p = kvps()
                    for kt in range(4):
                        MM(p.t[:], ckvn.t[:, kt * 512 + tt * 128:kt * 512 + tt * 128 + 128], wv.t[:, kt, cg * 512:(cg + 1) * 512],
                           kt == 0, kt == 3, [wv, ckvn], [p])
                    CP(ev_eng(), Vst.t[:, tt, cg * 512:(cg + 1) * 512], p.t[:], [p], [Vst])
            for tt in range(4):
                DMA('pool', V_all[:, :, 4 * j + tt, :].rearrange("h p d -> p h d"),
                    Vst.t[:, tt, :].rearrange("p (h d) -> p h d", d=128), [Vst], [], 'Vst')

        for j in range(NCH + 1):
            if j < NCH:
                partA(j)
            if j >= 1:
                partK(j - 1)
            if j < NCH:
                partB(j)
            if j >= 1:
                partV(j - 1)

    with phase() as ph:
        vec, ident, ones = consts(ph)
        ph.psring(6, "g2p")
        wxr = ph.sb("wxr", [128, 16, 2048], BF16)
        wga = ph.sb("wga", [128, 16, 128], BF16)
        wgx = ph.sb("wgx", [128, 16, 128], BF16)
        for kt in range(16):
            DMA('pool', wxr.t[:, kt, :], w_in[kt * 128:(kt + 1) * 128, 1344:3392], [], [wxr], 'wxr')
        DMA('pool', wga.t[:], w_rga.rearrange("b i j -> i b j"), [], [wga], 'wga')
        DMA('pool', wgx.t[:], w_rgx.rearrange("b i j -> i b j"), [], [wgx], 'wgx')
        cf = ph.sb("cf", [128, 48])
        ACT(cf.t[:, 0:16], vec.t[:, C_LAM:C_LAM + 16], AF.Exp, [vec], [cf], scale=-1.0)
        ACT(cf.t[:, 0:16], cf.t[:, 0:16], AF.Ln, [cf, vec], [cf], bias=vec.t[:, C_ONE:C_ONE + 1])
        TS('dve', cf.t[:, 16:32], cf.t[:, 0:16], -8.0, None, ALU.mult, None, [cf], [cf])
        TS('dve', cf.t[:, 32:48], cf.t[:, 0:16], -16.0, None, ALU.mult, None, [cf], [cf])
        hown = ph.sb("hown", [128, 16, 1024], BF16)
        MEMSET('pool', hown.t[:], 0.0, [hown])
        carry = ph.sb("carry", [128, 16]); MEMSET('pool', carry.t[:], 0.0, [carry])
        halo = ph.sb("halo", [128, 16, 4]); MEMSET('pool', halo.t[:], 0.0, [halo])
        nTb = [ph.sb("nTb%d" % i, [128, 8192], BF16) for i in range(2)]
        TS('dve', cf.t[:, 0:16], cf.t[:, 16:32], 0.5, None, ALU.mult, None, [cf], [cf])
        hb = ph.sb("hb", [128, 32])
        TS('dve', hb.t[:, 0:16], vec.t[:, C_BA:C_BA + 16], 0.5, None, ALU.mult, None, [vec], [hb])
        TS('dve', hb.t[:, 16:32], vec.t[:, C_BX:C_BX + 16], 0.5, None, ALU.mult, None, [vec], [hb])
        xre = [ph.sb("xre%d" % i, [128, 516]) for i in range(3)]
        xc = [ph.sb("xc%d" % i, [128, 512]) for i in range(8)]
        xcb = [ph.sb("xcb%d" % i, [128, 512], BF16) for i in range(2)]
        rr = [ph.sb("rr%d" % i, [128, 512]) for i in range(2)]
        ig = [ph.sb("ig%d" % i, [128, 512]) for i in range(5)]
        aa = [ph.sb("aa%d" % i, [128, 512]) for i in range(4)]
        mu = [ph.sb("mu%d" % i, [128, 512]) for i in range(3)]
        bb = [ph.sb("bb%d" % i, [128, 512]) for i in range(2)]
        hs = [ph.sb("hs%d" % i, [128, 512]) for i in range(2)]
        nbl = set()

        def stA(n):
            j, ct = n // 16, n % 16
            nb = nTb[j % 2]
            if j not in nbl:
                nbl.add(j)
                DMA('sp', nb.t[:].rearrange("p (k t) -> p k t", t=512),
                    nT_all[:, 512 * j:512 * j + 512].rearrange("(k p) t -> p k t", p=128), [], [nb], nb.b.name)
            pxr = ph.ps[n % 2]
            for kt in range(16):
                MM(pxr.t[:], wxr.t[:, kt, ct * 128:(ct + 1) * 128], nb.t[:, kt * 512:(kt + 1) * 512], kt == 0, kt == 15, [wxr, nb], [pxr])

        def stB(n):
            j, ct = n // 16, n % 16
            x_ = xre[n % 3]; pxr = ph.ps[n % 2]
            CP('pool', x_.t[:, 0:3], halo.t[:, ct, 0:3], [halo], [x_])
            CP('act', x_.t[:, 3:515], pxr.t[:], [pxr], [x_])
            CP('pool', halo.t[:, ct, 0:3], x_.t[:, 512:515], [x_], [halo])

        def stC(n):
            j, ct = n // 16, n % 16
            x_ = xre[n % 3]; c_ = xc[n % 8]
            cw = C_CW + 4 * ct
            TS('dve', c_.t[:], x_.t[:, 0:512], vec.t[:, cw:cw + 1], vec.t[:, C_CB + ct:C_CB + ct + 1], ALU.mult, ALU.add,
               [x_, vec], [c_])
            for w in range(1, 4):
                STT(c_.t[:], x_.t[:, w:w + 512], vec.t[:, cw + w:cw + w + 1], c_.t[:], ALU.mult, ALU.add,
                    [x_, vec, c_], [c_])

        def stD(n):
            CP('pool', xcb[n % 2].t[:], xc[n % 8].t[:], [xc[n % 8]], [xcb[n % 2]])

        def stE(n):
            j, ct = n // 16, n % 16
            cb_ = xcb[n % 2]
            pr = ph.ps[2 + (n % 2)]; pi = ph.ps[4 + (n % 2)]
            MM(pr.t[:], wga.t[:, ct, :], cb_.t[:], True, True, [wga, cb_], [pr])
            MM(pi.t[:], wgx.t[:, ct, :], cb_.t[:], True, True, [wgx, cb_], [pi])

        def stF(n):
            j, ct = n // 16, n % 16
            pr = ph.ps[2 + (n % 2)]; pi = ph.ps[4 + (n % 2)]
            ACT(rr[n % 2].t[:], pr.t[:], AF.Tanh, [pr, hb], [rr[n % 2]], bias=hb.t[:, ct:ct + 1], scale=0.5)
            ACT(ig[n % 5].t[:], pi.t[:], AF.Tanh, [pi, hb], [ig[n % 5]], bias=hb.t[:, 16 + ct:17 + ct], scale=0.5)

        def stG(n):
            j, ct = n // 16, n % 16
            ACT(aa[n % 4].t[:], rr[n % 2].t[:], AF.Exp, [rr[n % 2], cf], [aa[n % 4]], bias=cf.t[:, ct:ct + 1], scale=cf.t[:, ct:ct + 1])

        def stH(n):
            TT('pool', mu[n % 3].t[:], aa[n % 4].t[:], aa[n % 4].t[:], ALU.mult, [aa[n % 4]], [mu[n % 3]])

        def stI(n):
            j, ct = n // 16, n % 16
            m_ = mu[n % 3]
            ACT(m_.t[:], m_.t[:], AF.Sqrt, [m_, vec], [m_], bias=vec.t[:, C_ONE:C_ONE + 1], scale=-1.0)

        def stJ(n):
            j, ct = n // 16, n % 16
            m_ = mu[n % 3]; b_ = bb[n % 2]; h_ = hs[n % 2]; c_ = xc[n % 8]; a_ = aa[n % 4]; i_ = ig[n % 5]
            if j == 0:
                MEMSET('dve', m_.t[:, 0:1], 1.0, [m_])
            STT(b_.t[:], i_.t[:], 1.0, m_.t[:], ALU.add, ALU.mult, [i_, m_], [b_])
            STT(b_.t[:], b_.t[:], 0.5, c_.t[:], ALU.mult, ALU.mult, [b_, c_], [b_])
            S.op('dve', lambda e: e.tensor_tensor_scan(out=h_.t[:], data0=a_.t[:], data1=b_.t[:], initial=carry.t[:, ct:ct + 1],
                                                       op0=ALU.mult, op1=ALU.add), bl([a_, b_, carry]), bl([h_]))
            CP('pool', carry.t[:, ct:ct + 1], h_.t[:, 511:512], [h_], [carry])
            for half in ([0] if j < 8 else [1]):
                mc = C_OWN + 2 * j + half
                STT(hown.t[:, ct, half * 512:(half + 1) * 512], h_.t[:], vec.t[:, mc:mc + 1],
                    hown.t[:, ct, half * 512:(half + 1) * 512], ALU.mult, ALU.add, [h_, vec, hown], [hown])

        NU2 = 256
        stages = [stA, stB, stC, stD, stE, stF, stG, stH, stI, stJ]
        for m in range(NU2 + len(stages)):
            for k, st in enumerate(stages):
                n = m - k
                if 0 <= n < NU2:
                    st(n)
        DMA('sp', hown_d.rearrange("(k p) t -> p k t", p=128), hown.t[:], [hown], [], 'hownst')

    def linearT(ph, w_ap, KT, N, in_tl, T, evac, stgs, wbfs, cb=256):
        nblk = N // cb
        for b in range(nblk):
            wb = wbfs[b % len(wbfs)]
            DMA('pool', wb.t[:, :KT * cb].rearrange("p (k c) -> p k c", c=cb),
                w_ap[:, b * cb:(b + 1) * cb].rearrange("(k p) c -> p k c", p=128), [], [wb], wb.b.name)
            for o in range(cb // 128):
                for th in range(T // 512):
                    p = ph.nps()
                    for kt in range(KT):
                        MM(p.t[:], wb.t[:, kt * cb + o * 128:kt * cb + o * 128 + 128],
                           in_tl.t[:, kt * T + th * 512:kt * T + th * 512 + 512], kt == 0, kt == KT - 1, [wb, in_tl], [p])
                    evac(b * (cb // 128) + o, th, p)

    def load_norm_own(ph, ones, vec, src_d, gcol, dst, keep=None):
        sub = ExitStack()
        ph2 = Phase(); ph2.st = sub
        xh = [ph2.sb("lnx%d" % i, [128, 2048]) for i in range(4)]
        sq = ph2.sb("lnsq", [128, 8192], BF16)
        rt = ph2.sb("lnrt", [128, 512]); rstd = ph2.sb("lnrs", [128, 512])
        for th in range(2):
            for q in range(4):
                DMA('sp', xh[q].t[:].rearrange("p (k t) -> p k t", t=512),
                    src_d[q * 512:(q + 1) * 512, th * 512:(th + 1) * 512].rearrange("(k p) t -> p k t", p=128), [], [xh[q]], xh[q].b.name)
            psq = ph.nps()
            for kt in range(16):
                ACT(sq.t[:, kt * 512:(kt + 1) * 512], xh[kt // 4].t[:, (kt % 4) * 512:(kt % 4 + 1) * 512], AF.Square, [xh[kt // 4]], [sq])
            for kt in range(16):
                MM(psq.t[:], ones.t[:], sq.t[:, kt * 512:(kt + 1) * 512], kt == 0, kt == 15, [ones, sq], [psq])
            ACT(rt.t[:], psq.t[:], AF.Sqrt, [psq, vec], [rt], bias=vec.t[:, C_EPS:C_EPS + 1], scale=1.0 / 2048.0)
            RCP(rstd.t[:], rt.t[:], [rt], [rstd])
            for kt in range(16):
                STT(dst.t[:, kt * 1024 + th * 512:kt * 1024 + th * 512 + 512], xh[kt // 4].t[:, (kt % 4) * 512:(kt % 4 + 1) * 512],
                    vec.t[:, gcol + kt:gcol + kt + 1], rstd.t[:], ALU.mult, ALU.mult, [xh[kt // 4], vec, rstd], [dst])
                if keep is not None:
                    CP('pool', keep.t[:, kt * 1024 + th * 512:kt * 1024 + th * 512 + 512],
                       xh[kt // 4].t[:, (kt % 4) * 512:(kt % 4 + 1) * 512], [xh[kt // 4]], [keep])
        S.barrier()
        S.flush()
        sub.close()

    with phase() as ph:
        vec, ident, ones = consts(ph)
        ph.psring(8, "o1p")
        nown = ph.sb("nown", [128, 16384], BF16)
        load_norm_own(ph, ones, vec, xoT, C_ATTN, nown)
        DMA('pool', nown_d.rearrange("(k p) t -> p k t", p=128), nown.t[:].rearrange("p (k t) -> p k t", t=1024), [nown], [], 'nownst')
        stgs = None
        wbfs = [ph.sb("wbf%d" % i, [128, 4096], BF16) for i in range(4)]
        cq = ph.sb("cq", [128, 6 * 1024])
        linearT(ph, w_in[:, 0:768], 16, 768, nown, 1024,
                lambda ot, th, p: CP(ev_eng(), cq.t[:, ot * 1024 + th * 512:ot * 1024 + th * 512 + 512], p.t[:], [p], [cq]),
                stgs, wbfs)
        cqn = ph.sb("cqn", [128, 6 * 1024], BF16)
        sq6 = ph.sb("sq6", [128, 6 * 512], BF16)
        rt = ph.sb("rt", [128, 512]); rstd = ph.sb("rstd", [128, 512])
        for th in range(2):
            psq = ph.nps()
            for kt in range(6):
                ACT(sq6.t[:, kt * 512:(kt + 1) * 512], cq.t[:, kt * 1024 + th * 512:kt * 1024 + th * 512 + 512], AF.Square, [cq], [sq6])
            for kt in range(6):
                MM(psq.t[:], ones.t[:], sq6.t[:, kt * 512:(kt + 1) * 512], kt == 0, kt == 5, [ones, sq6], [psq])
            ACT(rt.t[:], psq.t[:], AF.Sqrt, [psq, vec], [rt], bias=vec.t[:, C_EPS:C_EPS + 1], scale=1.0 / 768.0)
            RCP(rstd.t[:], rt.t[:], [rt], [rstd])
            for kt in range(6):
                STT(cqn.t[:, kt * 1024 + th * 512:kt * 1024 + th * 512 + 512], cq.t[:, kt * 1024 + th * 512:kt * 1024 + th * 512 + 512],
                    vec.t[:, C_QN + kt:C_QN + kt + 1], rstd.t[:], ALU.mult, ALU.mult, [cq, vec, rstd], [cqn])
        cso = ph.sb("cso", [64, 2, 1024])
        DMA('sp', cso.t[:], cs[:, :, 8192:9216].rearrange("c p t -> p c t"), [], [cso], 'cso')
        qst = [ph.sb("qst%d" % i, [128, 1024], BF16) for i in range(2)]
        qpst = [ph.sb("qpst%d" % i, [64, 1024], BF16) for i in range(2)]
        q1 = ph.sb("q1", [64, 512]); q2 = ph.sb("q2", [64, 512])
        for h in range(16):
            wb = wbfs[h % 4]
            DMA('pool', wb.t[:, 0:6 * 256].rearrange("p (k c) -> p k c", c=256)[:, :, 0:192],
                w_uq[:, h * 192:(h + 1) * 192].rearrange("(k p) c -> p k c", p=128), [], [wb], wb.b.name)
            DMA('pool', wb.t[:, 0:6 * 256].rearrange("p (k c) -> p k c", c=256)[:, :, 192:256],
                w_uqsw[:, h * 64:(h + 1) * 64].rearrange("(k p) c -> p k c", p=128), [], [wb], wb.b.name)
            qs = qst[h % 2]; qp = qpst[h % 2]
            for th in range(2):
                p = ph.nps(); pp = ph.nps(); pw = ph.nps()
                for kt in range(6):
                    MM(p.t[:], wb.t[:, kt * 256:kt * 256 + 128], cqn.t[:, kt * 1024 + th * 512:kt * 1024 + th * 512 + 512], kt == 0, kt == 5, [wb, cqn], [p])
                for kt in range(6):
                    MM(pp.t[0:64, :], wb.t[:, kt * 256 + 128:kt * 256 + 192], cqn.t[:, kt * 1024 + th * 512:kt * 1024 + th * 512 + 512], kt == 0, kt == 5, [wb, cqn], [pp])
                for kt in range(6):
                    MM(pw.t[0:64, :], wb.t[:, kt * 256 + 192:kt * 256 + 256], cqn.t[:, kt * 1024 + th * 512:kt * 1024 + th * 512 + 512], kt == 0, kt == 5, [wb, cqn], [pw])
                CP('act', qs.t[:, th * 512:(th + 1) * 512], p.t[:], [p], [qs])
                TT('dve', q1.t[:], pp.t[0:64, :], cso.t[:, 0, th * 512:(th + 1) * 512], ALU.mult, [pp, cso], [q1])
                TT('dve', q2.t[:], pw.t[0:64, :], cso.t[:, 1, th * 512:(th + 1) * 512], ALU.mult, [pw, cso], [q2])
                TT('dve', qp.t[:, th * 512:(th + 1) * 512], q1.t[:], q2.t[:], ALU.add, [q1, q2], [qp])
            DMA('pool', QT_all[h], qs.t[:], [qs], [], qs.b.name + 'st')
            DMA('pool', QPE_all[h], qp.t[:], [qp], [], qp.b.name + 'st')

    with phase() as ph:
        vec, ident, ones = consts(ph)
        pS = [ph.psum("pS%d" % i, [128, 1024]) for i in range(3)]
        pO = [ph.psum("pO%d" % i, [128, 512]) for i in range(2)]
        ring = [0]
        slot = {}
        kpe = ph.sb("kpe", [64, TK], BF16)
        DMA('sp', kpe.t[:], kpe_all, [], [kpe], 'kpe')
        msk = ph.sb("msk", [128, 4, 512])
        MEMSET('pool', msk.t[:], 0.0, [msk])
        for d in range(4):
            S.op('pool', (lambda d=d: (lambda e: e.affine_select(out=msk.t[:, d, :], in_=msk.t[:, d, :], pattern=[[1, 512]],
                                                                 compare_op=ALU.is_ge, fill=-1.0e5, base=-128 * d,
                                                                 channel_multiplier=-1)))(), [msk.b], [msk.b])
        KTb = [ph.sb("KTb%d" % i, [128, TK], BF16) for i in range(2)]
        Vb = [ph.sb("Vb%d" % i, [128, 72, 128], BF16) for i in range(2)]
        Qb = [ph.sb("Qb%d" % i, [128, 1024], BF16) for i in range(2)]
        QPb = [ph.sb("QPb%d" % i, [64, 1024], BF16) for i in range(2)]
        PT = [ph.sb("PT%d" % i, [128, 1024], BF16) for i in range(3)]
        tmpm = [ph.sb("tmpm%d" % i, [128, 512]) for i in range(2)]
        rl = ph.sb("rl", [128, 1024])
        Ost = [ph.sb("Ost%d" % i, [128, 1024], BF16) for i in range(2)]
        scale = 192.0 ** -0.5
        Pacc = [ph.sb("Pacc%d" % i, [128, 1024]) for i in range(2)]
        Phi = ph.sb("Phi", [128, 1024], BF16)
        Plo = ph.sb("Plo", [128, 1024], BF16)
        KBS = list(range(60)) + list(range(64, 72))
        units = [(h, kb) for h in range(16) for kb in KBS]
        loaded = set()

        def groups(kb):
            if kb < 28:
                return [0, 1]
            if kb < 60:
                return [1]
            if kb < 68:
                return [0]
            return [1]

        def bufs(h):
            return KTb[h % 2], Vb[h % 2], Qb[h % 2], QPb[h % 2]

        def issue_qk(u):
            h, kb = units[u]
            kt_, vb, qb, qpb = bufs(h)
            if h not in loaded:
                loaded.add(h)
                DMA('sp', kt_.t[:], Kt_all[h], [], [kt_], kt_.b.name)
                DMA('sp', vb.t[:], V_all[h], [], [vb], vb.b.name)
                DMA('sp', qb.t[:], QT_all[h], [], [qb], qb.b.name)
                DMA('sp', qpb.t[:], QPE_all[h], [], [qpb], qpb.b.name)
            slot[u] = [x for x in range(3) if x not in slot.values()][0]
            ps = pS[slot[u]]
            gs = groups(kb)
            for g in gs:
                MM(ps.t[:, g * 512:(g + 1) * 512], kt_.t[:, kb * 128:(kb + 1) * 128], qb.t[:, g * 512:(g + 1) * 512], True, False, [kt_, qb], [ps])
            for g in gs:
                MM(ps.t[:, g * 512:(g + 1) * 512], kpe.t[0:64, kb * 128:(kb + 1) * 128], qpb.t[0:64, g * 512:(g + 1) * 512], False, True, [kpe, qpb], [ps])

        def issue_pv(u):
            h, kb = units[u]
            kt_, vb, qb, qpb = bufs(h)
            ps = pS[slot.pop(u)]; pt = PT[u % 3]
            pa = Pacc[h % 2]
            gs = groups(kb)
            if kb < 60:
                for g in gs:
                    bc = (C_VIS if g == 0 else C_VIS1) + kb // 4
                    ACT(pt.t[:, g * 512:(g + 1) * 512], ps.t[:, g * 512:(g + 1) * 512], AF.Exp, [ps, vec], [pt], bias=vec.t[:, bc:bc + 1], scale=scale)
            else:
                g = gs[0]
                jj = (kb - 64) % 4
                tm = tmpm[u % 2]
                TT('dve', tm.t[:], ps.t[:, g * 512:(g + 1) * 512], msk.t[:, jj, :], ALU.add, [ps, msk], [tm])
                ACT(pt.t[:, g * 512:(g + 1) * 512], tm.t[:], AF.Exp, [tm], [pt], scale=scale)
            for g in gs:
                last = (kb == 67) if g == 0 else (kb == 71)
                MM(pO[g].t[:], vb.t[:, kb, :], pt.t[:, g * 512:(g + 1) * 512], kb == 0, last, [vb, pt], [pO[g]])
            lo_, hi_ = gs[0] * 512, (gs[-1] + 1) * 512
            if kb == 0:
                CP('dve', pa.t[:], pt.t[:], [pt], [pa])
            else:
                TT('dve', pa.t[:, lo_:hi_], pt.t[:, lo_:hi_], pa.t[:, lo_:hi_], ALU.add, [pt, pa], [pa])
            if kb == 71:
                CP('dve', Phi.t[:], pa.t[:], [pa], [Phi])
                TT('dve', Plo.t[:], pa.t[:], Phi.t[:], ALU.subtract, [pa, Phi], [Plo])
                pL = pS[[x for x in range(3) if x not in slot.values()][0]]
                for g in range(2):
                    MM(pL.t[:, g * 512:(g + 1) * 512], ones.t[:], Phi.t[:, g * 512:(g + 1) * 512], True, False, [ones, Phi], [pL])
                    MM(pL.t[:, g * 512:(g + 1) * 512], ones.t[:], Plo.t[:, g * 512:(g + 1) * 512], False, True, [ones, Plo], [pL])
                os_ = Ost[h % 2]
                RCP(rl.t[:], pL.t[:], [pL], [rl])
                for g in range(2):
                    TT('dve', os_.t[:, g * 512:(g + 1) * 512], pO[g].t[:], rl.t[:, g * 512:(g + 1) * 512], ALU.mult, [pO[g], rl], [os_])
                DMA('pool', OT_all[h], os_.t[:], [os_], [], os_.b.name + 'st')

        NU = len(units)
        for u in range(NU + 2):
            if u < NU:
                issue_qk(u)
            if u >= 2:
                issue_pv(u - 2)

    with phase() as ph:
        vec, ident, ones = consts(ph)
        ph.psring(8, "o2p")
        stgs = None
        wbfs = [ph.sb("wbf%d" % i, [128, 4096], BF16) for i in range(4)]
        yg = ph.sb("yg", [128, 16384], BF16)
        DMA('sp', yg.t[:].rearrange("p (k t) -> p k t", t=1024), hown_d.rearrange("(k p) t -> p k t", p=128), [], [yg], 'ygl')
        mix = ph.sb("mix", [128, 16384], BF16)
        gas = ph.sb("gas", [128, 16384], BF16)
        gtmp = [ph.sb("gtmp%d" % i, [128, 512]) for i in range(2)]
        cgt = [0]
        with ExitStack() as sub:
            ph2 = Phase(); ph2.st = sub; ph2.ps = ph.ps
            ph2.nps = ph.nps
            nown = ph2.sb("nown", [128, 16384], BF16)
            DMA('sp', nown.t[:].rearrange("p (k t) -> p k t", t=1024), nown_d.rearrange("(k p) t -> p k t", p=128), [], [nown], 'nownl')

            def ev_yr(ot, th, p):
                g_ = gtmp[cgt[0] % 2]; cgt[0] += 1
                ACT(g_.t[:], p.t[:], AF.Gelu_apprx_tanh, [p], [g_])
                sl = slice(ot * 1024 + th * 512, ot * 1024 + th * 512 + 512)
                TT('dve', yg.t[:, sl], g_.t[:], yg.t[:, sl], ALU.mult, [g_, yg], [yg])
            linearT(ph, w_in[:, 3392:5440], 16, 2048, nown, 1024, ev_yr, stgs, wbfs)

            def ev_gr(ot, th, p):
                sl = slice(ot * 1024 + th * 512, ot * 1024 + th * 512 + 512)
                ACT(mix.t[:, sl], p.t[:], AF.Sigmoid, [p, vec], [mix], bias=vec.t[:, C_BGR + ot:C_BGR + ot + 1])
            linearT(ph, w_in[:, 7488:9536], 16, 2048, nown, 1024, ev_gr, stgs, wbfs)

            def ev_ga(ot, th, p):
                sl = slice(ot * 1024 + th * 512, ot * 1024 + th * 512 + 512)
                ACT(gas.t[:, sl], p.t[:], AF.Sigmoid, [p, vec], [gas], bias=vec.t[:, C_BGA + ot:C_BGA + ot + 1])
            linearT(ph, w_in[:, 5440:7488], 16, 2048, nown, 1024, ev_ga, stgs, wbfs)
            S.barrier()
            S.flush()

        def ev_rnn(ot, th, p):
            sl = slice(ot * 1024 + th * 512, ot * 1024 + th * 512 + 512)
            TT('dve', mix.t[:, sl], p.t[:], mix.t[:, sl], ALU.mult, [p, mix], [mix])
        linearT(ph, w_ro, 16, 2048, yg, 1024, ev_rnn, stgs, wbfs)
        S.barrier()
        DMA('sp', yg.t[:].rearrange("p (k t) -> p k t", t=1024), OT_all.rearrange("h p t -> p h t"), [], [yg], 'ygl')

        def ev_att(ot, th, p):
            g_ = gtmp[cgt[0] % 2]; cgt[0] += 1
            sl = slice(ot * 1024 + th * 512, ot * 1024 + th * 512 + 512)
            TT('dve', g_.t[:], p.t[:], gas.t[:, sl], ALU.mult, [p, gas], [g_])
            TT('dve', mix.t[:, sl], g_.t[:], mix.t[:, sl], ALU.add, [g_, mix], [mix])
        linearT(ph, w_ao, 16, 2048, yg, 1024, ev_att, stgs, wbfs)
        xr_ = [ph.sb("xres%d" % i, [128, 512]) for i in range(2)]

        def ev_out(ot, th, p):
            x_ = xr_[cgt[0] % 2]; cgt[0] += 1
            DMA('sp', x_.t[:], xoT[ot * 128:(ot + 1) * 128, th * 512:(th + 1) * 512], [], [x_], x_.b.name)
            TT('dve', x_.t[:], p.t[:], x_.t[:], ALU.add, [p, x_], [x_])
            DMA('pool', h1_d[ot * 128:(ot + 1) * 128, th * 512:(th + 1) * 512], x_.t[:], [x_], [], x_.b.name)
        linearT(ph, w_out, 16, 2048, mix, 1024, ev_out, stgs, wbfs)

    for half in range(2):
        with phase() as ph:
            vec, ident, ones = consts(ph)
            ph.psring(8, "pp")
            T0 = half * 512
            hn = ph.sb("hn", [128, 16 * 512], BF16)
            s12 = ph.sb("s12", [128, 4, 16, 128])
            tau = ph.sb("tau", [128, 32]); cbias = ph.sb("cbias", [128, 32])
            ntau = ph.sb("ntau", [128, 32]); b3 = ph.sb("b3", [128, 32])
            with ExitStack() as sub:
                ph2 = Phase(); ph2.st = sub; ph2.ps = ph.ps; ph2.nps = ph.nps
                xh = [ph2.sb("px%d" % i, [128, 2048]) for i in range(4)]
                sq = ph2.sb("psq", [128, 8192], BF16)
                rt = ph2.sb("prt", [128, 512]); rstd = ph2.sb("prs", [128, 512])
                for q in range(4):
                    DMA('sp', xh[q].t[:].rearrange("p (k t) -> p k t", t=512),
                        h1_d[q * 512:(q + 1) * 512, T0:T0 + 512].rearrange("(k p) t -> p k t", p=128), [], [xh[q]], xh[q].b.name)
                psq = ph.nps()
                rmsnorm_T(ph, ones, vec, lambda kt: (xh[kt // 4].t[:, (kt % 4) * 512:(kt % 4 + 1) * 512], xh[kt // 4]),
                          16, 512, C_FFN, hn, 2048.0, sq, rt, rstd, psq)
                stgs = None
                wbfs = [ph2.sb("wbf%d" % i, [128, 4096], BF16) for i in range(4)]
                qp = ph2.sb("qp", [128, 16 * 512], BF16)
                linearT(ph, w_pq, 16, 2048, hn, 512,
                        lambda ot, th, p: CP(ev_eng(), qp.t[:, ot * 512:(ot + 1) * 512], p.t[:], [p], [qp]), stgs, wbfs)
                skb = ph2.sb("skb", [128, 16, 128], BF16)
                DMA('pool', skb.t[:], skT, [], [skb], 'skb')
                top = ph2.sb("top", [128, 16, 16]); wk1 = ph2.sb("wk1", [128, 128])
                cand = ph2.sb("cand", [128, 8, 256]); wk2 = ph2.sb("wk2", [128, 256])
                best = ph2.sb("best", [128, 8, 16]); negm = ph2.sb("negm", [128, 8])
                e16 = ph2.sb("e16", [128, 8, 16]); Z = ph2.sb("Z", [128, 8]); lnZ = ph2.sb("lnZ", [128, 8])
                pst_top = top.t[:].ap[0][0]
                for tt in range(4):
                    for hh in range(16):
                        if hh % 4 == 0:
                            p = ph.nps()
                        MM(p.t[:, (hh % 4) * 128:(hh % 4 + 1) * 128], qp.t[:, hh * 512 + tt * 128:hh * 512 + tt * 128 + 128], skb.t[:, hh, :],
                           True, True, [qp, skb], [p])
                        if hh % 4 == 3:
                            CP(ev_eng(), s12.t[:, tt, hh - 3:hh + 1, :].rearrange("p a b -> p (a b)"), p.t[:], [p], [s12])
                    for hh in range(16):
                        S.op('dve', (lambda tt=tt, hh=hh: (lambda e: e.max(out=top.t[:, hh, 0:8], in_=s12.t[:, tt, hh, :])))(), bl([s12]), bl([top]))
                        S.op('dve', (lambda tt=tt, hh=hh: (lambda e: e.match_replace(out=wk1.t[:], in_to_replace=top.t[:, hh, 0:8],
                                                                                   in_values=s12.t[:, tt, hh, :], imm_value=-1.0e30)))(),
                             bl([s12, top]), bl([wk1]))
                        S.op('dve', (lambda hh=hh: (lambda e: e.max(out=top.t[:, hh, 8:16], in_=wk1.t[:])))(), bl([wk1]), bl([top]))
                    for h in range(8):
                        in0 = bass.AP(top.t, (2 * h) * 16, [[pst_top, 128], [1, 16], [0, 16]])
                        in1 = bass.AP(top.t, (2 * h + 1) * 16, [[pst_top, 128], [0, 16], [1, 16]])
                        TT('dve', cand.t[:, h, :].rearrange("p (a b) -> p a b", b=16), in0, in1, ALU.add, [top], [cand])
                        S.op('dve', (lambda h=h: (lambda e: e.max(out=best.t[:, h, 0:8], in_=cand.t[:, h, :])))(), bl([cand]), bl([best]))
                        S.op('dve', (lambda h=h: (lambda e: e.match_replace(out=wk2.t[:], in_to_replace=best.t[:, h, 0:8],
                                                                           in_values=cand.t[:, h, :], imm_value=-1.0e30)))(),
                             bl([cand, best]), bl([wk2]))
                        S.op('dve', (lambda h=h: (lambda e: e.max(out=best.t[:, h, 8:16], in_=wk2.t[:])))(), bl([wk2]), bl([best]))
                    TS('dve', negm.t[:], best.t[:, :, 0], -1.0, None, ALU.mult, None, [best], [negm])
                    CP('dve', tau.t[:, tt * 8:(tt + 1) * 8], best.t[:, :, 15], [best], [tau])
                    pst_b = best.t[:].ap[0][0]
                    TT('dve', e16.t[:], best.t[:], bass.AP(best.t, 0, [[pst_b, 128], [16, 8], [0, 16]]), ALU.subtract, [best], [e16])
                    ACT(e16.t[:], e16.t[:], AF.Exp, [e16], [e16])
                    S.op('dve', lambda e: e.tensor_reduce(out=Z.t[:], in_=e16.t[:], axis=mybir.AxisListType.X, op=ALU.add), bl([e16]), bl([Z]))
                    ACT(lnZ.t[:], Z.t[:], AF.Ln, [Z], [lnZ])
                    TT('dve', cbias.t[:, tt * 8:(tt + 1) * 8], negm.t[:], lnZ.t[:], ALU.subtract, [negm, lnZ], [cbias])
                    TS('dve', tau.t[:, tt * 8:(tt + 1) * 8], tau.t[:, tt * 8:(tt + 1) * 8], -1.0e-6, None, ALU.add, None, [tau], [tau])
                    TS('dve', ntau.t[:, tt * 8:(tt + 1) * 8], tau.t[:, tt * 8:(tt + 1) * 8], -1.0, None, ALU.mult, None, [tau], [ntau])
                    TT('dve', b3.t[:, tt * 8:(tt + 1) * 8], tau.t[:, tt * 8:(tt + 1) * 8], cbias.t[:, tt * 8:(tt + 1) * 8], ALU.add, [tau, cbias], [b3])
                S.barrier()
                S.flush()
            acc = ph.sb("acc", [128, 16 * 512])
            ubf2 = [ph.sb("ubf%d" % i, [128, 16, 512], BF16) for i in range(1)]
            vbf2 = [ph.sb("vbf%d" % i, [128, 4, 2048], BF16) for i in range(2)]
            gate = [ph.sb("gate%d" % i, [128, 8, 512], BF16) for i in range(2)]
            sc = [ph.sb("sc%d" % i, [128, 512]) for i in range(3)]
            ex = [ph.sb("ex%d" % i, [128, 512]) for i in range(3)]
            pst_s = s12.t[:].ap[0][0]
            GT = [ph.sb("GT%d" % i, [128, 4, 512], BF16) for i in range(2)]
            ge = [ph.sb("ge%d" % i, [128, 512]) for i in range(2)]
            WT = [ph.sb("WT%d" % i, [128, 4, 512], BF16) for i in range(4)]
            pG = ph.ps[0:2]; pA = ph.ps[2:4]; pD = ph.ps[4:8]
            NG = 32
            deferred = []
            gu = [0]
            kk = [0]
            pend = []

            def flush_pend():
                while pend:
                    g2, h2, s2_, e2_, t2 = pend.pop(0)
                    STT(g2.t[:, h2, :], s2_.t[:], tau.t[:, t2 * 8 + h2:t2 * 8 + h2 + 1], e2_.t[:], ALU.is_ge, ALU.mult, [s2_, tau, e2_], [g2])

            def gate_unit(G, u):
                tt, h = u // 8, u % 8
                k = kk[0] % 3
                kk[0] += 1
                s_ = sc[k]; e_ = ex[k]
                gt_ = gate[(G * 4 + tt) % 2]
                in0 = bass.AP(s12.t, tt * 2048 + (2 * h + 1) * 128, [[pst_s, 128], [0, 4], [1, 128]])
                in1 = bass.AP(s12.t, tt * 2048 + (2 * h) * 128 + G * 4, [[pst_s, 128], [1, 4], [0, 128]])
                TT('dve', s_.t[:].rearrange("p (a b) -> p a b", b=128), in0, in1, ALU.add, [s12], [s_])
                c_ = tt * 8 + h
                if h >= 5:
                    S.op('act', lambda e: e.activation(out=e_.t[:], in_=s_.t[:], func=AF.Prelu, bias=ntau.t[:, c_:c_ + 1], scale=1.0, alpha=1.0e9),
                         bl([s_, ntau]), bl([e_]))
                    ACT(gt_.t[:, h, :], e_.t[:], AF.Exp, [e_, b3], [gt_], bias=b3.t[:, c_:c_ + 1])
                    flush_pend()
                    return
                ACT(e_.t[:], s_.t[:], AF.Exp, [s_, cbias], [e_], bias=cbias.t[:, tt * 8 + h:tt * 8 + h + 1])
                pend.append((gt_, h, s_, e_, tt))
                if len(pend) > 1:
                    g2, h2, s2_, e2_, t2 = pend.pop(0)
                    STT(g2.t[:, h2, :], s2_.t[:], tau.t[:, t2 * 8 + h2:t2 * 8 + h2 + 1], e2_.t[:], ALU.is_ge, ALU.mult, [s2_, tau, e2_], [g2])

            def ident_mm(G, tt):
                gt_ = gate[(G * 4 + tt) % 2]
                pg = pG[(G * 4 + tt) % 2]
                for i in range(4):
                    for hh in range(8):
                        MM(pg.t[:, i * 128:(i + 1) * 128], gt_.t[:, hh, i * 128:(i + 1) * 128], ident.t[:], hh == 0, hh == 7, [gt_, ident], [pg])
                CP('act', GT[G % 2].t[:, :, tt * 128:(tt + 1) * 128], pg.t[:].rearrange("p (a b) -> p a b", b=128), [pg], [GT[G % 2]])

            def load_u(G):
                ub = ubf2[0]
                for q in range(4):
                    DMA('pool', ub.t[:, 4 * q:4 * q + 4, :],
                        puT[q * 512:(q + 1) * 512, G * 512:(G + 1) * 512].rearrange("(k p) c -> p k c", p=128), [], [ub], ub.b.name)

            def load_v(G):
                vb = vbf2[G % 2]
                for d in range(4):
                    DMA('pool', vb.t[:, d, :], pv[G * 512 + d * 128:G * 512 + d * 128 + 128, :], [], [vb], vb.b.name)

            def act_chain(G, i):
                pa = pA[i % 2]
                ub = ubf2[0]
                for kt in range(16):
                    MM(pa.t[:], ub.t[:, kt, i * 128:(i + 1) * 128], hn.t[:, kt * 512:(kt + 1) * 512], kt == 0, kt == 15, [ub, hn], [pa])
                g_ = ge[i % 2]
                ACT(g_.t[:], pa.t[:], AF.Gelu_apprx_tanh, [pa], [g_])
                TT('pool', WT[G % 4].t[:, i, :], g_.t[:], GT[G % 2].t[:, i, :], ALU.mult, [g_, GT[G % 2]], [WT[G % 4]])

            def v_chain(Ga, dt):
                pd = pD[dt % 4]
                n = 0
                for Gx in (Ga, Ga + 1):
                    vb = vbf2[Gx % 2]; wt = WT[Gx % 4]
                    for d in range(4):
                        MM(pd.t[:], vb.t[:, d, dt * 128:(dt + 1) * 128], wt.t[:, d, :], n == 0, n == 7, [vb, wt], [pd])
                        n += 1

                def add():
                    if Ga == 0:
                        CP('dve', acc.t[:, dt * 512:(dt + 1) * 512], pd.t[:], [pd], [acc])
                    else:
                        TT('dve', acc.t[:, dt * 512:(dt + 1) * 512], pd.t[:], acc.t[:, dt * 512:(dt + 1) * 512], ALU.add, [pd, acc], [acc])
                deferred.append((gu[0] + 3, add))

            def run_deferred(force=False):
                while deferred and (force or deferred[0][0] <= gu[0]):
                    deferred.pop(0)[1]()

            load_u(0); load_v(0); load_v(1)
            for G in range(NG + 1):
                for u in range(32):
                    gu[0] = G * 32 + u
                    if G < NG:
                        gate_unit(G, u)
                    elif u == 0:
                        flush_pend()
                    run_deferred()
                    if u % 8 == 4:
                        tq = u // 8 - 1
                        if tq >= 0 and G < NG:
                            ident_mm(G, tq)
                        elif tq < 0 and G >= 1:
                            ident_mm(G - 1, 3)
                    if G >= 1:
                        Gp = G - 1
                        if u in (6, 8, 10, 12):
                            act_chain(Gp, (u - 6) // 2)
                        if u == 13 and G < NG:
                            load_u(G)
                        if G % 2 == 0:
                            if 14 <= u < 30:
                                v_chain(G - 2, u - 14)
                            if u == 30:
                                if G < NG:
                                    load_v(G)
                                if G + 1 < NG:
                                    load_v(G + 1)
            run_deferred(force=True)
            hx = [ph.sb("hx%d" % i, [128, 512]) for i in range(2)]
            for dt in range(16):
                x_ = hx[dt % 2]
                DMA('sp', x_.t[:], h1_d[dt * 128:(dt + 1) * 128, T0:T0 + 512], [], [x_], x_.b.name)
                TT('dve', x_.t[:], x_.t[:], acc.t[:, dt * 512:(dt + 1) * 512], ALU.add, [x_, acc], [x_])
                DMA('pool', h2_d[dt * 128:(dt + 1) * 128, T0:T0 + 512], x_.t[:], [x_], [], x_.b.name)

    with phase() as ph:
        vec, ident, ones = consts(ph)
        ph.psring(8, "ep")
        h2 = ph.sb("h2", [128, 16384])
        hn3 = ph.sb("hn3", [128, 16384], BF16)
        load_norm_own(ph, ones, vec, h2_d, C_PLE, hn3, keep=h2)
        stgs = None
        wbfs = [ph.sb("wbf%d" % i, [128, 2048], BF16) for i in range(4)]
        gsb = ph.sb("gsb", [128, 16384], BF16)

        def ev_g(ot, th, p):
            sl = slice(ot * 1024 + th * 512, ot * 1024 + th * 512 + 512)
            ACT(gsb.t[:, sl], p.t[:], AF.Sigmoid, [p], [gsb])
        linearT(ph, w_pg, 16, 2048, hn3, 1024, ev_g, stgs, wbfs, cb=128)
        pb = ph.sb("pb", [128, 2048], BF16)
        DMA('pool', pb.t[:].rearrange("p (k t) -> p k t", t=1024), poT.rearrange("(k p) t -> p k t", p=128), [], [pb], 'pb')
        et = [ph.sb("et%d" % i, [128, 512]) for i in range(2)]
        ce = [0]

        def ev_p(ot, th, p):
            t_ = et[ce[0] % 2]; ce[0] += 1
            sl = slice(ot * 1024 + th * 512, ot * 1024 + th * 512 + 512)
            TT('dve', t_.t[:], p.t[:], gsb.t[:, sl], ALU.mult, [p, gsb], [t_])
            TT('dve', h2.t[:, sl], t_.t[:], h2.t[:, sl], ALU.add, [t_, h2], [h2])
        linearT(ph, w_pp, 2, 2048, pb, 1024, ev_p, stgs, wbfs, cb=128)
        sq = ph.sb("fsq", [128, 8192], BF16)
        rt = ph.sb("frt", [128, 512]); rstd = ph.sb("frs", [128, 512])
        ob = [ph.sb("ob%d" % i, [128, 512]) for i in range(2)]
        for th in range(2):
            psq = ph.nps()
            for kt in range(16):
                ACT(sq.t[:, kt * 512:(kt + 1) * 512], h2.t[:, kt * 1024 + th * 512:kt * 1024 + th * 512 + 512], AF.Square, [h2], [sq])
            for kt in range(16):
                MM(psq.t[:], ones.t[:], sq.t[:, kt * 512:(kt + 1) * 512], kt == 0, kt == 15, [ones, sq], [psq])
            ACT(rt.t[:], psq.t[:], AF.Sqrt, [psq, vec], [rt], bias=vec.t[:, C_EPS:C_EPS + 1], scale=1.0 / 2048.0)
            RCP(rstd.t[:], rt.t[:], [rt], [rstd])
            for kt in range(16):
                o_ = ob[kt % 2]
                STT(o_.t[:], h2.t[:, kt * 1024 + th * 512:kt * 1024 + th * 512 + 512], vec.t[:, C_FIN + kt:C_FIN + kt + 1], rstd.t[:],
                    ALU.mult, ALU.mult, [h2, vec, rstd], [o_])
                DMA('sp', outT[kt * 128:(kt + 1) * 128, th * 512:(th + 1) * 512], o_.t[:], [o_], [], o_.b.name)

    topstack.close()
    return nc


def _cm(v, n):
    return np.ascontiguousarray(np.asarray(v, np.float32).reshape(n, 128).T)


def prep_inputs(inp):
    f = lambda a: np.ascontiguousarray(np.asarray(a, dtype=np.float32))
    x = f(inp['x'])[0]
    p = f(inp['p'])[0, 0]
    xT = np.ascontiguousarray(x.T)
    w_in = f(inp['w_in'])[0]
    perm = np.concatenate([np.arange(32, 64), np.arange(0, 32)])
    w_krsw = np.ascontiguousarray(w_in[:, 1280:1344][:, perm])
    w_uq = f(inp['w_uq'])[0]
    w_uqsw = np.ascontiguousarray(w_uq.reshape(768, 16, 192)[:, :, 128:][:, :, perm].reshape(768, 1024))
    half = 32
    inv_freq = (1.0 / (np.float32(10000.0) ** (np.arange(half, dtype=np.float32) / np.float32(half)))).astype(np.float32)

    def tables(pos):
        ang = pos.astype(np.float32)[:, None] * inv_freq[None, :]
        c = np.cos(ang).astype(np.float32).T
        s = np.sin(ang).astype(np.float32).T
        return np.concatenate([c, c], 0), np.concatenate([-s, s], 0)
    vec_common = np.zeros((128, NV), np.float32)
    vec_common[:, C_ATTN:C_ATTN + 16] = _cm(inp['attn_norm'][0], 16)
    vec_common[:, C_QN:C_QN + 6] = _cm(inp['q_norm'][0], 6)
    vec_common[:, C_KVN:C_KVN + 4] = _cm(inp['kv_norm'][0], 4)
    vec_common[:, C_CB:C_CB + 16] = _cm(inp['conv_b'][0], 16)
    cw = np.asarray(inp['conv_w'], np.float32)[0]
    vec_common[:, C_CW:C_CW + 64] = cw.T.reshape(16, 128, 4).transpose(1, 0, 2).reshape(128, 64)
    vec_common[:, C_BA:C_BA + 16] = _cm(np.asarray(inp['b_rg_a'])[0].reshape(-1), 16)
    vec_common[:, C_BX:C_BX + 16] = _cm(np.asarray(inp['b_rg_x'])[0].reshape(-1), 16)
    vec_common[:, C_LAM:C_LAM + 16] = _cm(inp['lru_lambda'][0], 16)
    bg = np.asarray(inp['b_gate'], np.float32)[0]
    vec_common[:, C_BGA:C_BGA + 16] = _cm(bg[:2048], 16)
    vec_common[:, C_BGR:C_BGR + 16] = _cm(bg[2048:], 16)
    vec_common[:, C_FFN:C_FFN + 16] = _cm(inp['ffn_norm'][0], 16)
    vec_common[:, C_PLE:C_PLE + 16] = _cm(inp['ple_norm'][0], 16)
    vec_common[:, C_FIN:C_FIN + 16] = _cm(inp['final_norm'], 16)
    vec_common[:, C_EPS] = 1e-6
    vec_common[:, C_ONE] = 1.0
    vec_common[:, C_M1] = -1.0
    shared = {
        'xT': xT, 'w_in': w_in, 'w_krsw': w_krsw, 'w_uq': w_uq, 'w_uqsw': w_uqsw,
        'w_ukv': f(inp['w_ukv'])[0], 'w_attn_o': f(inp['w_attn_o'])[0],
        'w_rg_a': f(inp['w_rg_a'])[0], 'w_rg_x': f(inp['w_rg_x'])[0],
        'w_rnn_o': f(inp['w_rnn_o'])[0], 'w_out': f(inp['w_out'])[0], 'w_peer_q': f(inp['w_peer_q'])[0],
        'skT': np.ascontiguousarray(f(inp['peer_subkeys'])[0].reshape(16, 128, 128).transpose(2, 0, 1)),
        'puT': np.ascontiguousarray(f(inp['peer_u'])[0].T), 'pv': f(inp['peer_v'])[0],
        'w_ple_gate': f(inp['w_ple_gate'])[0], 'w_ple_proj': f(inp['w_ple_proj'])[0],
    }
    cg, sg = tables(np.arange(8192))
    maps = []
    for c in range(8):
        ca, cb_ = c, 15 - c
        own = np.concatenate([np.arange(512 * ca, 512 * ca + 512), np.arange(512 * cb_, 512 * cb_ + 512)])
        m = dict(shared)
        m['xoT'] = np.ascontiguousarray(xT[:, own])
        m['poT'] = np.ascontiguousarray(p[own].T)
        v = vec_common.copy()
        for j in range(16):
            v[:, C_VIS + j] = 0.0 if j < ca else NEG
            v[:, C_VIS1 + j] = 0.0 if j < cb_ else NEG
            v[:, C_OWN + 2 * j + 0] = 1.0 if j == ca else 0.0
            v[:, C_OWN + 2 * j + 1] = 1.0 if j == cb_ else 0.0
        m['vecs'] = v
        cs_ = np.zeros((2, 64, TK), np.float32)
        cs_[0, :, :8192] = cg; cs_[1, :, :8192] = sg
        cs_[0, :, 8192:] = cg[:, own]; cs_[1, :, 8192:] = sg[:, own]
        m['cs'] = cs_
        maps.append(m)
    return maps


def kernel(**inputs):
    maps = prep_inputs(inputs)
    nc = build_nc(False)
    res = run_bass_kernel_spmd(nc, maps, core_ids=list(range(8)))
    out = np.empty((8192, 2048), np.float32)
    for c, r in enumerate(res.results):
        o = np.asarray(r["outT"])
        out[512 * c:512 * c + 512] = o[:, :512].T
        out[512 * (15 - c):512 * (15 - c) + 512] = o[:, 512:].T
    return out[None]
```

```python
import numpy as np
from contextlib import ExitStack, contextmanager
import concourse.bass as bass
import concourse.mybir as mybir
from concourse.bass_utils import run_bass_kernel_spmd

F32 = mybir.dt.float32
BF16 = mybir.dt.bfloat16
AF = mybir.ActivationFunctionType
ALU = mybir.AluOpType

NV = 304
C_ATTN, C_QN, C_KVN, C_CB, C_CW, C_BA, C_BX, C_LAM, C_BGA, C_BGR, C_FFN, C_PLE, C_FIN = \
    0, 16, 22, 26, 42, 106, 122, 138, 154, 170, 186, 202, 218
C_EPS, C_ONE, C_ZERO, C_M1, C_VIS, C_OWN, C_VIS1 = 234, 235, 236, 237, 240, 256, 288
NEG = -30000.0
NCH = 18
TK = 9216


class Buf:
    def __init__(self, name):
        self.name = name
        self.w = None
        self.r = {}


class Tl:
    def __init__(self, t, name):
        self.t = t
        self.b = Buf(name)


_uid = [0]


def _un(name):
    _uid[0] += 1
    return "%s_%d" % (name, _uid[0])


class Sched:
    ENG = ('pe', 'act', 'dve', 'pool', 'sp')

    def __init__(self, nc, stack):
        self.nc = nc
        self.stack = stack
        self.prog = {k: [] for k in self.ENG}
        self.sems = {}
        self.cnt = {}
        self.waited = {k: {} for k in self.ENG}
        self.nsem = 0
        for k in self.ENG:
            self._newsem(k)

    def _newsem(self, key):
        s = self.stack.enter_context(self.nc.semaphore("s%d_%s" % (self.nsem, key[:12])))
        self.nsem += 1
        self.sems[key] = s
        self.cnt[key] = 0
        for e in self.ENG:
            self.waited[e].pop(key, None)

    def _need(self, eng, dep, waits):
        if dep is None:
            return
        key, sem, val = dep
        if self.sems.get(key) is not sem:
            return
        if key == 'pe' and eng == 'pe':
            return
        if self.waited[eng].get(key, 0) >= val:
            return
        waits[key] = max(waits.get(key, 0), val)

    def op(self, eng, fn, reads=(), writes=(), dma_key=None):
        waits = {}
        for b in reads:
            self._need(eng, b.w, waits)
        for b in writes:
            self._need(eng, b.w, waits)
            for k, (s, v) in b.r.items():
                self._need(eng, (k, s, v), waits)
        for k, v in waits.items():
            self.waited[eng][k] = v
        if dma_key is None:
            key, inc = eng, 1
        else:
            key, inc = dma_key, 16
            if key not in self.sems:
                self._newsem(key)
        self.cnt[key] += inc
        val = self.cnt[key]
        assert val < 60000, (key, val)
        sem = self.sems[key]
        self.prog[eng].append(([(self.sems[k], v) for k, v in waits.items()], fn, sem, inc))
        for b in reads:
            b.r[key] = (sem, val)
        for b in writes:
            b.w = (key, sem, val)
            b.r = {}

    def barrier(self, new_epoch=True):
        for eng in self.ENG:
            waits = {}
            for k, v in self.cnt.items():
                if v > 0:
                    self._need(eng, (k, self.sems[k], v), waits)
            for k, v in waits.items():
                self.waited[eng][k] = v
            if waits:
                self.prog[eng].append(([(self.sems[k], v) for k, v in waits.items()], None, None, 0))

    def flush(self):
        progs = self.prog
        with self.nc.Block() as block:
            def mk(engname):
                def body(engine):
                    for waits, fn, sem, inc in progs[engname]:
                        for s, v in waits:
                            engine.wait_ge(s, v)
                        if fn is not None:
                            fn(engine).then_inc(sem, inc)
                return body
            block.tensor(mk('pe'))
            block.scalar(mk('act'))
            block.vector(mk('dve'))
            block.gpsimd(mk('pool'))
            block.sync(mk('sp'))
        self.prog = {k: [] for k in self.ENG}


def build_nc(debug=False):
    nc = bass.Bass("TRN2", target_bir_lowering=False)

    def din(name, shape, dt=F32):
        return nc.dram_tensor(name, list(shape), dt, kind="ExternalInput").ap()

    def dscr(name, shape, dt):
        return nc.dram_tensor(name, list(shape), dt, kind="ExternalOutput" if debug else "Internal").ap()

    xT = din("xT", [2048, 8192]); xoT = din("xoT", [2048, 1024]); poT = din("poT", [256, 1024])
    vecs = din("vecs", [128, NV]); cs = din("cs", [2, 64, TK])
    w_in = din("w_in", [2048, 9536]); w_krsw = din("w_krsw", [2048, 64])
    w_uq = din("w_uq", [768, 3072]); w_uqsw = din("w_uqsw", [768, 1024])
    w_ukv = din("w_ukv", [512, 4096]); w_ao = din("w_attn_o", [2048, 2048])
    w_rga = din("w_rg_a", [16, 128, 128]); w_rgx = din("w_rg_x", [16, 128, 128])
    w_ro = din("w_rnn_o", [2048, 2048]); w_out = din("w_out", [2048, 2048]); w_pq = din("w_peer_q", [2048, 2048])
    skT = din("skT", [128, 16, 128]); puT = din("puT", [2048, 16384]); pv = din("pv", [16384, 2048])
    w_pg = din("w_ple_gate", [2048, 2048]); w_pp = din("w_ple_proj", [256, 2048])
    outT = nc.dram_tensor("outT", [2048, 1024], F32, kind="ExternalOutput").ap()

    nT_all = dscr("nT_all", [2048, 8192], BF16)
    Kt_all = dscr("Kt_all", [16, 128, TK], BF16)
    V_all = dscr("V_all", [16, 128, 72, 128], BF16)
    kpe_all = dscr("kpe_all", [64, TK], BF16)
    QT_all = dscr("QT_all", [16, 128, 1024], BF16)
    QPE_all = dscr("QPE_all", [16, 64, 1024], BF16)
    OT_all = dscr("OT_all", [16, 128, 1024], BF16)
    hown_d = dscr("hown_d", [2048, 1024], BF16)
    nown_d = dscr("nown_d", [2048, 1024], BF16)
    h1_d = dscr("h1_d", [2048, 1024], F32)
    h2_d = dscr("h2_d", [2048, 1024], F32)

    topstack = ExitStack()
    S = Sched(nc, topstack)

    def bl(x):
        return [y.b if isinstance(y, Tl) else y for y in x]

    def MM(ps, lhsT, rhs, start, stop, R, W):
        S.op('pe', lambda e: e.matmul(ps, lhsT=lhsT, rhs=rhs, start=start, stop=stop), bl(R), bl(W))

    def TR(ps, in_, ident, R, W):
        S.op('pe', lambda e: e.transpose(out=ps, in_=in_, identity=ident), bl(R), bl(W))

    def ACT(out, in_, func, R, W, bias=None, scale=None):
        kw = {}
        if bias is not None:
            kw['bias'] = bias
        if scale is not None:
            kw['scale'] = scale
        S.op('act', lambda e: e.activation(out=out, in_=in_, func=func, **kw), bl(R), bl(W))

    def TT(eng, out, in0, in1, op, R, W):
        S.op(eng, lambda e: e.tensor_tensor(out=out, in0=in0, in1=in1, op=op), bl(R), bl(W))

    def TS(eng, out, in0, s1, s2, op0, op1, R, W):
        if s2 is None:
            S.op(eng, lambda e: e.tensor_scalar(out=out, in0=in0, scalar1=s1, scalar2=None, op0=op0), bl(R), bl(W))
        else:
            S.op(eng, lambda e: e.tensor_scalar(out=out, in0=in0, scalar1=s1, scalar2=s2, op0=op0, op1=op1), bl(R), bl(W))

    def STT(out, in0, scalar, in1, op0, op1, R, W):
        S.op('dve', lambda e: e.scalar_tensor_tensor(out=out, in0=in0, scalar=scalar, in1=in1, op0=op0, op1=op1), bl(R), bl(W))

    def CP(eng, out, in_, R, W):
        if eng == 'act':
            S.op('act', lambda e: e.copy(out=out, in_=in_), bl(R), bl(W))
        else:
            S.op(eng, lambda e: e.tensor_copy(out=out, in_=in_), bl(R), bl(W))

    def RCP(out, in_, R, W):
        S.op('dve', lambda e: e.reciprocal(out=out, in_=in_), bl(R), bl(W))

    def MEMSET(eng, ap, val, W):
        S.op(eng, lambda e: e.memset(ap, val), [], bl(W))

    def DMA(eng, out, in_, R, W, key):
        S.op(eng, lambda e: e.dma_start(out=out, in_=in_), bl(R), bl(W), dma_key=key)

    class Phase:
        def __init__(self):
            self.st = ExitStack()
            self.ps = []
            self.psi = 0

        def sb(self, name, shape, dt=F32):
            return Tl(self.st.enter_context(nc.sbuf_tensor(_un(name), list(shape), dt)), name)

        def psum(self, name, shape, dt=F32):
            return Tl(self.st.enter_context(nc.psum_tensor(_un(name), list(shape), dt)), name)

        def psring(self, n, prefix):
            self.ps = [self.psum("%s%d" % (prefix, i), [128, 512]) for i in range(n)]
            self.psi = 0

        def nps(self):
            p = self.ps[self.psi % len(self.ps)]
            self.psi += 1
            return p

    @contextmanager
    def phase():
        ph = Phase()
        try:
            yield ph
            S.barrier()
            S.flush()
        finally:
            ph.st.close()
        for k in S.ENG:
            if S.cnt[k] > 28000:
                S._newsem(k)

    cnt = {'cast': 0, 'ev': 0}

    def cast_eng():
        cnt['cast'] += 1
        return ('pool', 'dve', 'act')[cnt['cast'] % 3] if False else 'pool'

    def ev_eng():
        cnt['ev'] += 1
        return 'act' if cnt['ev'] % 2 else 'dve'

    def consts(ph):
        vec = ph.sb("vec", [128, NV])
        DMA('sp', vec.t[:], vecs, [], [vec], 'vec')
        idf = ph.sb("idf", [128, 128])
        MEMSET('pool', idf.t[:], 0.0, [idf])
        S.op('pool', lambda e: e.affine_select(out=idf.t[:], in_=idf.t[:], pattern=[[-1, 128]],
                                               compare_op=ALU.not_equal, fill=1.0, base=0, channel_multiplier=1),
             [idf.b], [idf.b])
        ident = ph.sb("ident", [128, 128], BF16)
        CP('dve', ident.t[:], idf.t[:], [idf], [ident])
        ones = ph.sb("ones", [128, 128], BF16)
        MEMSET('pool', ones.t[:], 1.0, [ones])
        return vec, ident, ones

    def rmsnorm_T(ph, ones, vec, src_tiles, nkt, T, gcol, dst, D, sq, rt, rstd, psq):
        for kt in range(nkt):
            ap, tl = src_tiles(kt)
            ACT(sq.t[:, kt * T:(kt + 1) * T], ap, AF.Square, [tl], [sq])
        for kt in range(nkt):
            MM(psq.t[:, :T], ones.t[:], sq.t[:, kt * T:(kt + 1) * T], kt == 0, kt == nkt - 1, [ones, sq], [psq])
        ACT(rt.t[:, :T], psq.t[:, :T], AF.Sqrt, [psq, vec], [rt], bias=vec.t[:, C_EPS:C_EPS + 1], scale=1.0 / D)
        RCP(rstd.t[:, :T], rt.t[:, :T], [rt], [rstd])
        for kt in range(nkt):
            ap, tl = src_tiles(kt)
            STT(dst.t[:, kt * T:(kt + 1) * T], ap, vec.t[:, gcol + kt:gcol + kt + 1], rstd.t[:, :T], ALU.mult, ALU.mult,
                [tl, vec, rstd], [dst])

    with phase() as ph:
        vec, ident, ones = consts(ph)
        ph.psring(8, "g1p")
        wkv = ph.sb("wkv", [128, 16, 640], BF16)
        wk = ph.sb("wk", [128, 4, 2048], BF16)
        wv = ph.sb("wv", [128, 4, 2048], BF16)
        for kt in range(16):
            DMA('pool', wkv.t[:, kt, 0:576], w_in[kt * 128:(kt + 1) * 128, 768:1344], [], [wkv], 'wkv')
            DMA('pool', wkv.t[:, kt, 576:640], w_krsw[kt * 128:(kt + 1) * 128, :], [], [wkv], 'wkv')
        for kt in range(4):
            sv = w_ukv[kt * 128:(kt + 1) * 128, :].rearrange("p (h two d) -> p h two d", two=2, d=128)
            DMA('pool', wk.t[:, kt, :].rearrange("p (h d) -> p h d", d=128), sv[:, :, 0, :], [], [wk], 'wk')
            DMA('pool', wv.t[:, kt, :].rearrange("p (h d) -> p h d", d=128), sv[:, :, 1, :], [], [wv], 'wv')
        xp = [ph.sb("xp%d" % i, [128, 2048]) for i in range(4)]
        sq = ph.sb("sq", [128, 8192], BF16)
        nT = ph.sb("nT", [128, 8192], BF16)
        rt = ph.sb("rt", [128, 512]); rstd = ph.sb("rstd", [128, 512])
        rt2 = ph.sb("rt2", [128, 512]); rstd2 = ph.sb("rstd2", [128, 512])
        sqk = ph.sb("sqk", [128, 2048], BF16)
        ckvn2 = [ph.sb("ckvn%d" % i, [128, 2048], BF16) for i in range(2)]
        cst = ph.sb("cst", [64, 2, 512])
        t1 = ph.sb("t1", [64, 512]); t2 = ph.sb("t2", [64, 512])
        kpst = ph.sb("kpst", [64, 512], BF16)
        Kst = ph.sb("Kst", [128, 16, 512], BF16)
        Vst = ph.sb("Vst", [128, 4, 2048], BF16)
        P = ph.ps
        kvr = [0]

        def kvps():
            p = P[6 + (kvr[0] % 2)]
            kvr[0] += 1
            return p

        def partA(j):
            src = xT[:, 512 * j:512 * j + 512] if j < 16 else xoT[:, 512 * (j - 16):512 * (j - 16) + 512]
            for q in range(4):
                DMA('sp', xp[q].t[:].rearrange("p (k t) -> p k t", t=512),
                    src[q * 512:(q + 1) * 512, :].rearrange("(k p) t -> p k t", p=128), [], [xp[q]], xp[q].b.name)
            DMA('sp', cst.t[:], cs[:, :, 512 * j:512 * j + 512].rearrange("c p t -> p c t"), [], [cst], 'cst')
            rmsnorm_T(ph, ones, vec, lambda kt: (xp[kt // 4].t[:, (kt % 4) * 512:(kt % 4 + 1) * 512], xp[kt // 4]),
                      16, 512, C_ATTN, nT, 2048.0, sq, rt, rstd, P[0])
            if j < 16:
                DMA('pool', nT_all[:, 512 * j:512 * j + 512].rearrange("(k p) t -> p k t", p=128),
                    nT.t[:].rearrange("p (k t) -> p k t", t=512), [nT], [], 'nTst')

        def partB(j):
            ckvn = ckvn2[j % 2]
            pck = P[1:5]
            pkr = P[5]
            for o in range(4):
                for kt in range(16):
                    MM(pck[o].t[:], wkv.t[:, kt, o * 128:(o + 1) * 128], nT.t[:, kt * 512:(kt + 1) * 512], kt == 0, kt == 15,
                       [wkv, nT], [pck[o]])
            for kt in range(16):
                MM(pkr.t[0:64, :], wkv.t[:, kt, 512:576], nT.t[:, kt * 512:(kt + 1) * 512], kt == 0, kt == 15, [wkv, nT], [pkr])
            TT('dve', t1.t[:], pkr.t[0:64, :], cst.t[:, 0, :], ALU.mult, [pkr, cst], [t1])
            for kt in range(16):
                MM(pkr.t[0:64, :], wkv.t[:, kt, 576:640], nT.t[:, kt * 512:(kt + 1) * 512], kt == 0, kt == 15, [wkv, nT], [pkr])
            TT('dve', t2.t[:], pkr.t[0:64, :], cst.t[:, 1, :], ALU.mult, [pkr, cst], [t2])
            TT('dve', kpst.t[:], t1.t[:], t2.t[:], ALU.add, [t1, t2], [kpst])
            DMA('pool', kpe_all[:, 512 * j:512 * j + 512], kpst.t[:], [kpst], [], 'kpst')
            rmsnorm_T(ph, ones, vec, lambda o: (pck[o].t[:], pck[o]), 4, 512, C_KVN, ckvn, 512.0, sqk, rt2, rstd2, P[0])

        def partK(j):
            ckvn = ckvn2[j % 2]
            for h in range(16):
                p = kvps()
                for kt in range(4):
                    MM(p.t[:], wk.t[:, kt, h * 128:(h + 1) * 128], ckvn.t[:, kt * 512:(kt + 1) * 512], kt == 0, kt == 3, [wk, ckvn], [p])
                CP('act', Kst.t[:, h, :], p.t[:], [p], [Kst])
            DMA('pool', Kt_all[:, :, 512 * j:512 * j + 512].rearrange("h p t -> p h t"), Kst.t[:], [Kst], [], 'Kst')

        def partV(j):
            ckvn = ckvn2[j % 2]
            for tt in range(4):
                for cg in range(4):
                    p = kvps()
                    for kt in range(4):
                        MM(p.t[:], ckvn.t[:, kt * 512 + tt * 128:kt * 512 + tt * 128 + 128], wv.t[:, kt, cg * 512:(cg + 1) * 512],
                           kt == 0, kt == 3, [wv, ckvn], [p])
                    CP(ev_eng(), Vst.t[:, tt, cg * 512:(cg + 1) * 512], p.t[:], [p], [Vst])
            for tt in range(4):
                DMA('pool', V_all[:, :, 4 * j + tt, :].rearrange("h p d -> p h d"),
                    Vst.t[:, tt, :].rearrange("p (h d) -> p h d", d=128), [Vst], [], 'Vst')

        for j in range(NCH + 1):
            if j < NCH:
                partA(j)
            if j >= 1:
                partK(j - 1)
            if j < NCH:
                partB(j)
            if j >= 1:
                partV(j - 1)

    with phase() as ph:
        vec, ident, ones = consts(ph)
        ph.psring(6, "g2p")
        wxr = ph.sb("wxr", [128, 16, 2048], BF16)
        wga = ph.sb("wga", [128, 16, 128], BF16)
        wgx = ph.sb("wgx", [128, 16, 128], BF16)
        for kt in range(16):
            DMA('pool', wxr.t[:, kt, :], w_in[kt * 128:(kt + 1) * 128, 1344:3392], [], [wxr], 'wxr')
        DMA('pool', wga.t[:], w_rga.rearrange("b i j -> i b j"), [], [wga], 'wga')
        DMA('pool', wgx.t[:], w_rgx.rearrange("b i j -> i b j"), [], [wgx], 'wgx')
        cf = ph.sb("cf", [128, 48])
        ACT(cf.t[:, 0:16], vec.t[:, C_LAM:C_LAM + 16], AF.Exp, [vec], [cf], scale=-1.0)
        ACT(cf.t[:, 0:16], cf.t[:, 0:16], AF.Ln, [cf, vec], [cf], bias=vec.t[:, C_ONE:C_ONE + 1])
        TS('dve', cf.t[:, 16:32], cf.t[:, 0:16], -8.0, None, ALU.mult, None, [cf], [cf])
        TS('dve', cf.t[:, 32:48], cf.t[:, 0:16], -16.0, None, ALU.mult, None, [cf], [cf])
        hown = ph.sb("hown", [128, 16, 1024], BF16)
        MEMSET('pool', hown.t[:], 0.0, [hown])
        carry = ph.sb("carry", [128, 16]); MEMSET('pool', carry.t[:], 0.0, [carry])
        halo = ph.sb("halo", [128, 16, 4]); MEMSET('pool', halo.t[:], 0.0, [halo])
        nTb = [ph.sb("nTb%d" % i, [128, 8192], BF16) for i in range(2)]
        TS('dve', cf.t[:, 0:16], cf.t[:, 16:32], 0.5, None, ALU.mult, None, [cf], [cf])
        hb = ph.sb("hb", [128, 32])
        TS('dve', hb.t[:, 0:16], vec.t[:, C_BA:C_BA + 16], 0.5, None, ALU.mult, None, [vec], [hb])
        TS('dve', hb.t[:, 16:32], vec.t[:, C_BX:C_BX + 16], 0.5, None, ALU.mult, None, [vec], [hb])
        xre = [ph.sb("xre%d" % i, [128, 516]) for i in range(3)]
        xc = [ph.sb("xc%d" % i, [128, 512]) for i in range(8)]
        xcb = [ph.sb("xcb%d" % i, [128, 512], BF16) for i in range(2)]
        rr = [ph.sb("rr%d" % i, [128, 512]) for i in range(2)]
        ig = [ph.sb("ig%d" % i, [128, 512]) for i in range(5)]
        aa = [ph.sb("aa%d" % i, [128, 512]) for i in range(4)]
        mu = [ph.sb("mu%d" % i, [128, 512]) for i in range(3)]
        bb = [ph.sb("bb%d" % i, [128, 512]) for i in range(2)]
        hs = [ph.sb("hs%d" % i, [128, 512]) for i in range(2)]
        nbl = set()

        def stA(n):
            j, ct = n // 16, n % 16
            nb = nTb[j % 2]
            if j not in nbl:
                nbl.add(j)
                DMA('sp', nb.t[:].rearrange("p (k t) -> p k t", t=512),
                    nT_all[:, 512 * j:512 * j + 512].rearrange("(k p) t -> p k t", p=128), [], [nb], nb.b.name)
            pxr = ph.ps[n % 2]
            for kt in range(16):
                MM(pxr.t[:], wxr.t[:, kt, ct * 128:(ct + 1) * 128], nb.t[:, kt * 512:(kt + 1) * 512], kt == 0, kt == 15, [wxr, nb], [pxr])

        def stB(n):
            j, ct = n // 16, n % 16
            x_ = xre[n % 3]; pxr = ph.ps[n % 2]
            CP('pool', x_.t[:, 0:3], halo.t[:, ct, 0:3], [halo], [x_])
            CP('act', x_.t[:, 3:515], pxr.t[:], [pxr], [x_])
            CP('pool', halo.t[:, ct, 0:3], x_.t[:, 512:515], [x_], [halo])

        def stC(n):
            j, ct = n // 16, n % 16
            x_ = xre[n % 3]; c_ = xc[n % 8]
            cw = C_CW + 4 * ct
            TS('dve', c_.t[:], x_.t[:, 0:512], vec.t[:, cw:cw + 1], vec.t[:, C_CB + ct:C_CB + ct + 1], ALU.mult, ALU.add,
               [x_, vec], [c_])
            for w in range(1, 4):
                STT(c_.t[:], x_.t[:, w:w + 512], vec.t[:, cw + w:cw + w + 1], c_.t[:], ALU.mult, ALU.add,
                    [x_, vec, c_], [c_])

        def stD(n):
            CP('pool', xcb[n % 2].t[:], xc[n % 8].t[:], [xc[n % 8]], [xcb[n % 2]])

        def stE(n):
            j, ct = n // 16, n % 16
            cb_ = xcb[n % 2]
            pr = ph.ps[2 + (n % 2)]; pi = ph.ps[4 + (n % 2)]
            MM(pr.t[:], wga.t[:, ct, :], cb_.t[:], True, True, [wga, cb_], [pr])
            MM(pi.t[:], wgx.t[:, ct, :], cb_.t[:], True, True, [wgx, cb_], [pi])

        def stF(n):
            j, ct = n // 16, n % 16
            pr = ph.ps[2 + (n % 2)]; pi = ph.ps[4 + (n % 2)]
            ACT(rr[n % 2].t[:], pr.t[:], AF.Tanh, [pr, hb], [rr[n % 2]], bias=hb.t[:, ct:ct + 1], scale=0.5)
            ACT(ig[n % 5].t[:], pi.t[:], AF.Tanh, [pi, hb], [ig[n % 5]], bias=hb.t[:, 16 + ct:17 + ct], scale=0.5)

        def stG(n):
            j, ct = n // 16, n % 16
            ACT(aa[n % 4].t[:], rr[n % 2].t[:], AF.Exp, [rr[n % 2], cf], [aa[n % 4]], bias=cf.t[:, ct:ct + 1], scale=cf.t[:, ct:ct + 1])

        def stH(n):
            TT('pool', mu[n % 3].t[:], aa[n % 4].t[:], aa[n % 4].t[:], ALU.mult, [aa[n % 4]], [mu[n % 3]])

        def stI(n):
            j, ct = n // 16, n % 16
            m_ = mu[n % 3]
            ACT(m_.t[:], m_.t[:], AF.Sqrt, [m_, vec], [m_], bias=vec.t[:, C_ONE:C_ONE + 1], scale=-1.0)

        def stJ(n):
            j, ct = n // 16, n % 16
            m_ = mu[n % 3]; b_ = bb[n % 2]; h_ = hs[n % 2]; c_ = xc[n % 8]; a_ = aa[n % 4]; i_ = ig[n % 5]
            if j == 0:
                MEMSET('dve', m_.t[:, 0:1], 1.0, [m_])
            STT(b_.t[:], i_.t[:], 1.0, m_.t[:], ALU.add, ALU.mult, [i_, m_], [b_])
            STT(b_.t[:], b_.t[:], 0.5, c_.t[:], ALU.mult, ALU.mult, [b_, c_], [b_])
            S.op('dve', lambda e: e.tensor_tensor_scan(out=h_.t[:], data0=a_.t[:], data1=b_.t[:], initial=carry.t[:, ct:ct + 1],
                                                       op0=ALU.mult, op1=ALU.add), bl([a_, b_, carry]), bl([h_]))
            CP('pool', carry.t[:, ct:ct + 1], h_.t[:, 511:512], [h_], [carry])
            for half in ([0] if j < 8 else [1]):
                mc = C_OWN + 2 * j + half
                STT(hown.t[:, ct, half * 512:(half + 1) * 512], h_.t[:], vec.t[:, mc:mc + 1],
                    hown.t[:, ct, half * 512:(half + 1) * 512], ALU.mult, ALU.add, [h_, vec, hown], [hown])

        NU2 = 256
        stages = [stA, stB, stC, stD, stE, stF, stG, stH, stI, stJ]
        for m in range(NU2 + len(stages)):
            for k, st in enumerate(stages):
                n = m - k
                if 0 <= n < NU2:
                    st(n)
        DMA('sp', hown_d.rearrange("(k p) t -> p k t", p=128), hown.t[:], [hown], [], 'hownst')

    def linearT(ph, w_ap, KT, N, in_tl, T, evac, stgs, wbfs, cb=256):
        nblk = N // cb
        for b in range(nblk):
            wb = wbfs[b % len(wbfs)]
            DMA('pool', wb.t[:, :KT * cb].rearrange("p (k c) -> p k c", c=cb),
                w_ap[:, b * cb:(b + 1) * cb].rearrange("(k p) c -> p k c", p=128), [], [wb], wb.b.name)
            for o in range(cb // 128):
                for th in range(T // 512):
                    p = ph.nps()
                    for kt in range(KT):
                        MM(p.t[:], wb.t[:, kt * cb + o * 128:kt * cb + o * 128 + 128],
                           in_tl.t[:, kt * T + th * 512:kt * T + th * 512 + 512], kt == 0, kt == KT - 1, [wb, in_tl], [p])
                    evac(b * (cb // 128) + o, th, p)

    def load_norm_own(ph, ones, vec, src_d, gcol, dst, keep=None):
        sub = ExitStack()
        ph2 = Phase(); ph2.st = sub
        xh = [ph2.sb("lnx%d" % i, [128, 2048]) for i in range(4)]
        sq = ph2.sb("lnsq", [128, 8192], BF16)
        rt = ph2.sb("lnrt", [128, 512]); rstd = ph2.sb("lnrs", [128, 512])
        for th in range(2):
            for q in range(4):
                DMA('sp', xh[q].t[:].rearrange("p (k t) -> p k t", t=512),
                    src_d[q * 512:(q + 1) * 512, th * 512:(th + 1) * 512].rearrange("(k p) t -> p k t", p=128), [], [xh[q]], xh[q].b.name)
            psq = ph.nps()
            for kt in range(16):
                ACT(sq.t[:, kt * 512:(kt + 1) * 512], xh[kt // 4].t[:, (kt % 4) * 512:(kt % 4 + 1) * 512], AF.Square, [xh[kt // 4]], [sq])
            for kt in range(16):
                MM(psq.t[:], ones.t[:], sq.t[:, kt * 512:(kt + 1) * 512], kt == 0, kt == 15, [ones, sq], [psq])
            ACT(rt.t[:], psq.t[:], AF.Sqrt, [psq, vec], [rt], bias=vec.t[:, C_EPS:C_EPS + 1], scale=1.0 / 2048.0)
            RCP(rstd.t[:], rt.t[:], [rt], [rstd])
            for kt in range(16):
                STT(dst.t[:, kt * 1024 + th * 512:kt * 1024 + th * 512 + 512], xh[kt // 4].t[:, (kt % 4) * 512:(kt % 4 + 1) * 512],
                    vec.t[:, gcol + kt:gcol + kt + 1], rstd.t[:], ALU.mult, ALU.mult, [xh[kt // 4], vec, rstd], [dst])
                if keep is not None:
                    CP('pool', keep.t[:, kt * 1024 + th * 512:kt * 1024 + th * 512 + 512],
                       xh[kt // 4].t[:, (kt % 4) * 512:(kt % 4 + 1) * 512], [xh[kt // 4]], [keep])
        S.barrier()
        S.flush()
        sub.close()

    with phase() as ph:
        vec, ident, ones = consts(ph)
        ph.psring(8, "o1p")
        nown = ph.sb("nown", [128, 16384], BF16)
        load_norm_own(ph, ones, vec, xoT, C_ATTN, nown)
        DMA('pool', nown_d.rearrange("(k p) t -> p k t", p=128), nown.t[:].rearrange("p (k t) -> p k t", t=1024), [nown], [], 'nownst')
        stgs = None
        wbfs = [ph.sb("wbf%d" % i, [128, 4096], BF16) for i in range(4)]
        cq = ph.sb("cq", [128, 6 * 1024])
        linearT(ph, w_in[:, 0:768], 16, 768, nown, 1024,
                lambda ot, th, p: CP(ev_eng(), cq.t[:, ot * 1024 + th * 512:ot * 1024 + th * 512 + 512], p.t[:], [p], [cq]),
                stgs, wbfs)
        cqn = ph.sb("cqn", [128, 6 * 1024], BF16)
        sq6 = ph.sb("sq6", [128, 6 * 512], BF16)
        rt = ph.sb("rt", [128, 512]); rstd = ph.sb("rstd", [128, 512])
        for th in range(2):
            psq = ph.nps()
            for kt in range(6):
                ACT(sq6.t[:, kt * 512:(kt + 1) * 512], cq.t[:, kt * 1024 + th * 512:kt * 1024 + th * 512 + 512], AF.Square, [cq], [sq6])
            for kt in range(6):
                MM(psq.t[:], ones.t[:], sq6.t[:, kt * 512:(kt + 1) * 512], kt == 0, kt == 5, [ones, sq6], [psq])
            ACT(rt.t[:], psq.t[:], AF.Sqrt, [psq, vec], [rt], bias=vec.t[:, C_EPS:C_EPS + 1], scale=1.0 / 768.0)
            RCP(rstd.t[:], rt.t[:], [rt], [rstd])
            for kt in range(6):
                STT(cqn.t[:, kt * 1024 + th * 512:kt * 1024 + th * 512 + 512], cq.t[:, kt * 1024 + th * 512:kt * 1024 + th * 512 + 512],
                    vec.t[:, C_QN + kt:C_QN + kt + 1], rstd.t[:], ALU.mult, ALU.mult, [cq, vec, rstd], [cqn])
        cso = ph.sb("cso", [64, 2, 1024])
        DMA('sp', cso.t[:], cs[:, :, 8192:9216].rearrange("c p t -> p c t"), [], [cso], 'cso')
        qst = [ph.sb("qst%d" % i, [128, 1024], BF16) for i in range(2)]
        qpst = [ph.sb("qpst%d" % i, [64, 1024], BF16) for i in range(2)]
        q1 = ph.sb("q1", [64, 512]); q2 = ph.sb("q2", [64, 512])
        for h in range(16):
            wb = wbfs[h % 4]
            DMA('pool', wb.t[:, 0:6 * 256].rearrange("p (k c) -> p k c", c=256)[:, :, 0:192],
                w_uq[:, h * 192:(h + 1) * 192].rearrange("(k p) c -> p k c", p=128), [], [wb], wb.b.name)
            DMA('pool', wb.t[:, 0:6 * 256].rearrange("p (k c) -> p k c", c=256)[:, :, 192:256],
                w_uqsw[:, h * 64:(h + 1) * 64].rearrange("(k p) c -> p k c", p=128), [], [wb], wb.b.name)
            qs = qst[h % 2]; qp = qpst[h % 2]
            for th in range(2):
                p = ph.nps(); pp = ph.nps(); pw = ph.nps()
                for kt in range(6):
                    MM(p.t[:], wb.t[:, kt * 256:kt * 256 + 128], cqn.t[:, kt * 1024 + th * 512:kt * 1024 + th * 512 + 512], kt == 0, kt == 5, [wb, cqn], [p])
                for kt in range(6):
                    MM(pp.t[0:64, :], wb.t[:, kt * 256 + 128:kt * 256 + 192], cqn.t[:, kt * 1024 + th * 512:kt * 1024 + th * 512 + 512], kt == 0, kt == 5, [wb, cqn], [pp])
                for kt in range(6):
                    MM(pw.t[0:64, :], wb.t[:, kt * 256 + 192:kt * 256 + 256], cqn.t[:, kt * 1024 + th * 512:kt * 1024 + th * 512 + 512], kt == 0, kt == 5, [wb, cqn], [pw])
                CP('act', qs.t[:, th * 512:(th + 1) * 512], p.t[:], [p], [qs])
                TT('dve', q1.t[:], pp.t[0:64, :], cso.t[:, 0, th * 512:(th + 1) * 512], ALU.mult, [pp, cso], [q1])
                TT('dve', q2.t[:], pw.t[0:64, :], cso.t[:, 1, th * 512:(th + 1) * 512], ALU.mult, [pw, cso], [q2])
                TT('dve', qp.t[:, th * 512:(th + 1) * 512], q1.t[:], q2.t[:], ALU.add, [q1, q2], [qp])
            DMA('pool', QT_all[h], qs.t[:], [qs], [], qs.b.name + 'st')
            DMA('pool', QPE_all[h], qp.t[:], [qp], [], qp.b.name + 'st')

    with phase() as ph:
        vec, ident, ones = consts(ph)
        pS = [ph.psum("pS%d" % i, [128, 1024]) for i in range(3)]
        pO = [ph.psum("pO%d" % i, [128, 512]) for i in range(2)]
        ring = [0]
        slot = {}
        kpe = ph.sb("kpe", [64, TK], BF16)
        DMA('sp', kpe.t[:], kpe_all, [], [kpe], 'kpe')
        msk = ph.sb("msk", [128, 4, 512])
        MEMSET('pool', msk.t[:], 0.0, [msk])
        for d in range(4):
            S.op('pool', (lambda d=d: (lambda e: e.affine_select(out=msk.t[:, d, :], in_=msk.t[:, d, :], pattern=[[1, 512]],
                                                                 compare_op=ALU.is_ge, fill=-1.0e5, base=-128 * d,
                                                                 channel_multiplier=-1)))(), [msk.b], [msk.b])
        KTb = [ph.sb("KTb%d" % i, [128, TK], BF16) for i in range(2)]
        Vb = [ph.sb("Vb%d" % i, [128, 72, 128], BF16) for i in range(2)]
        Qb = [ph.sb("Qb%d" % i, [128, 1024], BF16) for i in range(2)]
        QPb = [ph.sb("QPb%d" % i, [64, 1024], BF16) for i in range(2)]
        PT = [ph.sb("PT%d" % i, [128, 1024], BF16) for i in range(3)]
        tmpm = [ph.sb("tmpm%d" % i, [128, 512]) for i in range(2)]
        rl = ph.sb("rl", [128, 1024])
        Ost = [ph.sb("Ost%d" % i, [128, 1024], BF16) for i in range(2)]
        scale = 192.0 ** -0.5
        Pacc = [ph.sb("Pacc%d" % i, [128, 1024]) for i in range(2)]
        Phi = ph.sb("Phi", [128, 1024], BF16)
        Plo = ph.sb("Plo", [128, 1024], BF16)
        KBS = list(range(60)) + list(range(64, 72))
        units = [(h, kb) for h in range(16) for kb in KBS]
        loaded = set()

        def groups(kb):
            if kb < 28:
                return [0, 1]
            if kb < 60:
                return [1]
            if kb < 68:
                return [0]
            return [1]

        def bufs(h):
            return KTb[h % 2], Vb[h % 2], Qb[h % 2], QPb[h % 2]

        def issue_qk(u):
            h, kb = units[u]
            kt_, vb, qb, qpb = bufs(h)
            if h not in loaded:
                loaded.add(h)
                DMA('sp', kt_.t[:], Kt_all[h], [], [kt_], kt_.b.name)
                DMA('sp', vb.t[:], V_all[h], [], [vb], vb.b.name)
                DMA('sp', qb.t[:], QT_all[h], [], [qb], qb.b.name)
                DMA('sp', qpb.t[:], QPE_all[h], [], [qpb], qpb.b.name)
            slot[u] = [x for x in range(3) if x not in slot.values()][0]
            ps = pS[slot[u]]
            gs = groups(kb)
            for g in gs:
                MM(ps.t[:, g * 512:(g + 1) * 512], kt_.t[:, kb * 128:(kb + 1) * 128], qb.t[:, g * 512:(g + 1) * 512], True, False, [kt_, qb], [ps])
            for g in gs:
                MM(ps.t[:, g * 512:(g + 1) * 512], kpe.t[0:64, kb * 128:(kb + 1) * 128], qpb.t[0:64, g * 512:(g + 1) * 512], False, True, [kpe, qpb], [ps])

        def issue_pv(u):
            h, kb = units[u]
            kt_, vb, qb, qpb = bufs(h)
            ps = pS[slot.pop(u)]; pt = PT[u % 3]
            pa = Pacc[h % 2]
            gs = groups(kb)
            if kb < 60:
                for g in gs:
                    bc = (C_VIS if g == 0 else C_VIS1) + kb // 4
                    ACT(pt.t[:, g * 512:(g + 1) * 512], ps.t[:, g * 512:(g + 1) * 512], AF.Exp, [ps, vec], [pt], bias=vec.t[:, bc:bc + 1], scale=scale)
            else:
                g = gs[0]
                jj = (kb - 64) % 4
                tm = tmpm[u % 2]
                TT('dve', tm.t[:], ps.t[:, g * 512:(g + 1) * 512], msk.t[:, jj, :], ALU.add, [ps, msk], [tm])
                ACT(pt.t[:, g * 512:(g + 1) * 512], tm.t[:], AF.Exp, [tm], [pt], scale=scale)
            for g in gs:
                last = (kb == 67) if g == 0 else (kb == 71)
                MM(pO[g].t[:], vb.t[:, kb, :], pt.t[:, g * 512:(g + 1) * 512], kb == 0, last, [vb, pt], [pO[g]])
            lo_, hi_ = gs[0] * 512, (gs[-1] + 1) * 512
            if kb == 0:
                CP('dve', pa.t[:], pt.t[:], [pt], [pa])
            else:
                TT('dve', pa.t[:, lo_:hi_], pt.t[:, lo_:hi_], pa.t[:, lo_:hi_], ALU.add, [pt, pa], [pa])
            if kb == 71:
                CP('dve', Phi.t[:], pa.t[:], [pa], [Phi])
                TT('dve', Plo.t[:], pa.t[:], Phi.t[:], ALU.subtract, [pa, Phi], [Plo])
                pL = pS[[x for x in range(3) if x not in slot.values()][0]]
                for g in range(2):
                    MM(pL.t[:, g * 512:(g + 1) * 512], ones.t[:], Phi.t[:, g * 512:(g + 1) * 512], True, False, [ones, Phi], [pL])
                    MM(pL.t[:, g * 512:(g + 1) * 512], ones.t[:], Plo.t[:, g * 512:(g + 1) * 512], False, True, [ones, Plo], [pL])
                os_ = Ost[h % 2]
                RCP(rl.t[:], pL.t[:], [pL], [rl])
                for g in range(2):
                    TT('dve', os_.t[:, g * 512:(g + 1) * 512], pO[g].t[:], rl.t[:, g * 512:(g + 1) * 512], ALU.mult, [pO[g], rl], [os_])
                DMA('pool', OT_all[h], os_.t[:], [os_], [], os_.b.name + 'st')

        NU = len(units)
        for u in range(NU + 2):
            if u < NU:
                issue_qk(u)
            if u >= 2:
                issue_pv(u - 2)

    with phase() as ph:
        vec, ident, ones = consts(ph)
        ph.psring(8, "o2p")
        stgs = None
        wbfs = [ph.sb("wbf%d" % i, [128, 4096], BF16) for i in range(4)]
        yg = ph.sb("yg", [128, 16384], BF16)
        DMA('sp', yg.t[:].rearrange("p (k t) -> p k t", t=1024), hown_d.rearrange("(k p) t -> p k t", p=128), [], [yg], 'ygl')
        mix = ph.sb("mix", [128, 16384], BF16)
        gas = ph.sb("gas", [128, 16384], BF16)
        gtmp = [ph.sb("gtmp%d" % i, [128, 512]) for i in range(2)]
        cgt = [0]
        with ExitStack() as sub:
            ph2 = Phase(); ph2.st = sub; ph2.ps = ph.ps
            ph2.nps = ph.nps
            nown = ph2.sb("nown", [128, 16384], BF16)
            DMA('sp', nown.t[:].rearrange("p (k t) -> p k t", t=1024), nown_d.rearrange("(k p) t -> p k t", p=128), [], [nown], 'nownl')

            def ev_yr(ot, th, p):
                g_ = gtmp[cgt[0] % 2]; cgt[0] += 1
                ACT(g_.t[:], p.t[:], AF.Gelu_apprx_tanh, [p], [g_])
                sl = slice(ot * 1024 + th * 512, ot * 1024 + th * 512 + 512)
                TT('dve', yg.t[:, sl], g_.t[:], yg.t[:, sl], ALU.mult, [g_, yg], [yg])
            linearT(ph, w_in[:, 3392:5440], 16, 2048, nown, 1024, ev_yr, stgs, wbfs)

            def ev_gr(ot, th, p):
                sl = slice(ot * 1024 + th * 512, ot * 1024 + th * 512 + 512)
                ACT(mix.t[:, sl], p.t[:], AF.Sigmoid, [p, vec], [mix], bias=vec.t[:, C_BGR + ot:C_BGR + ot + 1])
            linearT(ph, w_in[:, 7488:9536], 16, 2048, nown, 1024, ev_gr, stgs, wbfs)

            def ev_ga(ot, th, p):
                sl = slice(ot * 1024 + th * 512, ot * 1024 + th * 512 + 512)
                ACT(gas.t[:, sl], p.t[:], AF.Sigmoid, [p, vec], [gas], bias=vec.t[:, C_BGA + ot:C_BGA + ot + 1])
            linearT(ph, w_in[:, 5440:7488], 16, 2048, nown, 1024, ev_ga, stgs, wbfs)
            S.barrier()
            S.flush()

        def ev_rnn(ot, th, p):
            sl = slice(ot * 1024 + th * 512, ot * 1024 + th * 512 + 512)
            TT('dve', mix.t[:, sl], p.t[:], mix.t[:, sl], ALU.mult, [p, mix], [mix])
        linearT(ph, w_ro, 16, 2048, yg, 1024, ev_rnn, stgs, wbfs)
        S.barrier()
        DMA('sp', yg.t[:].rearrange("p (k t) -> p k t", t=1024), OT_all.rearrange("h p t -> p h t"), [], [yg], 'ygl')

        def ev_att(ot, th, p):
            g_ = gtmp[cgt[0] % 2]; cgt[0] += 1
            sl = slice(ot * 1024 + th * 512, ot * 1024 + th * 512 + 512)
            TT('dve', g_.t[:], p.t[:], gas.t[:, sl], ALU.mult, [p, gas], [g_])
            TT('dve', mix.t[:, sl], g_.t[:], mix.t[:, sl], ALU.add, [g_, mix], [mix])
        linearT(ph, w_ao, 16, 2048, yg, 1024, ev_att, stgs, wbfs)
        xr_ = [ph.sb("xres%d" % i, [128, 512]) for i in range(2)]

        def ev_out(ot, th, p):
            x_ = xr_[cgt[0] % 2]; cgt[0] += 1
            DMA('sp', x_.t[:], xoT[ot * 128:(ot + 1) * 128, th * 512:(th + 1) * 512], [], [x_], x_.b.name)
            TT('dve', x_.t[:], p.t[:], x_.t[:], ALU.add, [p, x_], [x_])
            DMA('pool', h1_d[ot * 128:(ot + 1) * 128, th * 512:(th + 1) * 512], x_.t[:], [x_], [], x_.b.name)
        linearT(ph, w_out, 16, 2048, mix, 1024, ev_out, stgs, wbfs)

    for half in range(2):
        with phase() as ph:
            vec, ident, ones = consts(ph)
            ph.psring(8, "pp")
            T0 = half * 512
            hn = ph.sb("hn", [128, 16 * 512], BF16)
            s12 = ph.sb("s12", [128, 4, 16, 128])
            tau = ph.sb("tau", [128, 32]); cbias = ph.sb("cbias", [128, 32])
            ntau = ph.sb("ntau", [128, 32]); b3 = ph.sb("b3", [128, 32])
            with ExitStack() as sub:
                ph2 = Phase(); ph2.st = sub; ph2.ps = ph.ps; ph2.nps = ph.nps
                xh = [ph2.sb("px%d" % i, [128, 2048]) for i in range(4)]
                sq = ph2.sb("psq", [128, 8192], BF16)
                rt = ph2.sb("prt", [128, 512]); rstd = ph2.sb("prs", [128, 512])
                for q in range(4):
                    DMA('sp', xh[q].t[:].rearrange("p (k t) -> p k t", t=512),
                        h1_d[q * 512:(q + 1) * 512, T0:T0 + 512].rearrange("(k p) t -> p k t", p=128), [], [xh[q]], xh[q].b.name)
                psq = ph.nps()
                rmsnorm_T(ph, ones, vec, lambda kt: (xh[kt // 4].t[:, (kt % 4) * 512:(kt % 4 + 1) * 512], xh[kt // 4]),
                          16, 512, C_FFN, hn, 2048.0, sq, rt, rstd, psq)
                stgs = None
                wbfs = [ph2.sb("wbf%d" % i, [128, 4096], BF16) for i in range(4)]
                qp = ph2.sb("qp", [128, 16 * 512], BF16)
                linearT(ph, w_pq, 16, 2048, hn, 512,
                        lambda ot, th, p: CP(ev_eng(), qp.t[:, ot * 512:(ot + 1) * 512], p.t[:], [p], [qp]), stgs, wbfs)
                skb = ph2.sb("skb", [128, 16, 128], BF16)
                DMA('pool', skb.t[:], skT, [], [skb], 'skb')
                top = ph2.sb("top", [128, 16, 16]); wk1 = ph2.sb("wk1", [128, 128])
                cand = ph2.sb("cand", [128, 8, 256]); wk2 = ph2.sb("wk2", [128, 256])
                best = ph2.sb("best", [128, 8, 16]); negm = ph2.sb("negm", [128, 8])
                e16 = ph2.sb("e16", [128, 8, 16]); Z = ph2.sb("Z", [128, 8]); lnZ = ph2.sb("lnZ", [128, 8])
                pst_top = top.t[:].ap[0][0]
                for tt in range(4):
                    for hh in range(16):
                        if hh % 4 == 0:
                            p = ph.nps()
                        MM(p.t[:, (hh % 4) * 128:(hh % 4 + 1) * 128], qp.t[:, hh * 512 + tt * 128:hh * 512 + tt * 128 + 128], skb.t[:, hh, :],
                           True, True, [qp, skb], [p])
                        if hh % 4 == 3:
                            CP(ev_eng(), s12.t[:, tt, hh - 3:hh + 1, :].rearrange("p a b -> p (a b)"), p.t[:], [p], [s12])
                    for hh in range(16):
                        S.op('dve', (lambda tt=tt, hh=hh: (lambda e: e.max(out=top.t[:, hh, 0:8], in_=s12.t[:, tt, hh, :])))(), bl([s12]), bl([top]))
                        S.op('dve', (lambda tt=tt, hh=hh: (lambda e: e.match_replace(out=wk1.t[:], in_to_replace=top.t[:, hh, 0:8],
                                                                                   in_values=s12.t[:, tt, hh, :], imm_value=-1.0e30)))(),
                             bl([s12, top]), bl([wk1]))
                        S.op('dve', (lambda hh=hh: (lambda e: e.max(out=top.t[:, hh, 8:16], in_=wk1.t[:])))(), bl([wk1]), bl([top]))
                    for h in range(8):
                        in0 = bass.AP(top.t, (2 * h) * 16, [[pst_top, 128], [1, 16], [0, 16]])
                        in1 = bass.AP(top.t, (2 * h + 1) * 16, [[pst_top, 128], [0, 16], [1, 16]])
                        TT('dve', cand.t[:, h, :].rearrange("p (a b) -> p a b", b=16), in0, in1, ALU.add, [top], [cand])
                        S.op('dve', (lambda h=h: (lambda e: e.max(out=best.t[:, h, 0:8], in_=cand.t[:, h, :])))(), bl([cand]), bl([best]))
                        S.op('dve', (lambda h=h: (lambda e: e.match_replace(out=wk2.t[:], in_to_replace=best.t[:, h, 0:8],
                                                                           in_values=cand.t[:, h, :], imm_value=-1.0e30)))(),
                             bl([cand, best]), bl([wk2]))
                        S.op('dve', (lambda h=h: (lambda e: e.max(out=best.t[:, h, 8:16], in_=wk2.t[:])))(), bl([wk2]), bl([best]))
                    TS('dve', negm.t[:], best.t[:, :, 0], -1.0, None, ALU.mult, None, [best], [negm])
                    CP('dve', tau.t[:, tt * 8:(tt + 1) * 8], best.t[:, :, 15], [best], [tau])
                    pst_b = best.t[:].ap[0][0]
                    TT('dve', e16.t[:], best.t[:], bass.AP(best.t, 0, [[pst_b, 128], [16, 8], [0, 16]]), ALU.subtract, [best], [e16])
                    ACT(e16.t[:], e16.t[:], AF.Exp, [e16], [e16])
                    S.op('dve', lambda e: e.tensor_reduce(out=Z.t[:], in_=e16.t[:], axis=mybir.AxisListType.X, op=ALU.add), bl([e16]), bl([Z]))
                    ACT(lnZ.t[:], Z.t[:], AF.Ln, [Z], [lnZ])
                    TT('dve', cbias.t[:, tt * 8:(tt + 1) * 8], negm.t[:], lnZ.t[:], ALU.subtract, [negm, lnZ], [cbias])
                    TS('dve', tau.t[:, tt * 8:(tt + 1) * 8], tau.t[:, tt * 8:(tt + 1) * 8], -1.0e-6, None, ALU.add, None, [tau], [tau])
                    TS('dve', ntau.t[:, tt * 8:(tt + 1) * 8], tau.t[:, tt * 8:(tt + 1) * 8], -1.0, None, ALU.mult, None, [tau], [ntau])
                    TT('dve', b3.t[:, tt * 8:(tt + 1) * 8], tau.t[:, tt * 8:(tt + 1) * 8], cbias.t[:, tt * 8:(tt + 1) * 8], ALU.add, [tau, cbias], [b3])
                S.barrier()
                S.flush()
            acc = ph.sb("acc", [128, 16 * 512])
            ubf2 = [ph.sb("ubf%d" % i, [128, 16, 512], BF16) for i in range(1)]
            vbf2 = [ph.sb("vbf%d" % i, [128, 4, 2048], BF16) for i in range(2)]
            gate = [ph.sb("gate%d" % i, [128, 8, 512], BF16) for i in range(2)]
            sc = [ph.sb("sc%d" % i, [128, 512]) for i in range(4)]
            ex = [ph.sb("ex%d" % i, [128, 512]) for i in range(4)]
            pst_s = s12.t[:].ap[0][0]
            GT = [ph.sb("GT%d" % i, [128, 4, 512], BF16) for i in range(2)]
            ge = [ph.sb("ge%d" % i, [128, 512]) for i in range(2)]
            WT = [ph.sb("WT%d" % i, [128, 4, 512], BF16) for i in range(4)]
            pG = ph.ps[0:2]; pA = ph.ps[2:4]; pD = ph.ps[4:8]
            NG = 32
            deferred = []
            gu = [0]
            kk = [0]
            pend = []

            def flush_pend(upto=None):
                while pend and (upto is None or pend[0][5] <= upto):
                    g2, h2, s2_, e2_, t2, _ = pend.pop(0)
                    STT(g2.t[:, h2, :], s2_.t[:], tau.t[:, t2 * 8 + h2:t2 * 8 + h2 + 1], e2_.t[:], ALU.is_ge, ALU.mult, [s2_, tau, e2_], [g2])

            def gate_unit(G, u):
                tt, h = u // 8, u % 8
                k = kk[0] % 4
                kk[0] += 1
                s_ = sc[k]; e_ = ex[k]
                gt_ = gate[(G * 4 + tt) % 2]
                in0 = bass.AP(s12.t, tt * 2048 + (2 * h + 1) * 128, [[pst_s, 128], [0, 4], [1, 128]])
                in1 = bass.AP(s12.t, tt * 2048 + (2 * h) * 128 + G * 4, [[pst_s, 128], [1, 4], [0, 128]])
                TT('dve', s_.t[:].rearrange("p (a b) -> p a b", b=128), in0, in1, ALU.add, [s12], [s_])
                c_ = tt * 8 + h
                if h >= 5:
                    S.op('act', lambda e: e.activation(out=e_.t[:], in_=s_.t[:], func=AF.Prelu, bias=ntau.t[:, c_:c_ + 1], scale=1.0, alpha=1.0e9),
                         bl([s_, ntau]), bl([e_]))
                    ACT(gt_.t[:, h, :], e_.t[:], AF.Exp, [e_, b3], [gt_], bias=b3.t[:, c_:c_ + 1])
                else:
                    ACT(e_.t[:], s_.t[:], AF.Exp, [s_, cbias], [e_], bias=cbias.t[:, c_:c_ + 1])
                    pend.append((gt_, h, s_, e_, tt, kk[0]))
                flush_pend(upto=kk[0] - 2)

            def ident_mm(G, tt):
                gt_ = gate[(G * 4 + tt) % 2]
                pg = pG[(G * 4 + tt) % 2]
                for i in range(4):
                    for hh in range(8):
                        MM(pg.t[:, i * 128:(i + 1) * 128], gt_.t[:, hh, i * 128:(i + 1) * 128], ident.t[:], hh == 0, hh == 7, [gt_, ident], [pg])
                CP('act', GT[G % 2].t[:, :, tt * 128:(tt + 1) * 128], pg.t[:].rearrange("p (a b) -> p a b", b=128), [pg], [GT[G % 2]])

            def load_u(G):
                ub = ubf2[0]
                for q in range(4):
                    DMA('pool', ub.t[:, 4 * q:4 * q + 4, :],
                        puT[q * 512:(q + 1) * 512, G * 512:(G + 1) * 512].rearrange("(k p) c -> p k c", p=128), [], [ub], ub.b.name)

            def load_v(G):
                vb = vbf2[G % 2]
                for d in range(4):
                    DMA('pool', vb.t[:, d, :], pv[G * 512 + d * 128:G * 512 + d * 128 + 128, :], [], [vb], vb.b.name)

            def act_chain(G, i):
                pa = pA[i % 2]
                ub = ubf2[0]
                for kt in range(16):
                    MM(pa.t[:], ub.t[:, kt, i * 128:(i + 1) * 128], hn.t[:, kt * 512:(kt + 1) * 512], kt == 0, kt == 15, [ub, hn], [pa])
                g_ = ge[i % 2]
                ACT(g_.t[:], pa.t[:], AF.Gelu_apprx_tanh, [pa], [g_])
                TT('pool', WT[G % 4].t[:, i, :], g_.t[:], GT[G % 2].t[:, i, :], ALU.mult, [g_, GT[G % 2]], [WT[G % 4]])

            def v_chain(Ga, dt):
                pd = pD[dt % 4]
                n = 0
                for Gx in (Ga, Ga + 1):
                    vb = vbf2[Gx % 2]; wt = WT[Gx % 4]
                    for d in range(4):
                        MM(pd.t[:], vb.t[:, d, dt * 128:(dt + 1) * 128], wt.t[:, d, :], n == 0, n == 7, [vb, wt], [pd])
                        n += 1

                def add():
                    if Ga == 0:
                        CP('dve', acc.t[:, dt * 512:(dt + 1) * 512], pd.t[:], [pd], [acc])
                    else:
                        TT('dve', acc.t[:, dt * 512:(dt + 1) * 512], pd.t[:], acc.t[:, dt * 512:(dt + 1) * 512], ALU.add, [pd, acc], [acc])
                deferred.append((gu[0] + 3, add))

            def run_deferred(force=False):
                while deferred and (force or deferred[0][0] <= gu[0]):
                    deferred.pop(0)[1]()

            load_u(0); load_v(0); load_v(1)
            for G in range(NG + 1):
                for u in range(32):
                    gu[0] = G * 32 + u
                    if G < NG:
                        gate_unit(G, u)
                    elif u == 0:
                        flush_pend()
                    run_deferred()
                    if u % 8 == 4:
                        tq = u // 8 - 1
                        if tq >= 0 and G < NG:
                            ident_mm(G, tq)
                        elif tq < 0 and G >= 1:
                            ident_mm(G - 1, 3)
                    if G >= 1:
                        Gp = G - 1
                        if u in (6, 8, 10, 12):
                            act_chain(Gp, (u - 6) // 2)
                        if u == 13 and G < NG:
                            load_u(G)
                        if G % 2 == 0:
                            if 14 <= u < 30:
                                v_chain(G - 2, u - 14)
                            if u == 30:
                                if G < NG:
                                    load_v(G)
                                if G + 1 < NG:
                                    load_v(G + 1)
            run_deferred(force=True)
            hx = [ph.sb("hx%d" % i, [128, 512]) for i in range(2)]
            for dt in range(16):
                x_ = hx[dt % 2]
                DMA('sp', x_.t[:], h1_d[dt * 128:(dt + 1) * 128, T0:T0 + 512], [], [x_], x_.b.name)
                TT('dve', x_.t[:], x_.t[:], acc.t[:, dt * 512:(dt + 1) * 512], ALU.add, [x_, acc], [x_])
                DMA('pool', h2_d[dt * 128:(dt + 1) * 128, T0:T0 + 512], x_.t[:], [x_], [], x_.b.name)

    with phase() as ph:
        vec, ident, ones = consts(ph)
        ph.psring(8, "ep")
        h2 = ph.sb("h2", [128, 16384])
        hn3 = ph.sb("hn3", [128, 16384], BF16)
        load_norm_own(ph, ones, vec, h2_d, C_PLE, hn3, keep=h2)
        stgs = None
        wbfs = [ph.sb("wbf%d" % i, [128, 2048], BF16) for i in range(4)]
        gsb = ph.sb("gsb", [128, 16384], BF16)

        def ev_g(ot, th, p):
            sl = slice(ot * 1024 + th * 512, ot * 1024 + th * 512 + 512)
            ACT(gsb.t[:, sl], p.t[:], AF.Sigmoid, [p], [gsb])
        linearT(ph, w_pg, 16, 2048, hn3, 1024, ev_g, stgs, wbfs, cb=128)
        pb = ph.sb("pb", [128, 2048], BF16)
        DMA('pool', pb.t[:].rearrange("p (k t) -> p k t", t=1024), poT.rearrange("(k p) t -> p k t", p=128), [], [pb], 'pb')
        et = [ph.sb("et%d" % i, [128, 512]) for i in range(2)]
        ce = [0]

        def ev_p(ot, th, p):
            t_ = et[ce[0] % 2]; ce[0] += 1
            sl = slice(ot * 1024 + th * 512, ot * 1024 + th * 512 + 512)
            TT('dve', t_.t[:], p.t[:], gsb.t[:, sl], ALU.mult, [p, gsb], [t_])
            TT('dve', h2.t[:, sl], t_.t[:], h2.t[:, sl], ALU.add, [t_, h2], [h2])
        linearT(ph, w_pp, 2, 2048, pb, 1024, ev_p, stgs, wbfs, cb=128)
        sq = ph.sb("fsq", [128, 8192], BF16)
        rt = ph.sb("frt", [128, 512]); rstd = ph.sb("frs", [128, 512])
        ob = [ph.sb("ob%d" % i, [128, 512]) for i in range(2)]
        for th in range(2):
            psq = ph.nps()
            for kt in range(16):
                ACT(sq.t[:, kt * 512:(kt + 1) * 512], h2.t[:, kt * 1024 + th * 512:kt * 1024 + th * 512 + 512], AF.Square, [h2], [sq])
            for kt in range(16):
                MM(psq.t[:], ones.t[:], sq.t[:, kt * 512:(kt + 1) * 512], kt == 0, kt == 15, [ones, sq], [psq])
            ACT(rt.t[:], psq.t[:], AF.Sqrt, [psq, vec], [rt], bias=vec.t[:, C_EPS:C_EPS + 1], scale=1.0 / 2048.0)
            RCP(rstd.t[:], rt.t[:], [rt], [rstd])
            for kt in range(16):
                o_ = ob[kt % 2]
                STT(o_.t[:], h2.t[:, kt * 1024 + th * 512:kt * 1024 + th * 512 + 512], vec.t[:, C_FIN + kt:C_FIN + kt + 1], rstd.t[:],
                    ALU.mult, ALU.mult, [h2, vec, rstd], [o_])
                DMA('sp', outT[kt * 128:(kt + 1) * 128, th * 512:(th + 1) * 512], o_.t[:], [o_], [], o_.b.name)

    topstack.close()
    return nc


def _cm(v, n):
    return np.ascontiguousarray(np.asarray(v, np.float32).reshape(n, 128).T)


def prep_inputs(inp):
    f = lambda a: np.ascontiguousarray(np.asarray(a, dtype=np.float32))
    x = f(inp['x'])[0]
    p = f(inp['p'])[0, 0]
    xT = np.ascontiguousarray(x.T)
    w_in = f(inp['w_in'])[0]
    perm = np.concatenate([np.arange(32, 64), np.arange(0, 32)])
    w_krsw = np.ascontiguousarray(w_in[:, 1280:1344][:, perm])
    w_uq = f(inp['w_uq'])[0]
    w_uqsw = np.ascontiguousarray(w_uq.reshape(768, 16, 192)[:, :, 128:][:, :, perm].reshape(768, 1024))
    half = 32
    inv_freq = (1.0 / (np.float32(10000.0) ** (np.arange(half, dtype=np.float32) / np.float32(half)))).astype(np.float32)

    def tables(pos):
        ang = pos.astype(np.float32)[:, None] * inv_freq[None, :]
        c = np.cos(ang).astype(np.float32).T
        s = np.sin(ang).astype(np.float32).T
        return np.concatenate([c, c], 0), np.concatenate([-s, s], 0)
    vec_common = np.zeros((128, NV), np.float32)
    vec_common[:, C_ATTN:C_ATTN + 16] = _cm(inp['attn_norm'][0], 16)
    vec_common[:, C_QN:C_QN + 6] = _cm(inp['q_norm'][0], 6)
    vec_common[:, C_KVN:C_KVN + 4] = _cm(inp['kv_norm'][0], 4)
    vec_common[:, C_CB:C_CB + 16] = _cm(inp['conv_b'][0], 16)
    cw = np.asarray(inp['conv_w'], np.float32)[0]
    vec_common[:, C_CW:C_CW + 64] = cw.T.reshape(16, 128, 4).transpose(1, 0, 2).reshape(128, 64)
    vec_common[:, C_BA:C_BA + 16] = _cm(np.asarray(inp['b_rg_a'])[0].reshape(-1), 16)
    vec_common[:, C_BX:C_BX + 16] = _cm(np.asarray(inp['b_rg_x'])[0].reshape(-1), 16)
    vec_common[:, C_LAM:C_LAM + 16] = _cm(inp['lru_lambda'][0], 16)
    bg = np.asarray(inp['b_gate'], np.float32)[0]
    vec_common[:, C_BGA:C_BGA + 16] = _cm(bg[:2048], 16)
    vec_common[:, C_BGR:C_BGR + 16] = _cm(bg[2048:], 16)
    vec_common[:, C_FFN:C_FFN + 16] = _cm(inp['ffn_norm'][0], 16)
    vec_common[:, C_PLE:C_PLE + 16] = _cm(inp['ple_norm'][0], 16)
    vec_common[:, C_FIN:C_FIN + 16] = _cm(inp['final_norm'], 16)
    vec_common[:, C_EPS] = 1e-6
    vec_common[:, C_ONE] = 1.0
    vec_common[:, C_M1] = -1.0
    shared = {
        'xT': xT, 'w_in': w_in, 'w_krsw': w_krsw, 'w_uq': w_uq, 'w_uqsw': w_uqsw,
        'w_ukv': f(inp['w_ukv'])[0], 'w_attn_o': f(inp['w_attn_o'])[0],
        'w_rg_a': f(inp['w_rg_a'])[0], 'w_rg_x': f(inp['w_rg_x'])[0],
        'w_rnn_o': f(inp['w_rnn_o'])[0], 'w_out': f(inp['w_out'])[0], 'w_peer_q': f(inp['w_peer_q'])[0],
        'skT': np.ascontiguousarray(f(inp['peer_subkeys'])[0].reshape(16, 128, 128).transpose(2, 0, 1)),
        'puT': np.ascontiguousarray(f(inp['peer_u'])[0].T), 'pv': f(inp['peer_v'])[0],
        'w_ple_gate': f(inp['w_ple_gate'])[0], 'w_ple_proj': f(inp['w_ple_proj'])[0],
    }
    cg, sg = tables(np.arange(8192))
    maps = []
    for c in range(8):
        ca, cb_ = c, 15 - c
        own = np.concatenate([np.arange(512 * ca, 512 * ca + 512), np.arange(512 * cb_, 512 * cb_ + 512)])
        m = dict(shared)
        m['xoT'] = np.ascontiguousarray(xT[:, own])
        m['poT'] = np.ascontiguousarray(p[own].T)
        v = vec_common.copy()
        for j in range(16):
            v[:, C_VIS + j] = 0.0 if j < ca else NEG
            v[:, C_VIS1 + j] = 0.0 if j < cb_ else NEG
            v[:, C_OWN + 2 * j + 0] = 1.0 if j == ca else 0.0
            v[:, C_OWN + 2 * j + 1] = 1.0 if j == cb_ else 0.0
        m['vecs'] = v
        cs_ = np.zeros((2, 64, TK), np.float32)
        cs_[0, :, :8192] = cg; cs_[1, :, :8192] = sg
        cs_[0, :, 8192:] = cg[:, own]; cs_[1, :, 8192:] = sg[:, own]
        m['cs'] = cs_
        maps.append(m)
    return maps


def kernel(**inputs):
    maps = prep_inputs(inputs)
    nc = build_nc(False)
    res = run_bass_kernel_spmd(nc, maps, core_ids=list(range(8)))
    out = np.empty((8192, 2048), np.float32)
    for c, r in enumerate(res.results):
        o = np.asarray(r["outT"])
        out[512 * c:512 * c + 512] = o[:, :512].T
        out[512 * (15 - c):512 * (15 - c) + 512] = o[:, 512:].T
    return out[None]
```
